# Optimizing a Trainium2 kernel written in Bass

```python
import math
import jax
import jax.numpy as jnp
from jax import lax
import numpy as np

D_MODEL = 1024
BATCH = 16
SEQ = 2048
DEPTH = 1
DEC_BATCH = 128
DEC_SEQ = 8
PAST_LEN = 8192
PAGE_SIZE = 128

A_GROUPS = ((128, 1), (512, 4), (2048, 16))
A_N_GROUPS = len(A_GROUPS)
A_HEADS = 4
A_HEAD_DIM = 64
A_WIDTH = A_N_GROUPS * A_HEADS * A_HEAD_DIM
A_OUT = A_HEADS * A_HEAD_DIM
B_HEADS = 4
B_DK = D_MODEL // 16
B_DV = D_MODEL // 8
B_KW = B_HEADS * B_DK
B_VW = B_HEADS * B_DV
B_RANK = 16
B_TAU = 16.0
B_CHUNK = 64
C_HEADS = 4
C_HEAD_DIM = D_MODEL // 8
C_WIDTH = C_HEADS * C_HEAD_DIM
N_MEM = 256
N_BRANCH = 3
IN_SPLITS = (A_WIDTH, A_WIDTH, A_WIDTH, B_KW, B_KW, B_VW, B_VW, B_RANK, C_WIDTH, N_BRANCH * D_MODEL)
IN_WIDTH = sum(IN_SPLITS)
N_EXPERT_GROUPS = 4
EXPERTS_PER_GROUP = 8
N_EXPERTS = N_EXPERT_GROUPS * EXPERTS_PER_GROUP
TOP_K = 2
D_EXPERT = D_MODEL // 4
MOE_BLOCK = 128
EPS = 1e-6

kernel_name = 'hybrid_dilated_gla_mem_hmoe_step'


def _rmsnorm(x, g):
    xf = x.astype(jnp.float32)
    y = xf * lax.rsqrt(jnp.mean(xf * xf, axis=-1, keepdims=True) + EPS)
    return (y * g.astype(jnp.float32)).astype(x.dtype)


def _mixer_inputs(x, norm_mix, w_in, qn_a, kn_a, qn_c, gla_gate_up, gla_gate_bias):
    Bx, T, _ = x.shape
    xn = _rmsnorm(x, norm_mix)
    h = jnp.einsum('btd,de->bte', xn, w_in)
    offsets = [int(o) for o in np.cumsum(IN_SPLITS)[:-1]]
    qa, ka, va, qb, kb, vb, rb, ab, qc, gl = jnp.split(h, offsets, axis=-1)
    shp_a = (Bx, T, A_N_GROUPS, A_HEADS, A_HEAD_DIM)
    qa = _rmsnorm(qa.reshape(shp_a), qn_a) * (A_HEAD_DIM ** -0.5)
    ka = _rmsnorm(ka.reshape(shp_a), kn_a)
    va = va.reshape(shp_a)
    qb = qb.reshape(Bx, T, B_HEADS, B_DK) * (B_DK ** -0.5)
    kb = kb.reshape(Bx, T, B_HEADS, B_DK)
    vb = vb.reshape(Bx, T, B_HEADS, B_DV)
    z = jnp.einsum('btr,rk->btk', ab, gla_gate_up).astype(jnp.float32) + gla_gate_bias.astype(jnp.float32)
    log_a = (jax.nn.log_sigmoid(z) / B_TAU).reshape(Bx, T, B_HEADS, B_DK)
    qc = _rmsnorm(qc.reshape(Bx, T, C_HEADS, C_HEAD_DIM), qn_c) * (C_HEAD_DIM ** -0.5)
    gates = jax.nn.sigmoid(gl.astype(jnp.float32)).reshape(Bx, T, N_BRANCH, D_MODEL)
    return qa, ka, va, qb, kb, vb, log_a, rb, qc, gates


def _dilated_band(q, k, v, J, r):
    B, S, H, D = q.shape
    M = S // r
    nb = -(-M // J)
    Mp = nb * J

    def sub(t):
        t = jnp.swapaxes(t.reshape(B, M, r, H, D), 1, 2)
        t = jnp.pad(t, ((0, 0), (0, 0), (0, Mp - M), (0, 0), (0, 0)))
        return t.reshape(B, r, nb, J, H, D)

    def band(t):
        prev = jnp.pad(t, ((0, 0), (0, 0), (1, 0), (0, 0), (0, 0), (0, 0)))[:, :, :-1]
        return jnp.concatenate([prev, t], axis=3)

    qb, kb, vb = sub(q), band(sub(k)), band(sub(v))
    s = jnp.einsum('brnqhd,brnkhd->brnhqk', qb, kb).astype(jnp.float32)
    qi = jnp.arange(J)[:, None] + J
    ki = jnp.arange(2 * J)[None, :]
    blk = jnp.arange(nb)[:, None, None]
    valid = (ki <= qi) & (ki >= qi - J) & (blk * J + ki[None] - J >= 0)
    s = jnp.where(valid[None, None, :, None], s, -jnp.inf)
    m = jnp.max(s, axis=-1, keepdims=True)
    p = jnp.exp(s - m)
    l = jnp.sum(p, axis=-1)
    o = jnp.einsum('brnhqk,brnkhd->brnqhd', p, vb.astype(jnp.float32))
    o = o / jnp.swapaxes(l, 3, 4)[..., None]
    lse = jnp.swapaxes(m[..., 0] + jnp.log(l), 3, 4)

    def unsub(t):
        t = t.reshape((B, r, Mp) + t.shape[4:])[:, :, :M]
        t = jnp.swapaxes(t, 1, 2)
        return t.reshape((B, S) + t.shape[3:])

    return unsub(o), unsub(lse)


def _combine_groups(outs, lses):
    o = jnp.stack(outs, axis=0)
    wts = jax.nn.softmax(jnp.stack(lses, axis=0), axis=0)
    return jnp.sum(wts[..., None] * o, axis=0)


def _dilated_prompt(q, k, v):
    T = q.shape[1]
    outs, lses, bufs = [], [], []
    for g, (w, r) in enumerate(A_GROUPS):
        o, lse = _dilated_band(q[:, :, g], k[:, :, g], v[:, :, g], w // r, r)
        outs.append(o)
        lses.append(lse)
        wb = min(w, T)
        bufs.append(jnp.stack([k[:, T - wb:, g], v[:, T - wb:, g]], axis=2))
    return _combine_groups(outs, lses), bufs


def _dilated_sample(q, k, v, bufs):
    L = q.shape[1]
    outs, lses, new_bufs = [], [], []
    for g, ((w, r), buf) in enumerate(zip(A_GROUPS, bufs)):
        Wb = buf.shape[1]
        kv_new = jnp.stack([k[:, :, g], v[:, :, g]], axis=2)
        kvc = jnp.concatenate([buf.astype(kv_new.dtype), kv_new], axis=1)
        J = w // r
        idx = Wb + jnp.arange(L)[:, None] - r * jnp.arange(J + 1)[None, :]
        valid = idx >= 0
        kvg = kvc[:, jnp.maximum(idx, 0)]
        s = jnp.einsum('blhd,bljhd->blhj', q[:, :, g], kvg[:, :, :, 0]).astype(jnp.float32)
        s = jnp.where(valid[None, :, None, :], s, -jnp.inf)
        m = jnp.max(s, axis=-1, keepdims=True)
        p = jnp.exp(s - m)
        l = jnp.sum(p, axis=-1)
        o = jnp.einsum('blhj,bljhd->blhd', p, kvg[:, :, :, 1].astype(jnp.float32)) / l[..., None]
        outs.append(o)
        lses.append(m[..., 0] + jnp.log(l))
        new_bufs.append(kvc[:, kvc.shape[1] - Wb:])
    return _combine_groups(outs, lses), new_bufs


def _gla(q, k, v, log_a, s0):
    B, T, H, dk = q.shape
    C = math.gcd(T, B_CHUNK)
    n = T // C

    def to_chunks(t):
        return t.astype(jnp.float32).reshape(B, n, C, H, t.shape[-1]).transpose(1, 0, 3, 2, 4)

    causal = jnp.tril(jnp.ones((C, C), dtype=bool))

    def step(S, inp):
        qc, kc, vc, ac = inp
        b = jnp.cumsum(ac, axis=2)
        o_inter = jnp.einsum('bhcd,bhdv->bhcv', qc * jnp.exp(b), S)
        diff = b[:, :, :, None, :] - b[:, :, None, :, :]
        decay = jnp.exp(jnp.where(causal[None, None, :, :, None], diff, -jnp.inf))
        att = jnp.einsum('bhtd,bhsd,bhtsd->bhts', qc, kc, decay)
        o_intra = jnp.einsum('bhts,bhsv->bhtv', att, vc)
        b_last = b[:, :, -1:, :]
        S_new = jnp.exp(b_last[:, :, 0, :])[..., None] * S + jnp.einsum('bhsd,bhsv->bhdv', kc * jnp.exp(b_last - b), vc)
        return S_new, o_inter + o_intra

    S_fin, o = lax.scan(step, s0.astype(jnp.float32), (to_chunks(q), to_chunks(k), to_chunks(v), to_chunks(log_a)))
    o = o.transpose(1, 0, 3, 2, 4).reshape(B, T, H, v.shape[-1])
    return o, S_fin


def _mem_kv(mem, mem_norm, w_mem_kv, kn_c):
    Bm, N, _ = mem.shape
    kv = jnp.einsum('bnd,de->bne', _rmsnorm(mem, mem_norm), w_mem_kv).reshape(Bm, N, 2, C_HEADS, C_HEAD_DIM)
    k = _rmsnorm(kv[:, :, 0], kn_c)
    return jnp.stack([k, kv[:, :, 1]], axis=2)


def _cross(q, mem_kv):
    s = jnp.einsum('bthd,bnhd->bhtn', q, mem_kv[:, :, 0].astype(q.dtype)).astype(jnp.float32)
    p = jax.nn.softmax(s, axis=-1)
    return jnp.einsum('bhtn,bnhd->bthd', p, mem_kv[:, :, 1].astype(jnp.float32))


def _expert_dispatch(xt, eidx, ew, w_exp_gate, w_exp_up, w_exp_down):
    N, D = xt.shape
    A = N * TOP_K
    flat_e = eidx.reshape(-1)
    order = jnp.argsort(flat_e)
    se = flat_e[order]
    tok = order // TOP_K
    sw = ew.reshape(-1)[order]
    counts = jnp.bincount(flat_e, length=N_EXPERTS)
    padded = (counts + MOE_BLOCK - 1) // MOE_BLOCK * MOE_BLOCK
    pad_end = jnp.cumsum(padded)
    pad_start = pad_end - padded
    start = jnp.cumsum(counts) - counts
    dest = pad_start[se] + jnp.arange(A) - start[se]
    n_blocks = -(-A // MOE_BLOCK) + N_EXPERTS
    P = n_blocks * MOE_BLOCK
    xs = jnp.zeros((P, D), xt.dtype).at[dest].set(xt[tok])
    block_e = jnp.minimum(jnp.searchsorted(pad_end, jnp.arange(n_blocks) * MOE_BLOCK, side='right'), N_EXPERTS - 1)

    def run(args):
        xb, e = args
        hid = jax.nn.silu(xb @ w_exp_gate[e]) * (xb @ w_exp_up[e])
        return hid @ w_exp_down[e]

    ys = lax.map(run, (xs.reshape(n_blocks, MOE_BLOCK, D), block_e))
    yt = ys.reshape(P, D)[dest] * sw[:, None].astype(xt.dtype)
    return jnp.zeros((N, D), xt.dtype).at[tok].add(yt)


def _hmoe(h, norm_ffn, w_router_group, b_router_group, w_router_expert, b_router_expert, w_exp_gate, w_exp_up, w_exp_down):
    Bx, T, D = h.shape
    xt = _rmsnorm(h, norm_ffn).reshape(Bx * T, D)
    g_logit = (xt @ w_router_group).astype(jnp.float32) + b_router_group.astype(jnp.float32)
    g_prob = jax.nn.softmax(g_logit, axis=-1)
    grp = jnp.argmax(g_logit, axis=-1)
    g_w = jnp.take_along_axis(g_prob, grp[:, None], axis=-1)
    e_logit = (xt @ w_router_expert).astype(jnp.float32) + b_router_expert.astype(jnp.float32)
    e_logit = e_logit.reshape(-1, N_EXPERT_GROUPS, EXPERTS_PER_GROUP)
    e_logit = jnp.take_along_axis(e_logit, grp[:, None, None], axis=1)[:, 0]
    top_v, top_i = lax.top_k(e_logit, TOP_K)
    w = jax.nn.softmax(top_v, axis=-1) * g_w
    eidx = grp[:, None] * EXPERTS_PER_GROUP + top_i
    y = _expert_dispatch(xt, eidx, w, w_exp_gate, w_exp_up, w_exp_down)
    return h + y.reshape(Bx, T, D)


def _merge_and_ffn(x, oa, ob, rb, oc, gates, gla_norm, w_branch_a, w_branch_b, w_branch_c, w_out,
                   norm_ffn, w_router_group, b_router_group, w_router_expert, b_router_expert,
                   w_exp_gate, w_exp_up, w_exp_down):
    Bx, T, _ = x.shape
    mu = jnp.mean(ob, axis=-1, keepdims=True)
    var = jnp.mean(jnp.square(ob - mu), axis=-1, keepdims=True)
    obn = (ob - mu) * lax.rsqrt(var + EPS) * gla_norm.astype(jnp.float32)
    ob2 = obn.reshape(Bx, T, B_VW) * jax.nn.silu(rb.astype(jnp.float32))
    pa = jnp.einsum('btk,kd->btd', oa.reshape(Bx, T, A_OUT).astype(x.dtype), w_branch_a)
    pb = jnp.einsum('btk,kd->btd', ob2.astype(x.dtype), w_branch_b)
    pc = jnp.einsum('btk,kd->btd', oc.reshape(Bx, T, C_WIDTH).astype(x.dtype), w_branch_c)
    merged = gates[:, :, 0] * pa + gates[:, :, 1] * pb + gates[:, :, 2] * pc
    h = x + jnp.einsum('btd,de->bte', merged.astype(x.dtype), w_out)
    return _hmoe(h, norm_ffn, w_router_group, b_router_group, w_router_expert, b_router_expert, w_exp_gate, w_exp_up, w_exp_down)


def setup_inputs(seed: int = 0) -> dict:
    key = jax.random.key(seed)
    keys = iter(jax.random.split(key, 40))

    def nrm(shape, scale=1.0):
        return jax.random.normal(next(keys), shape, jnp.float32) * scale

    def gain(n):
        return 1.0 + nrm((n,), 0.01)

    wb = [min(w, PAST_LEN) for w, _ in A_GROUPS]
    return {
        'x_prompt': nrm((BATCH, SEQ, D_MODEL)),
        'x_sample': nrm((DEC_BATCH, DEC_SEQ, D_MODEL)),
        'mem_prompt': nrm((BATCH, N_MEM, D_MODEL)),
        'cache_win1': nrm((DEC_BATCH, wb[0], 2, A_HEADS, A_HEAD_DIM)),
        'cache_win2': nrm((DEC_BATCH, wb[1], 2, A_HEADS, A_HEAD_DIM)),
        'cache_win3': nrm((DEC_BATCH, wb[2], 2, A_HEADS, A_HEAD_DIM)),
        'state_gla': nrm((DEC_BATCH, B_HEADS, B_DK, B_DV)),
        'cache_mem': nrm((DEC_BATCH, N_MEM, 2, C_HEADS, C_HEAD_DIM)),
        'norm_mix': gain(D_MODEL),
        'w_in': nrm((D_MODEL, IN_WIDTH), D_MODEL ** -0.5),
        'qn_a': gain(A_HEAD_DIM),
        'kn_a': gain(A_HEAD_DIM),
        'qn_c': gain(C_HEAD_DIM),
        'kn_c': gain(C_HEAD_DIM),
        'gla_gate_up': nrm((B_RANK, B_KW), B_RANK ** -0.5),
        'gla_gate_bias': nrm((B_KW,), 0.1),
        'gla_norm': gain(B_DV),
        'mem_norm': gain(D_MODEL),
        'w_mem_kv': nrm((D_MODEL, 2 * C_WIDTH), D_MODEL ** -0.5),
        'w_branch_a': nrm((A_OUT, D_MODEL), A_OUT ** -0.5),
        'w_branch_b': nrm((B_VW, D_MODEL), B_VW ** -0.5),
        'w_branch_c': nrm((C_WIDTH, D_MODEL), C_WIDTH ** -0.5),
        'w_out': nrm((D_MODEL, D_MODEL), D_MODEL ** -0.5),
        'norm_ffn': gain(D_MODEL),
        'w_router_group': nrm((D_MODEL, N_EXPERT_GROUPS), D_MODEL ** -0.5),
        'b_router_group': nrm((N_EXPERT_GROUPS,), 0.01),
        'w_router_expert': nrm((D_MODEL, N_EXPERTS), D_MODEL ** -0.5),
        'b_router_expert': nrm((N_EXPERTS,), 0.01),
        'w_exp_gate': nrm((N_EXPERTS, D_MODEL, D_EXPERT), D_MODEL ** -0.5),
        'w_exp_up': nrm((N_EXPERTS, D_MODEL, D_EXPERT), D_MODEL ** -0.5),
        'w_exp_down': nrm((N_EXPERTS, D_EXPERT, D_MODEL), D_EXPERT ** -0.5),
    }


def reference(x_prompt, x_sample, mem_prompt, cache_win1, cache_win2, cache_win3, state_gla, cache_mem,
              norm_mix, w_in, qn_a, kn_a, qn_c, kn_c, gla_gate_up, gla_gate_bias, gla_norm, mem_norm,
              w_mem_kv, w_branch_a, w_branch_b, w_branch_c, w_out, norm_ffn, w_router_group, b_router_group,
              w_router_expert, b_router_expert, w_exp_gate, w_exp_up, w_exp_down):
    qa, ka, va, qb, kb, vb, log_a, rb, qc, gates = _mixer_inputs(
        x_prompt, norm_mix, w_in, qn_a, kn_a, qn_c, gla_gate_up, gla_gate_bias)
    oa, (win1_p, win2_p, win3_p) = _dilated_prompt(qa, ka, va)
    s0 = jnp.zeros((x_prompt.shape[0], B_HEADS, B_DK, B_DV), jnp.float32)
    ob, gla_p = _gla(qb, kb, vb, log_a, s0)
    mem_p = _mem_kv(mem_prompt, mem_norm, w_mem_kv, kn_c)
    oc = _cross(qc, mem_p)
    y_prompt = _merge_and_ffn(x_prompt, oa, ob, rb, oc, gates, gla_norm, w_branch_a, w_branch_b, w_branch_c,
                              w_out, norm_ffn, w_router_group, b_router_group, w_router_expert,
                              b_router_expert, w_exp_gate, w_exp_up, w_exp_down)
    qa, ka, va, qb, kb, vb, log_a, rb, qc, gates = _mixer_inputs(
        x_sample, norm_mix, w_in, qn_a, kn_a, qn_c, gla_gate_up, gla_gate_bias)
    oa, (win1_s, win2_s, win3_s) = _dilated_sample(qa, ka, va, (cache_win1, cache_win2, cache_win3))
    ob, gla_s = _gla(qb, kb, vb, log_a, state_gla)
    oc = _cross(qc, cache_mem)
    y_sample = _merge_and_ffn(x_sample, oa, ob, rb, oc, gates, gla_norm, w_branch_a, w_branch_b, w_branch_c,
                              w_out, norm_ffn, w_router_group, b_router_group, w_router_expert,
                              b_router_expert, w_exp_gate, w_exp_up, w_exp_down)
    return (y_prompt, y_sample, win1_p, win2_p, win3_p, gla_p, mem_p, win1_s, win2_s, win3_s, gla_s)
```

```python
import contextlib
import numpy as np
import concourse.bass as bass
import concourse.mybir as mybir
from concourse.bass_utils import run_bass_kernel_spmd

F32 = mybir.dt.float32
BF16 = mybir.dt.bfloat16
I32 = mybir.dt.int32
AF = mybir.ActivationFunctionType
ALU = mybir.AluOpType
AX = mybir.AxisListType

NCORES = 8
D = 1024
SEQ = 2048
NPS = 2
NSB = 16
LS = 8
NTOK = NPS * SEQ + NSB * LS
NTILE = NTOK // 128
CAP = 512
NEXP = 32
EPS = 1e-6
NEG = -30000.0
WIN = (128, 512, 2048)
DIL = (1, 4, 16)
C_QA, C_KA, C_VA, C_QB, C_KB, C_VB, C_RB, C_AB, C_QC, C_GL = 0, 768, 1536, 2304, 2560, 2816, 3328, 3840, 3856, 4368

COMPUTE = ("pe", "act", "dve", "pool")
QUEUES = ("q_sp", "q_act", "q_pool")
Q_HOST = {"q_sp": "sp", "q_act": "act", "q_pool": "pool"}
RING = {"q_sp": 16, "q_act": 8, "q_pool": 12}


class Op:
    __slots__ = ("eng", "fn", "deps", "idx", "signal", "is_dma", "dma_no", "stream", "where")


class Sched:
    def __init__(self):
        self.ops = []
        self.last_w = {}
        self.readers = {}
        self.streams = {"pe": [], "act": [], "dve": [], "pool": [], "sp": []}
        self.dma_count = {q: 0 for q in QUEUES}
        self.barrier_dep = None

    def op(self, eng, fn, reads=(), writes=()):
        mx = DBG.get("maxops")
        if mx is not None and len(self.ops) >= mx and not DBG.get("_in_dump"):
            return None
        is_dma = eng in QUEUES
        psr = [k for k in reads if k.startswith("ps")]
        if psr:
            reads = [k for k in reads if not k.startswith("ps")]
            writes = list(writes) + [k for k in psr if k not in writes]
        deps = set()
        if self.barrier_dep is not None:
            deps.add(self.barrier_dep)
        for k in reads:
            w = self.last_w.get(k)
            if w is not None:
                deps.add(w)
        for k in writes:
            w = self.last_w.get(k)
            if w is not None:
                deps.add(w)
            for r in self.readers.get(k, ()):
                deps.add(r)
        o = Op()
        o.eng, o.fn, o.deps, o.is_dma, o.signal, o.dma_no = eng, fn, deps, is_dma, False, None
        o.idx = len(self.ops)
        o.where = None
        if DBG.get("trace"):
            import sys as _sys
            fr = _sys._getframe(1)
            w = []
            while fr is not None and len(w) < 4:
                w.append("%s:%d" % (fr.f_code.co_name, fr.f_lineno))
                fr = fr.f_back
            o.where = " < ".join(w)
        self.ops.append(o)
        o.stream = Q_HOST[eng] if is_dma else eng
        if is_dma:
            o.dma_no = self.dma_count[eng]
            self.dma_count[eng] += 1
        self.streams[o.stream].append(o)
        for k in writes:
            self.last_w[k] = o.idx
            self.readers[k] = []
        for k in reads:
            if k not in writes:
                self.readers.setdefault(k, []).append(o.idx)
        return o

    def barrier(self, nop_fn):
        deps = set()
        for st in self.streams.values():
            if st:
                deps.add(st[-1].idx)
        for q in QUEUES:
            n = self.dma_count[q]
            cnt = 0
            for o in reversed(self.ops):
                if o.is_dma and o.eng == q:
                    deps.add(o.idx)
                    cnt += 1
                    if cnt >= RING[q]:
                        break
        o = self.op("dve", nop_fn)
        if o is None:
            return
        o.deps |= deps
        o.deps.discard(o.idx)
        self.barrier_dep = o.idx
        self.last_w = {}
        self.readers = {}

    def emit(self, nc):
        ops = self.ops
        for o in ops:
            for d in o.deps:
                p = ops[d]
                if not p.is_dma:
                    p.signal = True
        sig_count = {}
        cnt = {e: 0 for e in COMPUTE}
        for o in ops:
            if not o.is_dma:
                if o.signal:
                    cnt[o.eng] += 1
                sig_count[o.idx] = cnt[o.eng]
        with contextlib.ExitStack() as es:
            sems = {e: es.enter_context(nc.semaphore("s_" + e)) for e in COMPUTE}
            rings = {q: [es.enter_context(nc.semaphore("r_%s_%d" % (q, i))) for i in range(RING[q])]
                     for q in QUEUES}
            block = es.enter_context(nc.Block())
            handles = {"pe": "tensor", "act": "scalar", "dve": "vector", "pool": "gpsimd", "sp": "sync"}

            def run_stream(stream, e):
                waited = {}
                for o in self.streams[stream]:
                    need = {}
                    for d in o.deps:
                        p = ops[d]
                        if p.is_dma:
                            R = RING[p.eng]
                            key = (p.eng, p.dma_no % R)
                            val = 16 * (p.dma_no // R + 1)
                        else:
                            if p.eng == "pe" and stream == "pe":
                                continue
                            key = (p.eng, -1)
                            val = sig_count[p.idx]
                        if need.get(key, 0) < val:
                            need[key] = val
                    if o.is_dma:
                        R = RING[o.eng]
                        if o.dma_no >= R:
                            key = (o.eng, o.dma_no % R)
                            val = 16 * (o.dma_no // R)
                            if need.get(key, 0) < val:
                                need[key] = val
                    for key, val in need.items():
                        if waited.get(key, 0) >= val:
                            continue
                        waited[key] = val
                        sem = sems[key[0]] if key[1] < 0 else rings[key[0]][key[1]]
                        e.wait_ge(sem, val)
                    ins = o.fn(e)
                    if o.is_dma:
                        ins.then_inc(rings[o.eng][o.dma_no % RING[o.eng]], 16)
                    elif o.signal:
                        ins.then_inc(sems[o.eng], 1)
                for q in QUEUES:
                    if Q_HOST[q] != stream:
                        continue
                    n = self.dma_count[q]
                    R = RING[q]
                    for r in range(R):
                        k = (n - r + R - 1) // R
                        if k > 0 and waited.get((q, r), 0) < 16 * k:
                            e.wait_ge(rings[q][r], 16 * k)

            for stream, attr in handles.items():
                if not self.streams[stream]:
                    continue

                def body(e, stream=stream):
                    run_stream(stream, e)
                getattr(block, attr)(body)


class SlotPool:
    def __init__(self, n, key):
        self.free = list(range(n))
        self.key = key

    def get(self):
        assert self.free, "pool %s exhausted" % self.key
        return self.free.pop(0)

    def put(self, i):
        self.free.append(i)


DBG = {"nseq": NPS, "nst": 4, "sample": True, "moe": True, "mix": ("attn", "gla", "cross"), "merge": True,
       "ncores": NCORES}


def build_program():
    nc = bass.Bass("TRN2", target_bir_lowering=False)
    S = Sched()

    def din(name, shape, dt=F32):
        return nc.dram_tensor(name, list(shape), dt, kind="ExternalInput").ap()

    def dout(name, shape, dt=F32):
        return nc.dram_tensor(name, list(shape), dt, kind="ExternalOutput").ap()

    def dint(name, shape, dt=F32):
        return nc.dram_tensor(name, list(shape), dt, kind="Internal").ap()

    xp = din("xp", [NPS, SEQ, D])
    xsm = din("xs", [128, D])
    memp = din("memp", [NPS, 256, D])
    cw = [din("cw1", [NSB, 128, 2, 4, 64]), din("cw2", [NSB, 512, 2, 4, 64]), din("cw3", [NSB, 2048, 2, 4, 64])]
    sgla = din("sgla", [NSB, 4, 64, 128])
    cmem = din("cmem", [NSB, 256, 2, 4, 128])
    norm_mix = din("norm_mix", [D])
    w_in = din("w_in", [D, 7440])
    qn_a = din("qn_a", [64])
    kn_a = din("kn_a", [64])
    qn_c = din("qn_c", [128])
    kn_c = din("kn_c", [128])
    gup = din("gla_gate_up", [16, 256])
    gbias = din("gla_gate_bias", [256])
    gla_norm = din("gla_norm", [128])
    mem_norm = din("mem_norm", [D])
    w_mem_kv = din("w_mem_kv", [D, 1024])
    w_br = [din("w_branch_a", [256, D]), din("w_branch_b", [512, D]), din("w_branch_c", [512, D])]
    w_out = din("w_out", [D, D])
    norm_ffn = din("norm_ffn", [D])
    w_rg = din("w_router_group", [D, 4])
    b_rg = din("b_router_group", [4])
    w_re = din("w_router_expert", [D, 32])
    b_re = din("b_router_expert", [32])
    w_eg = din("w_exp_gate", [NEXP, D, 256])
    w_eu = din("w_exp_up", [NEXP, D, 256])
    w_ed = din("w_exp_down", [NEXP, 256, D])

    yp = dout("yp", [NPS, SEQ, D])
    ysm = dout("ys", [128, D])
    wp = [dout("w1p", [NPS, 128, 2, 4, 64]), dout("w2p", [NPS, 512, 2, 4, 64]), dout("w3p", [NPS, 2048, 2, 4, 64])]
    glap = dout("glap", [NPS, 4, 64, 128])
    memkv = dout("memkv", [NPS, 256, 2, 4, 128])
    ws = [dout("w1s", [NSB, 128, 2, 4, 64]), dout("w2s", [NSB, 512, 2, 4, 64]), dout("w3s", [NSB, 2048, 2, 4, 64])]
    glas = dout("glas", [NSB, 4, 64, 128])

    h2s = dint("h2s", [NTOK, D])
    wbf_in = dint("wbf_in", [D, 7440], BF16)
    wbf_out = dint("wbf_out", [D, D], BF16)
    wbf_br = [dint("wbf_br%d" % i, [r, D], BF16) for i, r in enumerate((256, 512, 512))]
    wbf_mem = dint("wbf_mem", [D, 1024], BF16)
    xsc = dint("xsc", [NEXP * CAP, D], BF16)
    ysc = dint("ysc", [NEXP * CAP, D])

    es = contextlib.ExitStack()
    with es:
        def sb(name, shape, dt):
            return es.enter_context(nc.sbuf_tensor(name, list(shape), dt))

        def pst(name, shape, dt):
            return es.enter_context(nc.psum_tensor(name, list(shape), dt))

        NG = 4
        psg = [pst("psg%d" % i, [128, 512], F32) for i in range(NG)]
        pstr = [pst("pstr%d" % i, [128, 1024], BF16) for i in range(2)]
        psN = pst("psN", [128, 512], F32)
        psL = pst("psL", [128, 512], F32)
        gpool = SlotPool(NG, "psg")
        trpool = SlotPool(2, "pstr")

        NF = 12
        NB = 10
        NX = 3
        NW = 3
        WCOLS = 512
        f32p = sb("f32p", [128, NF, 512], F32)
        bfp = sb("bfp", [128, NB, 1024], BF16)
        xpl = sb("xpl", [128, NX, 1024], F32)
        wbuf = sb("wbuf", [128, NW, 8, WCOLS], BF16)
        smallp = sb("smallp", [128, 96, 8], F32)
        fpool, bpool, xpool, wpool, spool = (SlotPool(NF, "f"), SlotPool(NB, "b"), SlotPool(NX, "x"),
                                             SlotPool(NW, "w"), SlotPool(96, "s"))
        xnT = sb("xnT", [128, 8, 512], BF16)
        kAT = sb("kAT", [128, 6, SEQ], BF16)
        vA = sb("vA", [128, 16, 768], BF16)
        qAT = sb("qAT", [128, 6, 512], BF16)
        big1 = sb("big1", [128, 8, 512], BF16)
        vb_bf = big1[:, 0:4, :]
        rbT = big1[:, 4:8, :]
        mergedT = big1[:, :, :]
        oaT = sb("oaT", [64, 4, 512], BF16)
        ob2T = sb("ob2T", [128, 4, 512], BF16)
        ocT = sb("ocT", [128, 4, 512], BF16)
        wbr_sb = [sb("wbr_a", [64, 2, 4, 128], BF16), sb("wbr_b", [128, 2, 4, 128], BF16),
                  sb("wbr_c", [128, 2, 4, 128], BF16)]
        wab = sb("wab", [128, 8, 16], BF16)
        S32 = sb("S32", [128, 2, 2, 128], F32)
        Sbf = sb("Sbf", [128, 2, 2, 128], BF16)
        kcT = sb("kcT", [128, 2, 4, 256], BF16)
        vc = sb("vc", [128, 2, 2, 512], BF16)
        kcache = kAT[:, 0:2, 128:1152].rearrange("p s (kt c) -> p s kt c", kt=4)
        vcache = kAT[:, 2:4, 128:1152].rearrange("p s (kt c) -> p s kt c", kt=4)
        kcTA = kAT[:, 4:6, 128:1152].rearrange("p s (kt pr c) -> p s kt pr c", kt=4, pr=2)
        wts = sb("wts", [128, NTILE, 2], F32)
        dsti = sb("dsti", [128, NTILE, 2], I32)
        rbase = sb("rbase", [128, 32], F32)
        ident = sb("ident", [128, 128], BF16)
        cf32 = sb("cf32", [128, 128], F32)
        ones_bf = sb("ones_bf", [128, 128], BF16)
        zeros_bf = sb("zeros_bf", [128, 128], BF16)
        stri_bf = sb("stri_bf", [128, 128], BF16)
        onesdiv = sb("onesdiv", [128, 128], F32)
        tri_p = sb("tri_p", [128, 128], F32)
        tri_s = sb("tri_s", [128, 128], F32)
        blk_p = sb("blk_p", [128, 16], F32)
        blk_s = sb("blk_s", [128, 16], F32)
        E_r = {4: sb("E4", [4, 32, 4], F32), 16: sb("E16", [16, 8, 16], F32)}
        masks = sb("masks", [128, 9, 128], BF16)
        maskS = sb("maskS", [128, 3, 128], BF16)
        maskC = sb("maskC", [128, 6, 4, 8], BF16)
        nmix_rep = sb("nmix_rep", [128, D], F32)
        nffn_rep = sb("nffn_rep", [128, D], F32)
        qna_rep = sb("qna_rep", [128, 64], F32)
        kna_rep = sb("kna_rep", [128, 64], F32)
        qnc_rep = sb("qnc_rep", [128, 128], F32)
        knc_rep = sb("knc_rep", [128, 128], F32)
        gbias_rep = sb("gbias_rep", [128, 256], F32)
        rbias_rep = sb("rbias_rep", [128, 36], F32)
        ecap = sb("ecap", [128, 32], F32)
        gnorm_c = sb("gnorm_c", [128, 1], F32)
        eps_c = sb("eps_c", [128, 1], F32)
        gup_bf = sb("gup_bf", [16, 256], BF16)
        wr_bf = sb("wr_bf", [128, 8, 36], BF16)

        def fk(i): return "f%d" % i
        def bk(i): return "b%d" % i
        def xk(i): return "x%d" % i
        def wk(i): return "w%d" % i
        def sk(i): return "s%d" % i
        def gk(i): return "psg%d" % i
        def tk(i): return "pstr%d" % i

        def mm(out, lhsT, rhs, start, stop, reads, writes):
            S.op("pe", lambda e: e.matmul(out, lhsT=lhsT, rhs=rhs, start=start, stop=stop, skip_group_check=True),
                 reads, writes)

        def tr(out, in_, reads, writes):
            S.op("pe", lambda e: e.transpose(out=out, in_=in_, identity=ident[:]), list(reads) + ["ident"], writes)

        def act(out, in_, func, reads, writes, bias=None, scale=None, accum_out=None):
            kw = {}
            if bias is not None:
                kw["bias"] = bias
            if scale is not None:
                kw["scale"] = scale
            if accum_out is not None:
                kw["accum_out"] = accum_out
            S.op("act", lambda e: e.activation(out=out, in_=in_, func=func, **kw), reads, writes)

        def tt(eng, out, in0, in1, op, reads, writes):
            S.op(eng, lambda e: e.tensor_tensor(out=out, in0=in0, in1=in1, op=op), reads, writes)

        def ts(out, in0, s1, s2, op0, op1, reads, writes):
            if s2 is None:
                S.op("dve", lambda e: e.tensor_scalar(out=out, in0=in0, scalar1=s1, scalar2=None, op0=op0),
                     reads, writes)
            else:
                S.op("dve", lambda e: e.tensor_scalar(out=out, in0=in0, scalar1=s1, scalar2=s2, op0=op0, op1=op1),
                     reads, writes)

        def stt(out, in0, scalar, in1, op0, op1, reads, writes):
            S.op("dve", lambda e: e.scalar_tensor_tensor(out=out, in0=in0, scalar=scalar, in1=in1, op0=op0, op1=op1),
                 reads, writes)

        def cp(eng, out, in_, reads, writes):
            if eng == "act":
                S.op("act", lambda e: e.activation(out=out, in_=in_, func=AF.Copy), reads, writes)
            else:
                S.op(eng, lambda e: e.tensor_copy(out=out, in_=in_), reads, writes)

        def recip(out, in_, reads, writes):
            S.op("dve", lambda e: e.reciprocal(out=out, in_=in_), reads, writes)

        def red(out, in_, op, reads, writes):
            S.op("dve", lambda e: e.tensor_reduce(out=out, in_=in_, axis=AX.X, op=op), reads, writes)

        def mset(eng, ap, val, writes):
            S.op(eng, lambda e: e.memset(ap, val), (), writes)

        def dma(q, out, in_, reads, writes):
            S.op(q, lambda e: e.dma_start(out=out, in_=in_), reads, writes)

        def asel(out, in_, pattern, cmp_op, fill, base, cm, reads, writes):
            S.op("pool", lambda e: e.affine_select(out=out, in_=in_, pattern=pattern, compare_op=cmp_op, fill=fill,
                                                   base=base, channel_multiplier=cm), reads, writes)

        mset("pool", eps_c[:], EPS, ["eps_c"])
        mset("pool", cf32[:], 1.0, ["cf32"])
        asel(cf32[:], cf32[:], [[-1, 128]], ALU.is_equal, 0.0, 0, 1, ["cf32"], ["cf32"])
        cp("dve", ident[:], cf32[:], ["cf32"], ["ident"])
        mset("pool", ones_bf[:], 1.0, ["ones_bf"])
        mset("pool", zeros_bf[:], 0.0, ["zeros_bf"])
        mset("pool", onesdiv[:], 1.0 / 128, ["onesdiv"])
        mset("pool", tri_p[:], 1.0, ["tri_p"])
        asel(tri_p[:], tri_p[:], [[1, 128]], ALU.is_ge, 0.0, 0, -1, ["tri_p"], ["tri_p"])
        mset("pool", cf32[:], 1.0, ["cf32"])
        asel(cf32[:], cf32[:], [[1, 128]], ALU.is_gt, 0.0, 0, -1, ["cf32"], ["cf32"])
        cp("dve", stri_bf[:], cf32[:], ["cf32"], ["stri_bf"])
        cp("pool", tri_s[:], tri_p[:], ["tri_p"], ["tri_s"])
        asel(tri_s[:].rearrange("p (b l) -> p b l", l=8), tri_s[:].rearrange("p (b l) -> p b l", l=8),
             [[-8, 16], [0, 8]], ALU.is_ge, 0.0, 0, 1, ["tri_s"], ["tri_s"])
        mset("pool", blk_p[:], 0.0, ["blk_p"])
        mset("pool", blk_p[:, 0:1], 1.0, ["blk_p"])
        mset("pool", blk_s[:], 1.0, ["blk_s"])
        asel(blk_s[:], blk_s[:], [[-8, 16]], ALU.is_ge, 0.0, 0, 1, ["blk_s"], ["blk_s"])
        asel(blk_s[:], blk_s[:], [[8, 16]], ALU.is_ge, 0.0, 7, -1, ["blk_s"], ["blk_s"])
        ecap_i = sb("ecap_i", [128, 32], I32)
        S.op("pool", lambda e: e.iota(ecap_i[:], pattern=[[CAP, 32]], base=0, channel_multiplier=0), (), ["ecap_i"])
        cp("dve", ecap[:], ecap_i[:], ["ecap_i"], ["ecap"])
        mset("pool", rbase[:], 0.0, ["rbase"])
        dma("q_sp", nmix_rep[:], norm_mix.partition_broadcast(128), (), ["nmix_rep"])
        dma("q_sp", nffn_rep[:], norm_ffn.partition_broadcast(128), (), ["nffn_rep"])
        dma("q_sp", qna_rep[:], qn_a.partition_broadcast(128), (), ["qna_rep"])
        dma("q_sp", kna_rep[:], kn_a.partition_broadcast(128), (), ["kna_rep"])
        dma("q_sp", qnc_rep[:], qn_c.partition_broadcast(128), (), ["qnc_rep"])
        dma("q_sp", knc_rep[:], kn_c.partition_broadcast(128), (), ["knc_rep"])
        dma("q_sp", gbias_rep[:], gbias.partition_broadcast(128), (), ["gbias_rep"])
        dma("q_sp", rbias_rep[:, 0:4], b_rg.partition_broadcast(128), (), ["rbias_rep"])
        dma("q_sp", rbias_rep[:, 4:36], b_re.partition_broadcast(128), ["rbias_rep"], ["rbias_rep"])
        dma("q_sp", gnorm_c[:], gla_norm.rearrange("(p o) -> p o", o=1), (), ["gnorm_c"])
        ts(qna_rep[:], qna_rep[:], 0.125, None, ALU.mult, None, ["qna_rep"], ["qna_rep"])
        ts(qnc_rep[:], qnc_rep[:], float(128 ** -0.5), None, ALU.mult, None, ["qnc_rep"], ["qnc_rep"])
        dma("q_pool", gup_bf[:], gup, (), ["gup_bf"])
        dma("q_pool", wr_bf[:, :, 0:4], w_rg.rearrange("(k p) c -> p k c", p=128), (), ["wr_bf"])
        dma("q_pool", wr_bf[:, :, 4:36], w_re.rearrange("(k p) c -> p k c", p=128), ["wr_bf"], ["wr_bf"])
        dma("q_pool", wab[:], w_in[:, C_AB:C_AB + 16].rearrange("(k p) c -> p k c", p=128), (), ["wab"])
        for gi, r in enumerate(DIL):
            g = gpool.get()
            if r == 1:
                mset("dve", psg[g][:, 0:128], 1.0, [gk(g)])
            else:
                er = E_r[r]
                mset("pool", er[:], 1.0, ["E%d" % r])
                asel(er[:], er[:], [[0, 128 // r], [1, r]], ALU.is_equal, 0.0, 0, -1, ["E%d" % r], ["E%d" % r])
                er2 = er[:].rearrange("p a b -> p (a b)")
                mm(psg[g][:, 0:128], er2, er2, True, True, ["E%d" % r], [gk(g)])
            f = fpool.get()
            ts(f32p[:, f, 0:128], psg[g][:, 0:128], -1.0, -NEG, ALU.add, ALU.mult, [gk(g)], [fk(f)])
            gpool.put(g)
            cp("dve", masks[:, 3 * gi + 1, :], f32p[:, f, 0:128], [fk(f)], ["masks"])
            f2 = fpool.get()
            asel(f32p[:, f2, 0:128], f32p[:, f, 0:128], [[1, 128]], ALU.is_ge, NEG, 0, -1, [fk(f)], [fk(f2)])
            cp("dve", masks[:, 3 * gi + 0, :], f32p[:, f2, 0:128], [fk(f2)], ["masks"])
            asel(f32p[:, f2, 0:128], f32p[:, f, 0:128], [[-1, 128]], ALU.is_ge, NEG, 0, 1, [fk(f)], [fk(f2)])
            cp("dve", masks[:, 3 * gi + 2, :], f32p[:, f2, 0:128], [fk(f2)], ["masks"])
            asel(f32p[:, f2, 0:128], f32p[:, f, 0:128], [[1, 128]], ALU.is_ge, NEG, 0, -1, [fk(f)], [fk(f2)])
            v3 = f32p[:, f2, 0:128].rearrange("p (b l) -> p b l", l=8)
            asel(v3, v3, [[-8, 16], [0, 8]], ALU.is_ge, NEG, 0, 1, [fk(f2)], [fk(f2)])
            cp("dve", maskS[:, gi, :], f32p[:, f2, 0:128], [fk(f2)], ["maskS"])
            fpool.put(f)
            fpool.put(f2)
            for hh in range(4):
                cp("pool", maskC[:, 2 * gi + 0, hh, :], masks[:, 3 * gi + 2, 0:8], ["masks"], ["maskC"])
                cp("pool", maskC[:, 2 * gi + 1, hh, :], masks[:, 3 * gi + 1, 0:8], ["masks"], ["maskC"])

        def rms_stats(src_ap, n, reads, tag):
            s = spool.get()
            jb = bpool.get()
            mset("pool", smallp[:, s, 0:1], 0.0, [sk(s)])
            act(bfp[:, jb, 0:n], src_ap, AF.Square, list(reads) + [sk(s)], [bk(jb), sk(s)],
                accum_out=smallp[:, s, 0:1])
            bpool.put(jb)
            act(smallp[:, s, 1:2], smallp[:, s, 0:1], AF.Sqrt, [sk(s), "eps_c"], [sk(s)], bias=eps_c[:], scale=1.0 / n)
            recip(smallp[:, s, 0:1], smallp[:, s, 1:2], [sk(s)], [sk(s)])
            return s

        def transposes_to(dst3, src2, nblk, reads, writes):
            t = trpool.get()
            for j in range(nblk):
                tr(pstr[t][:, j * 128:(j + 1) * 128], src2[:, j * 128:(j + 1) * 128], reads, [tk(t)])
            cp("dve", dst3, pstr[t][:, 0:nblk * 128].rearrange("p (k c) -> p k c", k=nblk), [tk(t)], writes)
            trpool.put(t)

        def front_a(x_src, grep, grep_key):
            x = xpool.get()
            dma("q_sp", xpl[:, x, :], x_src, (), [xk(x)])
            s = rms_stats(xpl[:, x, :], D, [xk(x)], "fr")
            b = bpool.get()
            stt(bfp[:, b, :], xpl[:, x, :], smallp[:, s, 0:1], grep, ALU.mult, ALU.mult,
                [xk(x), sk(s), grep_key], [bk(b)])
            spool.put(s)
            xpool.put(x)
            return b

        def front_b(b, dstT, dst_key):
            transposes_to(dstT, bfp[:, b, :], 8, [bk(b)], [dst_key])
            bpool.put(b)

        def front(x_src, grep, grep_key, dstT, dst_key, keep_x=False):
            front_b(front_a(x_src, grep, grep_key), dstT, dst_key)

        def wload(src3, kk, cols):
            src3, skey = src3
            w = wpool.get()
            dma("q_pool", wbuf[:, w, 0:kk, 0:cols], src3, [skey], [wk(w)])
            return w

        conv_blocks = ([(C_QA + i * 256, 256) for i in range(9)] + [(C_VB, 512), (C_RB, 512), (C_QC, 512), (C_QB, 512)]
                       + [(C_GL + i * 512, 512) for i in range(6)])
        for part in range(2):
            dma("q_pool", wbf_mem[:, part * 512:(part + 1) * 512], w_mem_kv[:, part * 512:(part + 1) * 512], (),
                ["wbfmem%d" % part])
        for (c0, cols) in conv_blocks:
            dma("q_pool", wbf_in[:, c0:c0 + cols], w_in[:, c0:c0 + cols], (), ["wbfin%d" % c0])
        for b_ in range(3):
            dma("q_pool", wbf_br[b_][:, :], w_br[b_][:, :], (), ["wbfbr%d" % b_])
        for hf in range(2):
            dma("q_pool", wbf_out[:, hf * 512:(hf + 1) * 512], w_out[:, hf * 512:(hf + 1) * 512], (),
                ["wbfout%d" % hf])

        def w_in_blk(c0, cols):
            return (wbf_in[:, c0:c0 + cols].rearrange("(k p) c -> p k c", p=128), "wbfin%d" % c0)

        def proj_tok(actT, act_key, t0, w, cols, ps_ap, ps_key, wc0=0):
            for k in range(8):
                mm(ps_ap, actT[:, k, t0:t0 + 128], wbuf[:, w, k, wc0:wc0 + cols], k == 0, k == 7,
                   [act_key, wk(w)], [ps_key])

        def headnorm(ps_ap, ps_key, nh, hd, grep, grep_key, outs):
            f = fpool.get()
            s = spool.get()
            act(f32p[:, f, 0:nh * hd], ps_ap, AF.Square, [ps_key], [fk(f)])
            red(smallp[:, s, 0:nh], f32p[:, f, 0:nh * hd].rearrange("p (h d) -> p h d", h=nh), ALU.add,
                [fk(f)], [sk(s)])
            fpool.put(f)
            s2 = spool.get()
            act(smallp[:, s2, 0:nh], smallp[:, s, 0:nh], AF.Sqrt, [sk(s), "eps_c"], [sk(s2)], bias=eps_c[:],
                scale=1.0 / hd)
            recip(smallp[:, s, 0:nh], smallp[:, s2, 0:nh], [sk(s2)], [sk(s)])
            spool.put(s2)
            for (dst3, dkey) in outs:
                for h in range(nh):
                    stt(dst3[:, h, :], ps_ap[:, h * hd:(h + 1) * hd], smallp[:, s, h:h + 1], grep[:, 0:hd],
                        ALU.mult, ALU.mult, [ps_key, sk(s), grep_key], [dkey])
            spool.put(s)

        state = {"tile_no": 0}

        def attn_blocks(blocks, hsel=None):
            for _ in attn_blocks_gen(blocks):
                pass

        def attn_blocks_gen(blocks):
            groups = []
            i = 0
            while i < len(blocks):
                grp = []
                tot = 0
                while i < len(blocks) and tot + blocks[i]["ncol"] <= 512:
                    grp.append((blocks[i], tot))
                    tot += blocks[i]["ncol"]
                    i += 1
                groups.append((grp, tot))

            def pv_stage(grp, pb):
                for (b, c0) in grp:
                    for (v, cc, cn, oN, oL) in b["pv"]:
                        mm(oN, v, bfp[:, pb, c0 + cc:c0 + cc + cn], False, False, [b["vkey"], bk(pb)], ["psN"])
                        mm(oL, ones_bf[:, 0:64], bfp[:, pb, c0 + cc:c0 + cc + cn], False, False,
                           ["ones_bf", bk(pb)], ["psL"])
                bpool.put(pb)

            prev = None
            for (grp, tot) in groups:
                g = gpool.get()
                for (b, c0) in grp:
                    n = b["ncol"]
                    if len(b["sub"]) == 1:
                        mm(psg[g][:, c0:c0 + n], ident[:], b["mask"], True, False, ["ident", b["mkey"]], [gk(g)])
                        (kT, q, cc, cn) = b["sub"][0]
                        mm(psg[g][:, c0 + cc:c0 + cc + cn], kT, q, False, True, [b["kkey"], b["qkey"]], [gk(g)])
                    else:
                        hn = n // 2
                        first = True
                        for half in range(2):
                            mm(psg[g][:, c0 + half * hn:c0 + (half + 1) * hn], ident[:],
                               b["mask"][:, half * hn:(half + 1) * hn], first, False, ["ident", b["mkey"]], [gk(g)])
                            first = False
                            for (kT, q, cc, cn) in b["sub"][half * 2:half * 2 + 2]:
                                mm(psg[g][:, c0 + cc:c0 + cc + cn], kT, q, False, True, [b["kkey"], b["qkey"]],
                                   [gk(g)])
                pb = bpool.get()
                act(bfp[:, pb, 0:tot], psg[g][:, 0:tot], AF.Exp, [gk(g)], [bk(pb)])
                gpool.put(g)
                if prev is not None:
                    pv_stage(*prev)
                prev = (grp, pb)
                yield
            if prev is not None:
                pv_stage(*prev)
            yield

        def attn_finish(t0):
            f = fpool.get()
            recip(f32p[0:64, f, :], psL[0:64, :], ["psL"], [fk(f)])
            tt("dve", oaT[:, :, t0:t0 + 128], psN[0:64, :].rearrange("p (h t) -> p h t", h=4),
               f32p[0:64, f, :].rearrange("p (h t) -> p h t", h=4), ALU.mult, ["psN", fk(f)], ["oaT"])
            fpool.put(f)

        def attn_prompt_tile(qt, t0):
            mset("dve", psN[0:64, :], 0.0, ["psN"])
            mset("dve", psL[0:64, :], 0.0, ["psL"])
            blocks = []
            for h in range(4):
                hp, pr = (h % 2) * 64, h // 2
                for gi in range(3):
                    nd = WIN[gi] // 128
                    ch = gi * 2 + pr
                    for kt in range(max(0, qt - nd), qt + 1):
                        dlt = qt - kt
                        typ = 0 if dlt == 0 else (2 if dlt == nd else 1)
                        blocks.append(dict(
                            mask=masks[:, 3 * gi + typ, :], mkey="masks", kkey="kAT", qkey="qAT", vkey="vA", ncol=128,
                            sub=[(kAT[hp:hp + 64, ch, kt * 128:(kt + 1) * 128], qAT[hp:hp + 64, ch, t0:t0 + 128], 0, 128)],
                            pv=[(vA[:, kt, (gi * 4 + h) * 64:(gi * 4 + h + 1) * 64], 0, 128,
                                 psN[0:64, h * 128:(h + 1) * 128], psL[0:64, h * 128:(h + 1) * 128])]))
            yield from attn_blocks_gen(blocks)
            attn_finish(t0)
            yield

        def run_threads(gens):
            gens = list(gens)
            while gens:
                for gtor in list(gens):
                    try:
                        next(gtor)
                    except StopIteration:
                        gens.remove(gtor)

        def chain(makers):
            for mk in makers:
                yield from mk()

        def gla_tile(ti, t0, tri, tri_key, blk, blk_key, nblk, s_loader, s_store):
            bw = 128 // nblk
            g = gpool.get()
            for k in range(8):
                mm(psg[g][:, 0:16], xnT[:, k, t0:t0 + 128], wab[:, k, :], k == 0, k == 7, ["xnT", "wab"], [gk(g)])
            b1 = bpool.get()
            cp("act", bfp[:, b1, 0:16], psg[g][:, 0:16], [gk(g)], [bk(b1)])
            gpool.put(g)
            yield
            t = trpool.get()
            tr(pstr[t][0:16, 0:128], bfp[:, b1, 0:16], [bk(b1)], [tk(t)])
            cp("dve", bfp[0:16, b1, 128:256], pstr[t][0:16, 0:128], [tk(t)], [bk(b1)])
            trpool.put(t)
            yield
            g = gpool.get()
            mm(psg[g][:, 0:256], bfp[0:16, b1, 128:256], gup_bf[:], True, True, [bk(b1), "gup_bf"], [gk(g)])
            bpool.put(b1)
            fz = fpool.get()
            tt("dve", f32p[:, fz, 0:256], psg[g][:, 0:256], gbias_rep[:], ALU.add, [gk(g), "gbias_rep"], [fk(fz)])
            gpool.put(g)
            act(f32p[:, fz, 256:512], f32p[:, fz, 0:256], AF.Exp, [fk(fz)], [fk(fz)], scale=-1.0)
            act(f32p[:, fz, 0:256], f32p[:, fz, 256:512], AF.Ln, [fk(fz)], [fk(fz)], bias=1.0)
            yield
            g = gpool.get()
            mm(psg[g][:, 0:256], tri, f32p[:, fz, 0:256], True, True, [tri_key, fk(fz)], [gk(g)])
            for pr in range(2):
                mm(psg[g][:, 256 + pr * 16:256 + pr * 16 + nblk], f32p[:, fz, pr * 128:(pr + 1) * 128], blk[:, 0:nblk],
                   True, True, [fk(fz), blk_key], [gk(g)])
            fe = fpool.get()
            act(f32p[:, fe, 0:256], psg[g][:, 0:256], AF.Exp, [gk(g)], [fk(fe)], scale=-1.0 / 16)
            act(f32p[:, fe, 256:512], psg[g][:, 0:256], AF.Exp, [gk(g)], [fk(fe)], scale=1.0 / 16)
            ebl = f32p[:, fz, 256:256 + 32]
            act(ebl, psg[g][:, 256:288], AF.Exp, [gk(g), fk(fz)], [fk(fz)], scale=-1.0 / 16)
            gpool.put(g)
            yield
            w = state["w_qkb"]
            g = gpool.get()
            proj_tok(xnT, "xnT", t0, w, 512, psg[g][:, :], gk(g))
            bq = bpool.get()
            stt(bfp[:, bq, 0:256], psg[g][:, 0:256], 0.125, f32p[:, fe, 0:256], ALU.mult, ALU.mult,
                [gk(g), fk(fe)], [bk(bq)])
            tt("dve", bfp[:, bq, 256:512], psg[g][:, 256:512], f32p[:, fe, 256:512], ALU.mult, [gk(g), fk(fe)],
               [bk(bq)])
            gpool.put(g)
            fpool.put(fe)
            yield
            bT = bpool.get()
            transposes_to(bfp[:, bT, 0:512].rearrange("p (k c) -> p k c", k=4), bfp[:, bq, 0:512], 4, [bk(bq)],
                          [bk(bT)])
            DBG.setdefault("gla_slots", dict(bq=bq, bT=bT, fz=fz))
            yield
            gAB = [gpool.get(), gpool.get()]
            for h in (0, 2, 1, 3):
                hp, pr = (h % 2) * 64, h // 2
                g = gAB[h % 2]
                mm(psg[g][:, pr * 128:(pr + 1) * 128], bfp[hp:hp + 64, bT, 256 + pr * 128:256 + (pr + 1) * 128],
                   bfp[hp:hp + 64, bT, pr * 128:(pr + 1) * 128], True, True, [bk(bT)], [gk(g)])
            ba = bpool.get()
            for h in range(4):
                g = gAB[h % 2]
                pr = h // 2
                tt("dve", bfp[:, ba, h * 128:(h + 1) * 128], psg[g][:, pr * 128:(pr + 1) * 128], tri, ALU.mult,
                   [gk(g), tri_key], [bk(ba)])
            gpool.put(gAB[0])
            gpool.put(gAB[1])
            yield
            go = gpool.get()
            if nblk == 1:
                slot, skey = s_loader(0, "o")
                for h in range(4):
                    hp, pr = (h % 2) * 64, h // 2
                    mm(psg[go][:, h * 128:(h + 1) * 128], vb_bf[:, ti, h * 128:(h + 1) * 128],
                       bfp[:, ba, h * 128:(h + 1) * 128], True, False, ["big1", bk(ba)], [gk(go)])
                    mm(psg[go][:, h * 128:(h + 1) * 128], Sbf[hp:hp + 64, slot, pr, :],
                       bfp[hp:hp + 64, bT, pr * 128:(pr + 1) * 128], False, True, [skey + "bf", bk(bT)], [gk(go)])
            else:
                mset("dve", psg[go][:, :], 0.0, [gk(go)])
                for h in range(4):
                    mm(psg[go][:, h * 128:(h + 1) * 128], vb_bf[:, ti, h * 128:(h + 1) * 128],
                       bfp[:, ba, h * 128:(h + 1) * 128], False, False, ["big1", bk(ba)], [gk(go)])
                for bi in range(nblk):
                    slot, skey = s_loader(bi, "o")
                    for h in (0, 2, 1, 3):
                        hp, pr = (h % 2) * 64, h // 2
                        if h in (0, 1):
                            mm(psg[go][:, 0:8], zeros_bf[:, :], ones_bf[:, 0:8], False, False,
                               ["zeros_bf", "ones_bf"], [gk(go)])
                        mm(psg[go][:, h * 128 + bi * bw:h * 128 + (bi + 1) * bw], Sbf[hp:hp + 64, slot, pr, :],
                           bfp[hp:hp + 64, bT, pr * 128 + bi * bw:pr * 128 + (bi + 1) * bw], False, False,
                           [skey + "bf", bk(bT)], [gk(go)])
            bpool.put(ba)
            yield
            for bi in range(nblk):
                slot, skey = s_loader(bi, "u")
                if nblk == 1:
                    kesrc, kkey, kb_ = bfp[:, bq, 256:512], bk(bq), None
                else:
                    kb_ = bpool.get()
                    ts(bfp[:, kb_, 0:256], bfp[:, bq, 256:512], blk[:, bi:bi + 1], None, ALU.mult, None,
                       [bk(bq), blk_key], [bk(kb_)])
                    kesrc, kkey = bfp[:, kb_, 0:256], bk(kb_)
                g = gpool.get()
                for h in range(4):
                    pr = h // 2
                    mm(psg[g][:, h * 128:(h + 1) * 128], kesrc[:, pr * 128:(pr + 1) * 128],
                       vb_bf[:, ti, h * 128:(h + 1) * 128], True, True, [kkey, "big1"], [gk(g)])
                if kb_ is not None:
                    bpool.put(kb_)
                for h in range(4):
                    hp, pr = (h % 2) * 64, h // 2
                    tt("dve", S32[hp:hp + 64, slot, pr, :], psg[g][hp:hp + 64, h * 128:(h + 1) * 128],
                       S32[hp:hp + 64, slot, pr, :], ALU.add, [gk(g), skey], [skey])
                    ts(S32[hp:hp + 64, slot, pr, :], S32[hp:hp + 64, slot, pr, :],
                       f32p[hp:hp + 64, fz, 256 + pr * 16 + bi:256 + pr * 16 + bi + 1], None, ALU.mult, None,
                       [skey, fk(fz)], [skey])
                gpool.put(g)
                s_store(bi, slot, skey)
            bpool.put(bq)
            bpool.put(bT)
            fpool.put(fz)
            yield
            fo = fpool.get()
            fq = fpool.get()
            cp("dve", f32p[:, fo, :], psg[go][:, :], [gk(go)], [fk(fo)])
            act(f32p[:, fq, :], psg[go][:, :], AF.Square, [gk(go)], [fk(fq)])
            gpool.put(go)
            yield
            gm = gpool.get()
            gs = gpool.get()
            mm(psg[gm][:, :], onesdiv[:], f32p[:, fo, :], True, True, ["onesdiv", fk(fo)], [gk(gm)])
            mm(psg[gs][:, :], onesdiv[:], f32p[:, fq, :], True, True, ["onesdiv", fk(fq)], [gk(gs)])
            act(f32p[:, fq, :], psg[gm][:, :], AF.Square, [gk(gm), fk(fq)], [fk(fq)])
            tt("dve", f32p[:, fq, :], psg[gs][:, :], f32p[:, fq, :], ALU.subtract, [gk(gs), fk(fq)], [fk(fq)])
            gpool.put(gs)
            ts(f32p[:, fq, :], f32p[:, fq, :], 0.0, None, ALU.max, None, [fk(fq)], [fk(fq)])
            act(f32p[:, fq, :], f32p[:, fq, :], AF.Sqrt, [fk(fq), "eps_c"], [fk(fq)], bias=eps_c[:])
            recip(f32p[:, fq, :], f32p[:, fq, :], [fk(fq)], [fk(fq)])
            tt("dve", f32p[:, fo, :], f32p[:, fo, :], psg[gm][:, :], ALU.subtract, [fk(fo), gk(gm)], [fk(fo)])
            gpool.put(gm)
            tt("dve", f32p[:, fo, :], f32p[:, fo, :], f32p[:, fq, :], ALU.mult, [fk(fo), fk(fq)], [fk(fo)])
            fpool.put(fq)
            stt(ob2T[:, :, t0:t0 + 128], f32p[:, fo, :].rearrange("p (h t) -> p h t", h=4), gnorm_c[:, 0:1],
                rbT[:, :, t0:t0 + 128], ALU.mult, ALU.mult, [fk(fo), "gnorm_c", "big1"], ["ob2T"])
            fpool.put(fo)
            yield

        def cross_tile(t0, qc_b, groups):
            goc = gpool.get()
            gl = gpool.get()
            for pair in range(2):
                g = gpool.get()
                for hh in range(2):
                    h = pair * 2 + hh
                    for blk_i in range(2):
                        for (slot, key, c0, cn) in groups:
                            col = (hh * 2 + blk_i) * 128 + c0
                            mm(psg[g][:, col:col + cn], kcT[:, slot, h, blk_i * 128:(blk_i + 1) * 128],
                               bfp[:, qc_b, h * 128 + c0:h * 128 + c0 + cn], True, True, [key + "k", bk(qc_b)],
                               [gk(g)])
                pb = bpool.get()
                act(bfp[:, pb, 0:512], psg[g][:, :], AF.Exp, [gk(g)], [bk(pb)])
                gpool.put(g)
                for hh in range(2):
                    h = pair * 2 + hh
                    for (slot, key, c0, cn) in groups:
                        for blk_i in range(2):
                            col = (hh * 2 + blk_i) * 128 + c0
                            mm(psg[goc][:, h * 128 + c0:h * 128 + c0 + cn],
                               vc[:, slot, blk_i, h * 128:(h + 1) * 128], bfp[:, pb, col:col + cn],
                               blk_i == 0, blk_i == 1, [key + "v", bk(pb)], [gk(goc)])
                        for blk_i in range(2):
                            col = (hh * 2 + blk_i) * 128 + c0
                            mm(psg[gl][:, h * 128 + c0:h * 128 + c0 + cn], ones_bf[:, :], bfp[:, pb, col:col + cn],
                               blk_i == 0, blk_i == 1, ["ones_bf", bk(pb)], [gk(gl)])
                bpool.put(pb)
            f = fpool.get()
            recip(f32p[:, f, :], psg[gl][:, :], [gk(gl)], [fk(f)])
            gpool.put(gl)
            tt("dve", ocT[:, :, t0:t0 + 128], psg[goc][:, :].rearrange("p (h t) -> p h t", h=4),
               f32p[:, f, :].rearrange("p (h t) -> p h t", h=4), ALU.mult, [gk(goc), fk(f)], ["ocT"])
            gpool.put(goc)
            fpool.put(f)

        def route_tile(x, tile_no):
            s = rms_stats(xpl[:, x, :], D, [xk(x)], "ffn")
            bx = bpool.get()
            stt(bfp[:, bx, :], xpl[:, x, :], smallp[:, s, 0:1], nffn_rep[:], ALU.mult, ALU.mult,
                [xk(x), sk(s), "nffn_rep"], [bk(bx)])
            spool.put(s)
            bT = bpool.get()
            transposes_to(bfp[:, bT, :].rearrange("p (k c) -> p k c", k=8), bfp[:, bx, :], 8, [bk(bx)], [bk(bT)])
            g = gpool.get()
            for k in range(8):
                mm(psg[g][:, 0:36], bfp[:, bT, k * 128:(k + 1) * 128], wr_bf[:, k, :], k == 0, k == 7,
                   [bk(bT), "wr_bf"], [gk(g)])
            bpool.put(bT)
            f = fpool.get()
            F_ = f32p[:, f, :]
            tt("dve", F_[:, 0:36], psg[g][:, 0:36], rbias_rep[:], ALU.add, [gk(g), "rbias_rep"], [fk(f)])
            gpool.put(g)
            s = spool.get()
            sm = smallp[:, s, :]
            red(sm[:, 0:1], F_[:, 0:4], ALU.max, [fk(f)], [sk(s)])
            ts(F_[:, 40:44], F_[:, 0:4], sm[:, 0:1], None, ALU.is_ge, None, [fk(f), sk(s)], [fk(f)])
            ts(sm[:, 1:2], sm[:, 0:1], -1.0, None, ALU.mult, None, [sk(s)], [sk(s)])
            mset("pool", sm[:, 2:3], 0.0, [sk(s)])
            act(F_[:, 44:48], F_[:, 0:4], AF.Exp, [fk(f), sk(s)], [fk(f), sk(s)], bias=sm[:, 1:2], accum_out=sm[:, 2:3])
            recip(sm[:, 3:4], sm[:, 2:3], [sk(s)], [sk(s)])
            ts(F_[:, 44:48], F_[:, 40:44], -NEG, NEG, ALU.mult, ALU.add, [fk(f)], [fk(f)])
            tt("dve", F_[:, 64:96].rearrange("p (g e) -> p g e", g=4),
               F_[:, 4:36].rearrange("p (g e) -> p g e", g=4),
               F_[:, 44:48].unsqueeze(2).to_broadcast([128, 4, 8]), ALU.add, [fk(f)], [fk(f)])
            S.op("dve", lambda e: e.max(out=F_[:, 96:104], in_=F_[:, 64:96]), [fk(f)], [fk(f)])
            ts(F_[:, 128:160], F_[:, 64:96], F_[:, 96:97], None, ALU.is_ge, None, [fk(f)], [fk(f)])
            ts(F_[:, 160:192], F_[:, 64:96], F_[:, 97:98], None, ALU.is_ge, None, [fk(f)], [fk(f)])
            tt("dve", F_[:, 160:192], F_[:, 160:192], F_[:, 128:160], ALU.subtract, [fk(f)], [fk(f)])
            ts(sm[:, 4:5], F_[:, 96:97], -1.0, None, ALU.mult, None, [fk(f)], [sk(s)])
            act(sm[:, 5:6], F_[:, 97:98], AF.Exp, [fk(f), sk(s)], [sk(s)], bias=sm[:, 4:5])
            ts(sm[:, 6:7], sm[:, 5:6], 1.0, None, ALU.add, None, [sk(s)], [sk(s)])
            recip(sm[:, 6:7], sm[:, 6:7], [sk(s)], [sk(s)])
            tt("dve", wts[:, tile_no, 0:1], sm[:, 6:7], sm[:, 3:4], ALU.mult, [sk(s)], ["wts"])
            tt("dve", wts[:, tile_no, 1:2], wts[:, tile_no, 0:1], sm[:, 5:6], ALU.mult, [sk(s), "wts"], ["wts"])
            ba = bpool.get()
            cp("dve", bfp[:, ba, 0:64], F_[:, 128:192], [fk(f)], [bk(ba)])
            g = gpool.get()
            mm(psg[g][:, 0:64], stri_bf[:], bfp[:, ba, 0:64], True, True, ["stri_bf", bk(ba)], [gk(g)])
            mm(psg[g][:, 64:128], ones_bf[:], bfp[:, ba, 0:64], True, True, ["ones_bf", bk(ba)], [gk(g)])
            bpool.put(ba)
            tt("dve", F_[:, 256:288], rbase[:], ecap[:], ALU.add, ["rbase", "ecap"], [fk(f)])
            tt("dve", F_[:, 288:320], F_[:, 256:288], psg[g][:, 64:96], ALU.add, [fk(f), gk(g)], [fk(f)])
            tt("dve", F_[:, 256:288], F_[:, 256:288], psg[g][:, 0:32], ALU.add, [fk(f), gk(g)], [fk(f)])
            tt("dve", F_[:, 288:320], F_[:, 288:320], psg[g][:, 32:64], ALU.add, [fk(f), gk(g)], [fk(f)])
            tt("dve", F_[:, 256:288], F_[:, 256:288], F_[:, 128:160], ALU.mult, [fk(f)], [fk(f)])
            tt("dve", F_[:, 288:320], F_[:, 288:320], F_[:, 160:192], ALU.mult, [fk(f)], [fk(f)])
            red(sm[:, 0:2], F_[:, 256:320].rearrange("p (a e) -> p a e", a=2), ALU.add, [fk(f)], [sk(s)])
            tt("dve", rbase[:], rbase[:], psg[g][:, 64:96], ALU.add, ["rbase", gk(g)], ["rbase"])
            tt("dve", rbase[:], rbase[:], psg[g][:, 96:128], ALU.add, ["rbase", gk(g)], ["rbase"])
            gpool.put(g)
            cp("dve", dsti[:, tile_no, :], sm[:, 0:2], [sk(s)], ["dsti"])
            spool.put(s)
            fpool.put(f)
            for j in range(2):
                S.op("q_pool", lambda e, j=j: e.indirect_dma_start(
                    out=xsc[:, :], out_offset=bass.IndirectOffsetOnAxis(ap=dsti[:, tile_no, j:j + 1], axis=0),
                    in_=bfp[:, bx, :], in_offset=None), [bk(bx), "dsti"], ["xsc"])
            bpool.put(bx)

        def merge_supertile(ntile, x_src_of, tok0, prefetch=None):
            T = ntile * 128
            if prefetch is not None:
                prefetch()
            srcs = [(oaT, "oaT", 64, 4), (ob2T, "ob2T", 128, 4), (ocT, "ocT", 128, 4)]
            order = [("g", half, b) for half in range(2) for b in range(3)] + [("o", hf, 0) for hf in range(2)]
            wq = {}

            def ensure(n):
                if n < len(order) and n not in wq:
                    kind, p0, p1 = order[n]
                    if kind == "g":
                        wq[n] = wload(w_in_blk(C_GL + p1 * 1024 + p0 * 512, 512), 8, 512)
                    else:
                        wq[n] = wload((wbf_out[:, p0 * 512:(p0 + 1) * 512].rearrange("(k p) c -> p k c", p=128),
                                       "wbfout%d" % p0), 8, 512)
            ensure(0)
            acc = None
            for n in range(6):
                ensure(n + 1)
                _, half, b = order[n]
                w = wq[n]
                if b == 0:
                    acc = [fpool.get() for _ in range(4)]
                src, skey, np_, nj = srcs[b]
                for cc in range(4):
                    c = half * 4 + cc
                    par = cc % 2
                    wkey = "wbr%d_%d" % (b, par)
                    dma("q_pool", wbr_sb[b][:, par, :, :],
                        wbf_br[b][:, c * 128:(c + 1) * 128].rearrange("(j p) c -> p j c", p=np_), ["wbfbr%d" % b],
                        [wkey])
                    g = gpool.get()
                    for k in range(8):
                        mm(psg[g][:, 0:T], wbuf[:, w, k, cc * 128:(cc + 1) * 128], xnT[:, k, 0:T],
                           k == 0, k == 7, [wk(w), "xnT"], [gk(g)])
                    sgb = bpool.get()
                    act(bfp[:, sgb, 0:T], psg[g][:, 0:T], AF.Sigmoid, [gk(g)], [bk(sgb)])
                    gpool.put(g)
                    g = gpool.get()
                    for j in range(nj):
                        mm(psg[g][:, 0:T], wbr_sb[b][0:np_, par, j, :], src[0:np_, j, 0:T], j == 0, j == nj - 1,
                           [wkey, skey], [gk(g)])
                    if b == 0:
                        tt("dve", f32p[:, acc[cc], 0:T], psg[g][:, 0:T], bfp[:, sgb, 0:T], ALU.mult,
                           [gk(g), bk(sgb)], [fk(acc[cc])])
                    else:
                        ft = fpool.get()
                        tt("dve", f32p[:, ft, 0:T], psg[g][:, 0:T], bfp[:, sgb, 0:T], ALU.mult, [gk(g), bk(sgb)],
                           [fk(ft)])
                        if b == 1:
                            tt("dve", f32p[:, acc[cc], 0:T], f32p[:, acc[cc], 0:T], f32p[:, ft, 0:T], ALU.add,
                               [fk(acc[cc]), fk(ft)], [fk(acc[cc])])
                        else:
                            tt("dve", mergedT[:, c, 0:T], f32p[:, acc[cc], 0:T], f32p[:, ft, 0:T], ALU.add,
                               [fk(acc[cc]), fk(ft)], ["big1"])
                        fpool.put(ft)
                    gpool.put(g)
                    bpool.put(sgb)
                wpool.put(w)
                if b == 2:
                    for a_ in acc:
                        fpool.put(a_)
            ensure(7)
            wo = [wq[6], wq[7]]
            prev = None
            for i in range(ntile):
                x = xpool.get()
                dma("q_sp", xpl[:, x, :], x_src_of(i), (), [xk(x)])
                for hf in range(2):
                    g = gpool.get()
                    for c in range(8):
                        mm(psg[g][:, :], mergedT[:, c, i * 128:(i + 1) * 128], wbuf[:, wo[hf], c, :], c == 0, c == 7,
                           ["big1", wk(wo[hf])], [gk(g)])
                    tt("dve", xpl[:, x, hf * 512:(hf + 1) * 512], xpl[:, x, hf * 512:(hf + 1) * 512], psg[g][:, :],
                       ALU.add, [xk(x), gk(g)], [xk(x)])
                    gpool.put(g)
                tn = state["tile_no"]
                state["tile_no"] += 1
                dma("q_sp", h2s[tn * 128:(tn + 1) * 128, :], xpl[:, x, :], [xk(x)], ["h2s"])
                if prev is not None:
                    route_tile(*prev)
                    xpool.put(prev[0])
                prev = (x, tn)
            route_tile(*prev)
            xpool.put(prev[0])
            for w in wo:
                wpool.put(w)

        def run_blocks(blocks, depth=2):
            pend = []
            wq = {}

            def ensure_w(n):
                if n < len(blocks) and n not in wq:
                    wq[n] = wload(*blocks[n]["wsrc"])
            ensure_w(0)
            for n, blk in enumerate(blocks):
                ensure_w(n + 1)
                w = wq[n]
                for (fa, fb) in blk["items"]:
                    ctx = fa(w)
                    pend.append((fb, ctx))
                    if len(pend) > depth:
                        fb_, ctx_ = pend.pop(0)
                        fb_(ctx_)
                wpool.put(w)
            while pend:
                fb_, ctx_ = pend.pop(0)
                fb_(ctx_)

        def project_supertile(ntile, seq_tile0, is_prompt, out_kv, cross_fn):
            T = ntile * 128
            blocks = []

            def A_tok(i, cols):
                def fa(w):
                    g = gpool.get()
                    proj_tok(xnT, "xnT", i * 128, w, cols, psg[g][:, 0:cols], gk(g))
                    return g
                return fa

            for gi in range(3):
                items = []
                for i in range(ntile):
                    def fb(g, gi=gi, i=i):
                        bq = bpool.get()
                        headnorm(psg[g][:, 0:256], gk(g), 4, 64, qna_rep, "qna_rep",
                                 [(bfp[:, bq, 0:256].rearrange("p (h d) -> p h d", h=4), bk(bq))])
                        gpool.put(g)
                        transposes_to(qAT[:, gi * 2:gi * 2 + 2, i * 128:(i + 1) * 128], bfp[:, bq, 0:256], 2,
                                      [bk(bq)], ["qAT"])
                        bpool.put(bq)
                    items.append((A_tok(i, 256), fb))
                blocks.append(dict(wsrc=(w_in_blk(C_QA + gi * 256, 256), 8, 256), items=items))
            for gi in range(3):
                items = []
                for i in range(ntile):
                    def fb(g, gi=gi, i=i):
                        bq = bpool.get()
                        f = fpool.get()
                        headnorm(psg[g][:, 0:256], gk(g), 4, 64, kna_rep, "kna_rep",
                                 [(f32p[:, f, 0:256].rearrange("p (h d) -> p h d", h=4), fk(f))])
                        gpool.put(g)
                        cp("act", bfp[:, bq, 0:256], f32p[:, f, 0:256], [fk(f)], [bk(bq)])
                        out_kv(0, gi, i, f32p[:, f, 0:256], fk(f))
                        fpool.put(f)
                        kt = seq_tile0 + i
                        transposes_to(kAT[:, gi * 2:gi * 2 + 2, kt * 128:(kt + 1) * 128], bfp[:, bq, 0:256], 2,
                                      [bk(bq)], ["kAT"])
                        bpool.put(bq)
                    items.append((A_tok(i, 256), fb))
                blocks.append(dict(wsrc=(w_in_blk(C_KA + gi * 256, 256), 8, 256), items=items))
            for gi in range(3):
                items = []
                for i in range(ntile):
                    def fb(g, gi=gi, i=i):
                        f = fpool.get()
                        cp("dve", f32p[:, f, 0:256], psg[g][:, 0:256], [gk(g)], [fk(f)])
                        cp("act", vA[:, seq_tile0 + i, gi * 256:(gi + 1) * 256], psg[g][:, 0:256], [gk(g)], ["vA"])
                        gpool.put(g)
                        out_kv(1, gi, i, f32p[:, f, 0:256], fk(f))
                        fpool.put(f)
                    items.append((A_tok(i, 256), fb))
                blocks.append(dict(wsrc=(w_in_blk(C_VA + gi * 256, 256), 8, 256), items=items))
            items = []
            for i in range(ntile):
                def fb(g, i=i):
                    cp("act", vb_bf[:, i, :], psg[g][:, :], [gk(g)], ["big1"])
                    gpool.put(g)
                items.append((A_tok(i, 512), fb))
            blocks.append(dict(wsrc=(w_in_blk(C_VB, 512), 8, 512), items=items))
            items = []
            for j in range(4):
                def fa(w, j=j):
                    g = gpool.get()
                    for k in range(8):
                        mm(psg[g][:, 0:T], wbuf[:, w, k, j * 128:(j + 1) * 128], xnT[:, k, 0:T], k == 0, k == 7,
                           [wk(w), "xnT"], [gk(g)])
                    return g

                def fb(g, j=j):
                    act(rbT[:, j, 0:T], psg[g][:, 0:T], AF.Silu, [gk(g)], ["big1"])
                    gpool.put(g)
                items.append((fa, fb))
            blocks.append(dict(wsrc=(w_in_blk(C_RB, 512), 8, 512), items=items))
            run_blocks(blocks)
            w = wload(w_in_blk(C_QC, 512), 8, 512)
            for i in range(ntile):
                g = gpool.get()
                proj_tok(xnT, "xnT", i * 128, w, 512, psg[g][:, :], gk(g))
                bq = bpool.get()
                headnorm(psg[g][:, :], gk(g), 4, 128, qnc_rep, "qnc_rep",
                         [(bfp[:, bq, 0:512].rearrange("p (h d) -> p h d", h=4), bk(bq))])
                gpool.put(g)
                bT = bpool.get()
                transposes_to(bfp[:, bT, 0:512].rearrange("p (k c) -> p k c", k=4), bfp[:, bq, 0:512], 4, [bk(bq)],
                              [bk(bT)])
                bpool.put(bq)
                cross_fn(i, bT)
                bpool.put(bT)
            wpool.put(w)
            state["w_qkb"] = wload(w_in_blk(C_QB, 512), 8, 512)

        def mem_kv_prompt(sq):
            mT = [bpool.get(), bpool.get()]
            x = xpool.get()
            dma("q_sp", xpl[:, x, :], mem_norm.partition_broadcast(128), (), [xk(x)])
            for t in range(2):
                front(memp[sq, t * 128:(t + 1) * 128, :], xpl[:, x, :], xk(x),
                      bfp[:, mT[t], :].rearrange("p (k c) -> p k c", k=8), bk(mT[t]))
            xpool.put(x)
            for part in range(2):
                w = wload((wbf_mem[:, part * 512:(part + 1) * 512].rearrange("(k p) c -> p k c", p=128),
                           "wbfmem%d" % part), 8, 512)
                for t in range(2):
                    g = gpool.get()
                    for k in range(8):
                        mm(psg[g][:, :], bfp[:, mT[t], k * 128:(k + 1) * 128], wbuf[:, w, k, :], k == 0, k == 7,
                           [bk(mT[t]), wk(w)], [gk(g)])
                    f = fpool.get()
                    if part == 0:
                        headnorm(psg[g][:, :], gk(g), 4, 128, knc_rep, "knc_rep",
                                 [(f32p[:, f, :].rearrange("p (h d) -> p h d", h=4), fk(f))])
                        gpool.put(g)
                        bq = bpool.get()
                        cp("act", bfp[:, bq, 0:512], f32p[:, f, :], [fk(f)], [bk(bq)])
                        transposes_to(kcT[:, 0, :, t * 128:(t + 1) * 128], bfp[:, bq, 0:512], 4, [bk(bq)], ["kv0k"])
                        bpool.put(bq)
                    else:
                        cp("dve", f32p[:, f, :], psg[g][:, :], [gk(g)], [fk(f)])
                        cp("act", vc[:, 0, t, :], psg[g][:, :], [gk(g)], ["kv0v"])
                        gpool.put(g)
                    dma("q_sp", memkv[sq, t * 128:(t + 1) * 128, part, :, :],
                        f32p[:, f, :].rearrange("p (h d) -> p h d", h=4), [fk(f)], ["memkv"])
                    fpool.put(f)
                wpool.put(w)
            for m in mT:
                bpool.put(m)

        pending_copies = [(gi, b) for b in range(NSB) for gi in range(3)]

        def issue_copies(n):
            for _ in range(n):
                if not pending_copies:
                    return
                gi, b = pending_copies.pop(0)
                Wb = WIN[gi]
                dma("q_sp", ws[gi][b, 0:Wb - LS], cw[gi][b, LS:Wb], (), ["wsc%d_%d" % (gi, b)])

        def prompt_seq(sq):
            mem_kv_prompt(sq)
            mset("pool", S32[:, 0, :, :], 0.0, ["S0"])
            mset("pool", Sbf[:, 0, :, :], 0.0, ["S0bf"])
            for st in range(DBG["nst"]):
                tile0 = st * 4
                pre = state.pop("pre", None)
                if pre is None:
                    pre = [front_a(xp[sq, (tile0 + i) * 128:(tile0 + i + 1) * 128, :], nmix_rep[:], "nmix_rep")
                           for i in range(4)]
                for i in range(4):
                    front_b(pre[i], xnT[:, :, i * 128:(i + 1) * 128], "xnT")

                def prefetch(sq=sq, st=st):
                    if st + 1 < DBG["nst"]:
                        nsq, nt0 = sq, (st + 1) * 4
                    elif sq + 1 < DBG["nseq"]:
                        nsq, nt0 = sq + 1, 0
                    else:
                        if DBG["sample"]:
                            state["pre"] = [front_a(xsm[:, :], nmix_rep[:], "nmix_rep")]
                        return
                    state["pre"] = [front_a(xp[nsq, (nt0 + i) * 128:(nt0 + i + 1) * 128, :], nmix_rep[:],
                                            "nmix_rep") for i in range(4)]

                def out_kv(kind, gi, i, f32view, key, tile0=tile0):
                    pos0 = (tile0 + i) * 128
                    lo = SEQ - WIN[gi]
                    if pos0 >= lo:
                        dma("q_sp", wp[gi][sq, pos0 - lo:pos0 - lo + 128, kind, :, :],
                            f32view.rearrange("p (h d) -> p h d", h=4), [key], ["wp%d" % gi])

                project_supertile(4, tile0, True, out_kv,
                                  (lambda i, bT: cross_tile(i * 128, bT, [(0, "kv0", 0, 128)])) if "cross" in DBG["mix"]
                                  else (lambda i, bT: None))
                issue_copies(6)

                def s_loader(bi, phase):
                    return 0, "S0"

                def s_store(bi, slot, skey):
                    cp("act", Sbf[:, 0, :, :], S32[:, 0, :, :], ["S0"], ["S0bf"])

                threads = []
                if "attn" in DBG["mix"]:
                    threads.append(chain([(lambda i=i: attn_prompt_tile(tile0 + i, i * 128)) for i in range(4)]))
                if "gla" in DBG["mix"]:
                    threads.append(chain([(lambda i=i: gla_tile(i, i * 128, tri_p[:], "tri_p", blk_p, "blk_p", 1,
                                                                s_loader, s_store)) for i in range(4)]))
                run_threads(threads)
                wpool.put(state["w_qkb"])
                if DBG["merge"]:
                    merge_supertile(4, lambda i, tile0=tile0: xp[sq, (tile0 + i) * 128:(tile0 + i + 1) * 128, :], 0,
                                    prefetch)
            dma("q_sp", glap[sq].rearrange("(pr hh) dk dv -> (hh dk) pr dv", hh=2), S32[:, 0, :, :], ["S0"], ["glap"])

        def sample_tile():
            issue_copies(1000)
            pre = state.pop("pre", None)
            if pre is None:
                pre = [front_a(xsm[:, :], nmix_rep[:], "nmix_rep")]
            front_b(pre[0], xnT[:, :, 0:128], "xnT")

            def out_kv(kind, gi, i, f32view, key):
                Wb = WIN[gi]
                for b in range(NSB):
                    dma("q_sp", ws[gi][b, Wb - LS:Wb, kind, :, :],
                        f32view[b * LS:(b + 1) * LS, :].rearrange("p (h d) -> p h d", h=4), [key],
                        ["wsn%d_%d_%d" % (gi, b, kind)])

            project_supertile(1, 0, False, out_kv, lambda i, bT: cross_sample(bT))
            mset("dve", psN[0:64, :], 0.0, ["psN"])
            mset("dve", psL[0:64, :], 0.0, ["psL"])
            blocks = []
            for h in range(4):
                hp, pr = (h % 2) * 64, h // 2
                for gi in range(3):
                    ch = gi * 2 + pr
                    blocks.append(dict(
                        mask=maskS[:, gi, :], mkey="maskS", kkey="kAT", qkey="qAT", vkey="vA", ncol=128,
                        sub=[(kAT[hp:hp + 64, ch, 0:128], qAT[hp:hp + 64, ch, 0:128], 0, 128)],
                        pv=[(vA[:, 0, (gi * 4 + h) * 64:(gi * 4 + h + 1) * 64], 0, 128,
                             psN[0:64, h * 128:(h + 1) * 128], psL[0:64, h * 128:(h + 1) * 128])]))
            attn_blocks(blocks, None)
            ld = 0
            for b in range(NSB):
                for gi in range(3):
                    nkt_all = WIN[gi] // 128
                    for k0 in range(0, nkt_all, 4):
                        nkt = min(4, nkt_all - k0)
                        slot = ld % 2
                        ld += 1
                        ckey = "cache%d" % slot
                        dma("q_pool", kcache[:, slot, 0:nkt, :],
                            cw[gi][b, k0 * 128:(k0 + nkt) * 128, 0].rearrange("(kt p) h d -> p kt (h d)", p=128),
                            (), [ckey + "k"])
                        dma("q_pool", vcache[:, slot, 0:nkt, :],
                            cw[gi][b, k0 * 128:(k0 + nkt) * 128, 1].rearrange("(kt p) h d -> p kt (h d)", p=128),
                            (), [ckey + "v"])
                        t = trpool.get()
                        for kt in range(nkt):
                            for pr in range(2):
                                tr(pstr[t][:, (kt * 2 + pr) * 128:(kt * 2 + pr + 1) * 128],
                                   kcache[:, slot, kt, pr * 128:(pr + 1) * 128], [ckey + "k"], [tk(t)])
                        cp("dve", kcTA[:, slot, 0:nkt, :, :],
                           pstr[t][:, 0:nkt * 256].rearrange("p (kt pr c) -> p kt pr c", kt=nkt, pr=2),
                           [tk(t)], [ckey + "kT"])
                        trpool.put(t)
                        blocks = []
                        for kt in range(nkt):
                            typ = 0 if (k0 + kt) == 0 else 1
                            sub, pv = [], []
                            for ci, h in enumerate((0, 2, 1, 3)):
                                hp, pr = (h % 2) * 64, h // 2
                                sub.append((kcTA[hp:hp + 64, slot, kt, pr, :],
                                            qAT[hp:hp + 64, gi * 2 + pr, b * LS:(b + 1) * LS], ci * 8, 8))
                                pv.append((vcache[:, slot, kt, h * 64:(h + 1) * 64], ci * 8, 8,
                                           psN[0:64, h * 128 + b * LS:h * 128 + (b + 1) * LS],
                                           psL[0:64, h * 128 + b * LS:h * 128 + (b + 1) * LS]))
                            blocks.append(dict(mask=maskC[:, 2 * gi + typ, :, :].rearrange("p h l -> p (h l)"),
                                               mkey="maskC", kkey=ckey + "kT", qkey="qAT", vkey=ckey + "v", ncol=32,
                                               sub=sub, pv=pv))
                        attn_blocks(blocks, None)
            attn_finish(0)
            def s_loader(bi, phase):
                slot = bi % 2
                key = "S%d" % slot
                src = sgla[bi].rearrange("(pr hh) dk dv -> (hh dk) pr dv", hh=2)
                if phase == "o":
                    dma("q_pool", Sbf[:, slot, :, :], src, (), [key + "bf"])
                else:
                    dma("q_sp", S32[:, slot, :, :], src, (), [key])
                return slot, key

            def s_store(bi, slot, skey):
                dma("q_sp", glas[bi].rearrange("(pr hh) dk dv -> (hh dk) pr dv", hh=2), S32[:, slot, :, :], [skey],
                    ["glas"])

            for _ in gla_tile(0, 0, tri_s[:], "tri_s", blk_s, "blk_s", NSB, s_loader, s_store):
                pass
            wpool.put(state["w_qkb"])
            merge_supertile(1, lambda i: xsm[:, :], 0)

        def cross_sample(qc_b):
            goc = gpool.get()
            gl = gpool.get()
            for b in range(NSB):
                slot = b % 2
                key = "kvs%d" % slot
                bk_ = bpool.get()
                dma("q_pool", bfp[:, bk_, :].rearrange("p (blk c) -> p blk c", blk=2),
                    cmem[b, :, 0].rearrange("(blk p) h d -> p blk (h d)", p=128), (), [bk(bk_)])
                dma("q_pool", vc[:, slot, :, :], cmem[b, :, 1].rearrange("(blk p) h d -> p blk (h d)", p=128), (),
                    [key + "v"])
                t = trpool.get()
                for blk_i in range(2):
                    for h in range(4):
                        tr(pstr[t][:, (h * 2 + blk_i) * 128:(h * 2 + blk_i + 1) * 128],
                           bfp[:, bk_, blk_i * 512 + h * 128:blk_i * 512 + (h + 1) * 128], [bk(bk_)], [tk(t)])
                cp("dve", kcT[:, slot, :, :], pstr[t][:, :].rearrange("p (h n) -> p h n", h=4), [tk(t)], [key + "k"])
                trpool.put(t)
                bpool.put(bk_)
                c0, cn = b * LS, LS
                g = gpool.get()
                for h in range(4):
                    for blk_i in range(2):
                        col = (h * 2 + blk_i) * 8
                        mm(psg[g][:, col:col + 8], kcT[:, slot, h, blk_i * 128:(blk_i + 1) * 128],
                           bfp[:, qc_b, h * 128 + c0:h * 128 + c0 + cn], True, True, [key + "k", bk(qc_b)], [gk(g)])
                pb = bpool.get()
                act(bfp[:, pb, 0:64], psg[g][:, 0:64], AF.Exp, [gk(g)], [bk(pb)])
                gpool.put(g)
                for h in range(4):
                    for blk_i in range(2):
                        col = (h * 2 + blk_i) * 8
                        mm(psg[goc][:, h * 128 + c0:h * 128 + c0 + cn], vc[:, slot, blk_i, h * 128:(h + 1) * 128],
                           bfp[:, pb, col:col + 8], blk_i == 0, blk_i == 1,
                           [key + "v", bk(pb)], [gk(goc)])
                        mm(psg[gl][:, h * 128 + c0:h * 128 + c0 + cn], ones_bf[:, :], bfp[:, pb, col:col + 8],
                           blk_i == 0, blk_i == 1, ["ones_bf", bk(pb)], [gk(gl)])
                bpool.put(pb)
            f = fpool.get()
            recip(f32p[:, f, :], psg[gl][:, :], [gk(gl)], [fk(f)])
            gpool.put(gl)
            tt("dve", ocT[:, :, 0:128], psg[goc][:, :].rearrange("p (h t) -> p h t", h=4),
               f32p[:, f, :].rearrange("p (h t) -> p h t", h=4), ALU.mult, [gk(goc), fk(f)], ["ocT"])
            gpool.put(goc)
            fpool.put(f)

        def experts_phase():
            XeT = kAT[:, 0:4, :].rearrange("p a (k c) -> p (a k) c", c=512)
            Wv = vA[:, :, :].rearrange("p a c -> p (a c)")

            def views(e_):
                sl = e_ % 2
                base = sl * 6144
                return (sl, Wv[:, base:base + 2048].rearrange("p (k f) -> p k f", k=8),
                        Wv[:, base + 2048:base + 4096].rearrange("p (k f) -> p k f", k=8),
                        Wv[:, base + 4096:base + 6144].rearrange("p (k c) -> p k c", k=2))

            def loads_w(e_):
                sl, wg_v, wu_v, wd_v = views(e_)
                wkey = "ew%d" % sl
                dma("q_pool", wg_v, w_eg[e_].rearrange("(k p) f -> p k f", p=128), (), [wkey + "g"])
                dma("q_pool", wu_v, w_eu[e_].rearrange("(k p) f -> p k f", p=128), (), [wkey + "u"])
                dma("q_pool", wd_v, w_ed[e_].rearrange("(k p) c -> p k c", p=128), (), [wkey + "d"])

            def loads_x(e_):
                xs_slots = []
                for sb_ in range(4):
                    bx = bpool.get()
                    dma("q_sp", bfp[:, bx, :], xsc[e_ * CAP + sb_ * 128:e_ * CAP + (sb_ + 1) * 128, :], ["xsc"],
                        [bk(bx)])
                    xs_slots.append(bx)
                return xs_slots

            def tstage(e_, xs_slots):
                sl, wg_v, wu_v, wd_v = views(e_)
                xkey = "xe%d" % sl
                for sb_, bx in enumerate(xs_slots):
                    t = trpool.get()
                    for k in range(8):
                        tr(pstr[t][:, k * 128:(k + 1) * 128], bfp[:, bx, k * 128:(k + 1) * 128], [bk(bx)], [tk(t)])
                    cp("dve" if sb_ % 2 == 0 else "act", XeT[:, sl * 8:(sl + 1) * 8, sb_ * 128:(sb_ + 1) * 128],
                       pstr[t][:, :].rearrange("p (k c) -> p k c", k=8), [tk(t)], [xkey])
                    trpool.put(t)
                    bpool.put(bx)

            def compute(e_):
                sl, wg_v, wu_v, wd_v = views(e_)
                wkey = "ew%d" % sl
                xkey = "xe%d" % sl
                hb = bpool.get()
                for fc in range(2):
                    gg = gpool.get()
                    gu = gpool.get()
                    for k in range(8):
                        mm(psg[gg][:, :], wg_v[:, k, fc * 128:(fc + 1) * 128], XeT[:, sl * 8 + k, :], k == 0, k == 7,
                           [wkey + "g", xkey], [gk(gg)])
                    for k in range(8):
                        mm(psg[gu][:, :], wu_v[:, k, fc * 128:(fc + 1) * 128], XeT[:, sl * 8 + k, :], k == 0, k == 7,
                           [wkey + "u", xkey], [gk(gu)])
                    f = fpool.get()
                    act(f32p[:, f, :], psg[gg][:, :], AF.Silu, [gk(gg)], [fk(f)])
                    gpool.put(gg)
                    tt("dve", bfp[:, hb, fc * 512:(fc + 1) * 512], psg[gu][:, :], f32p[:, f, :], ALU.mult,
                       [gk(gu), fk(f)], [bk(hb)])
                    gpool.put(gu)
                    fpool.put(f)
                stores = []
                for sb_ in range(4):
                    x = xpool.get()
                    for hf in range(2):
                        g = gpool.get()
                        for fc in range(2):
                            mm(psg[g][:, :], bfp[:, hb, fc * 512 + sb_ * 128:fc * 512 + (sb_ + 1) * 128],
                               wd_v[:, fc, hf * 512:(hf + 1) * 512], fc == 0, fc == 1, [bk(hb), wkey + "d"], [gk(g)])
                        if hf == 0:
                            cp("act", xpl[:, x, 0:512], psg[g][:, :], [gk(g)], [xk(x)])
                        else:
                            cp("dve", xpl[:, x, 512:1024], psg[g][:, :], [gk(g)], [xk(x)])
                        gpool.put(g)
                    dma("q_sp", ysc[e_ * CAP + sb_ * 128:e_ * CAP + (sb_ + 1) * 128, :], xpl[:, x, :], [xk(x)],
                        ["ysc"])
                    xpool.put(x)
                bpool.put(hb)

            loads_w(0)
            tstage(0, loads_x(0))
            nxt = loads_x(1)
            for e_ in range(NEXP):
                if e_ + 1 < NEXP:
                    loads_w(e_ + 1)
                    tstage(e_ + 1, nxt)
                if e_ + 2 < NEXP:
                    nxt = loads_x(e_ + 2)
                compute(e_)

        def combine_phase():
            npair = NF // 2
            ntl = state["tile_no"]

            def loads(tn):
                x = xpool.get()
                dma("q_sp", xpl[:, x, :], h2s[tn * 128:(tn + 1) * 128, :], ["h2s"], [xk(x)])
                ys = []
                for j in range(2):
                    pi = (tn * 2 + j) % npair
                    yv = f32p[:, 2 * pi:2 * pi + 2, :].rearrange("p a c -> p (a c)")
                    keys = [fk(2 * pi), fk(2 * pi + 1)]
                    S.op("q_pool", lambda e, tn=tn, j=j, yv=yv: e.indirect_dma_start(
                        out=yv, out_offset=None, in_=ysc[:, :],
                        in_offset=bass.IndirectOffsetOnAxis(ap=dsti[:, tn, j:j + 1], axis=0)),
                        ["ysc", "dsti"], keys)
                    ys.append((yv, keys))
                return x, ys

            def finish(tn, x, ys):
                for j, (yv, keys) in enumerate(ys):
                    stt(xpl[:, x, :], yv, wts[:, tn, j:j + 1], xpl[:, x, :], ALU.mult, ALU.add,
                        keys + ["wts", xk(x)], [xk(x)])
                if tn < NPS * 16:
                    sq, tt_ = tn // 16, tn % 16
                    dst = yp[sq, tt_ * 128:(tt_ + 1) * 128, :]
                else:
                    dst = ysm[:, :]
                dma("q_sp", dst, xpl[:, x, :], [xk(x)], ["yout%d" % tn])
                xpool.put(x)

            if ntl == 0:
                return
            nxt = loads(0)
            for tn in range(ntl):
                cur = nxt
                if tn + 1 < ntl:
                    nxt = loads(tn + 1)
                finish(tn, *cur)

        mset("pool", big1[:], 0.0, ["big1"])
        for r in range(NEXP * CAP // 512):
            dma("q_sp", xsc[r * 512:(r + 1) * 512, :].rearrange("(p j) d -> p (j d)", j=4),
                big1[:].rearrange("p a c -> p (a c)"), ["big1"], ["xsc"])
        for sq in range(DBG["nseq"]):
            prompt_seq(sq)
        if DBG["sample"]:
            S.barrier(lambda e: e.memset(eps_c[:], EPS))
            sample_tile()
        if DBG["moe"]:
            S.barrier(lambda e: e.memset(eps_c[:], EPS))
            experts_phase()
            S.barrier(lambda e: e.memset(eps_c[:], EPS))
            combine_phase()
        if DBG.get("dump"):
            DBG["_in_dump"] = True
            DBG["dump"](locals())
        S.emit(nc)
    return nc


_PROG = {}


def _get_prog():
    if "nc" not in _PROG:
        _PROG["nc"] = build_program()
    return _PROG["nc"]


def kernel(x_prompt, x_sample, mem_prompt, cache_win1, cache_win2, cache_win3, state_gla, cache_mem,
           norm_mix, w_in, qn_a, kn_a, qn_c, kn_c, gla_gate_up, gla_gate_bias, gla_norm, mem_norm,
           w_mem_kv, w_branch_a, w_branch_b, w_branch_c, w_out, norm_ffn, w_router_group, b_router_group,
           w_router_expert, b_router_expert, w_exp_gate, w_exp_up, w_exp_down):
    nc = _get_prog()
    f = lambda a: np.ascontiguousarray(np.asarray(a, dtype=np.float32))
    shared = {
        "norm_mix": f(norm_mix), "w_in": f(w_in), "qn_a": f(qn_a), "kn_a": f(kn_a), "qn_c": f(qn_c), "kn_c": f(kn_c),
        "gla_gate_up": f(gla_gate_up), "gla_gate_bias": f(gla_gate_bias), "gla_norm": f(gla_norm),
        "mem_norm": f(mem_norm), "w_mem_kv": f(w_mem_kv), "w_branch_a": f(w_branch_a), "w_branch_b": f(w_branch_b),
        "w_branch_c": f(w_branch_c), "w_out": f(w_out), "norm_ffn": f(norm_ffn), "w_router_group": f(w_router_group),
        "b_router_group": f(b_router_group), "w_router_expert": f(w_router_expert),
        "b_router_expert": f(b_router_expert), "w_exp_gate": f(w_exp_gate), "w_exp_up": f(w_exp_up),
        "w_exp_down": f(w_exp_down),
    }
    x_prompt, x_sample, mem_prompt = f(x_prompt), f(x_sample), f(mem_prompt)
    cache_win1, cache_win2, cache_win3 = f(cache_win1), f(cache_win2), f(cache_win3)
    state_gla, cache_mem = f(state_gla), f(cache_mem)
    in_maps = []
    for c in range(NCORES):
        m = dict(shared)
        m["xp"] = x_prompt[c * NPS:(c + 1) * NPS]
        m["xs"] = x_sample[c * NSB:(c + 1) * NSB].reshape(128, D)
        m["memp"] = mem_prompt[c * NPS:(c + 1) * NPS]
        m["cw1"] = cache_win1[c * NSB:(c + 1) * NSB]
        m["cw2"] = cache_win2[c * NSB:(c + 1) * NSB]
        m["cw3"] = cache_win3[c * NSB:(c + 1) * NSB]
        m["sgla"] = state_gla[c * NSB:(c + 1) * NSB]
        m["cmem"] = cache_mem[c * NSB:(c + 1) * NSB]
        in_maps.append(m)
    ncr = DBG["ncores"]
    res = run_bass_kernel_spmd(nc, in_maps[:ncr], core_ids=list(range(ncr)))
    R = res.results
    cat = lambda k: np.concatenate([np.asarray(r[k]) for r in R], axis=0)
    y_prompt = cat("yp")
    y_sample = cat("ys").reshape(NCORES * NSB, LS, D)
    return (y_prompt, y_sample, cat("w1p"), cat("w2p"), cat("w3p"), cat("glap"), cat("memkv"),
            cat("w1s"), cat("w2s"), cat("w3s"), cat("glas"))
```

```python
import contextlib
import numpy as np
import concourse.bass as bass
import concourse.mybir as mybir
from concourse.bass_utils import run_bass_kernel_spmd

F32 = mybir.dt.float32
BF16 = mybir.dt.bfloat16
I32 = mybir.dt.int32
AF = mybir.ActivationFunctionType
ALU = mybir.AluOpType
AX = mybir.AxisListType

NCORES = 8
D = 1024
SEQ = 2048
NPS = 2
NSB = 16
LS = 8
NTOK = NPS * SEQ + NSB * LS
NTILE = NTOK // 128
CAP = 512
NEXP = 32
EPS = 1e-6
NEG = -30000.0
WIN = (128, 512, 2048)
DIL = (1, 4, 16)
C_QA, C_KA, C_VA, C_QB, C_KB, C_VB, C_RB, C_AB, C_QC, C_GL = 0, 768, 1536, 2304, 2560, 2816, 3328, 3840, 3856, 4368

COMPUTE = ("pe", "act", "dve", "pool")
QUEUES = ("q_sp", "q_act", "q_pool")
Q_HOST = {"q_sp": "sp", "q_act": "act", "q_pool": "pool"}
RING = {"q_sp": 16, "q_act": 8, "q_pool": 12}


class Op:
    __slots__ = ("eng", "fn", "deps", "idx", "signal", "is_dma", "dma_no", "stream", "where")


class Sched:
    def __init__(self):
        self.ops = []
        self.last_w = {}
        self.readers = {}
        self.streams = {"pe": [], "act": [], "dve": [], "pool": [], "sp": []}
        self.dma_count = {q: 0 for q in QUEUES}
        self.barrier_dep = None

    def op(self, eng, fn, reads=(), writes=()):
        mx = DBG.get("maxops")
        if mx is not None and len(self.ops) >= mx and not DBG.get("_in_dump"):
            return None
        is_dma = eng in QUEUES
        psr = [k for k in reads if k.startswith("ps")]
        if psr:
            reads = [k for k in reads if not k.startswith("ps")]
            writes = list(writes) + [k for k in psr if k not in writes]
        deps = set()
        if self.barrier_dep is not None:
            deps.add(self.barrier_dep)
        for k in reads:
            w = self.last_w.get(k)
            if w is not None:
                deps.add(w)
        for k in writes:
            w = self.last_w.get(k)
            if w is not None:
                deps.add(w)
            for r in self.readers.get(k, ()):
                deps.add(r)
        o = Op()
        o.eng, o.fn, o.deps, o.is_dma, o.signal, o.dma_no = eng, fn, deps, is_dma, False, None
        o.idx = len(self.ops)
        o.where = None
        if DBG.get("trace"):
            import sys as _sys
            fr = _sys._getframe(1)
            w = []
            while fr is not None and len(w) < 4:
                w.append("%s:%d" % (fr.f_code.co_name, fr.f_lineno))
                fr = fr.f_back
            o.where = " < ".join(w)
        self.ops.append(o)
        o.stream = Q_HOST[eng] if is_dma else eng
        if is_dma:
            o.dma_no = self.dma_count[eng]
            self.dma_count[eng] += 1
        self.streams[o.stream].append(o)
        for k in writes:
            self.last_w[k] = o.idx
            self.readers[k] = []
        for k in reads:
            if k not in writes:
                self.readers.setdefault(k, []).append(o.idx)
        return o

    def barrier(self, nop_fn):
        deps = set()
        for st in self.streams.values():
            if st:
                deps.add(st[-1].idx)
        for q in QUEUES:
            n = self.dma_count[q]
            cnt = 0
            for o in reversed(self.ops):
                if o.is_dma and o.eng == q:
                    deps.add(o.idx)
                    cnt += 1
                    if cnt >= RING[q]:
                        break
        o = self.op("dve", nop_fn)
        if o is None:
            return
        o.deps |= deps
        o.deps.discard(o.idx)
        self.barrier_dep = o.idx
        self.last_w = {}
        self.readers = {}

    def emit(self, nc):
        ops = self.ops
        for o in ops:
            for d in o.deps:
                p = ops[d]
                if not p.is_dma:
                    p.signal = True
        sig_count = {}
        cnt = {e: 0 for e in COMPUTE}
        for o in ops:
            if not o.is_dma:
                if o.signal:
                    cnt[o.eng] += 1
                sig_count[o.idx] = cnt[o.eng]
        with contextlib.ExitStack() as es:
            sems = {e: es.enter_context(nc.semaphore("s_" + e)) for e in COMPUTE}
            rings = {q: [es.enter_context(nc.semaphore("r_%s_%d" % (q, i))) for i in range(RING[q])]
                     for q in QUEUES}
            block = es.enter_context(nc.Block())
            handles = {"pe": "tensor", "act": "scalar", "dve": "vector", "pool": "gpsimd", "sp": "sync"}

            def run_stream(stream, e):
                waited = {}
                for o in self.streams[stream]:
                    need = {}
                    for d in o.deps:
                        p = ops[d]
                        if p.is_dma:
                            R = RING[p.eng]
                            key = (p.eng, p.dma_no % R)
                            val = 16 * (p.dma_no // R + 1)
                        else:
                            if p.eng == "pe" and stream == "pe":
                                continue
                            key = (p.eng, -1)
                            val = sig_count[p.idx]
                        if need.get(key, 0) < val:
                            need[key] = val
                    if o.is_dma:
                        R = RING[o.eng]
                        if o.dma_no >= R:
                            key = (o.eng, o.dma_no % R)
                            val = 16 * (o.dma_no // R)
                            if need.get(key, 0) < val:
                                need[key] = val
                    for key, val in need.items():
                        if waited.get(key, 0) >= val:
                            continue
                        waited[key] = val
                        sem = sems[key[0]] if key[1] < 0 else rings[key[0]][key[1]]
                        e.wait_ge(sem, val)
                    ins = o.fn(e)
                    if o.is_dma:
                        ins.then_inc(rings[o.eng][o.dma_no % RING[o.eng]], 16)
                    elif o.signal:
                        ins.then_inc(sems[o.eng], 1)
                for q in QUEUES:
                    if Q_HOST[q] != stream:
                        continue
                    n = self.dma_count[q]
                    R = RING[q]
                    for r in range(R):
                        k = (n - r + R - 1) // R
                        if k > 0 and waited.get((q, r), 0) < 16 * k:
                            e.wait_ge(rings[q][r], 16 * k)

            for stream, attr in handles.items():
                if not self.streams[stream]:
                    continue

                def body(e, stream=stream):
                    run_stream(stream, e)
                getattr(block, attr)(body)


class SlotPool:
    def __init__(self, n, key):
        self.free = list(range(n))
        self.key = key

    def get(self):
        assert self.free, "pool %s exhausted" % self.key
        return self.free.pop(0)

    def put(self, i):
        self.free.append(i)


DBG = {"nseq": NPS, "nst": 4, "sample": True, "moe": True, "mix": ("attn", "gla", "cross"), "merge": True,
       "ncores": NCORES}


def build_program():
    nc = bass.Bass("TRN2", target_bir_lowering=False)
    S = Sched()

    def din(name, shape, dt=F32):
        return nc.dram_tensor(name, list(shape), dt, kind="ExternalInput").ap()

    def dout(name, shape, dt=F32):
        return nc.dram_tensor(name, list(shape), dt, kind="ExternalOutput").ap()

    def dint(name, shape, dt=F32):
        return nc.dram_tensor(name, list(shape), dt, kind="Internal").ap()

    xp = din("xp", [NPS, SEQ, D])
    xsm = din("xs", [128, D])
    memp = din("memp", [NPS, 256, D])
    cw = [din("cw1", [NSB, 128, 2, 4, 64]), din("cw2", [NSB, 512, 2, 4, 64]), din("cw3", [NSB, 2048, 2, 4, 64])]
    sgla = din("sgla", [NSB, 4, 64, 128])
    cmem = din("cmem", [NSB, 256, 2, 4, 128])
    norm_mix = din("norm_mix", [D])
    w_in = din("w_in", [D, 7440])
    qn_a = din("qn_a", [64])
    kn_a = din("kn_a", [64])
    qn_c = din("qn_c", [128])
    kn_c = din("kn_c", [128])
    gup = din("gla_gate_up", [16, 256])
    gbias = din("gla_gate_bias", [256])
    gla_norm = din("gla_norm", [128])
    mem_norm = din("mem_norm", [D])
    w_mem_kv = din("w_mem_kv", [D, 1024])
    w_br = [din("w_branch_a", [256, D]), din("w_branch_b", [512, D]), din("w_branch_c", [512, D])]
    w_out = din("w_out", [D, D])
    norm_ffn = din("norm_ffn", [D])
    w_rg = din("w_router_group", [D, 4])
    b_rg = din("b_router_group", [4])
    w_re = din("w_router_expert", [D, 32])
    b_re = din("b_router_expert", [32])
    w_eg = din("w_exp_gate", [NEXP, D, 256])
    w_eu = din("w_exp_up", [NEXP, D, 256])
    w_ed = din("w_exp_down", [NEXP, 256, D])

    yp = dout("yp", [NPS, SEQ, D])
    ysm = dout("ys", [128, D])
    wp = [dout("w1p", [NPS, 128, 2, 4, 64]), dout("w2p", [NPS, 512, 2, 4, 64]), dout("w3p", [NPS, 2048, 2, 4, 64])]
    glap = dout("glap", [NPS, 4, 64, 128])
    memkv = dout("memkv", [NPS, 256, 2, 4, 128])
    ws = [dout("w1s", [NSB, 128, 2, 4, 64]), dout("w2s", [NSB, 512, 2, 4, 64]), dout("w3s", [NSB, 2048, 2, 4, 64])]
    glas = dout("glas", [NSB, 4, 64, 128])

    h2s = dint("h2s", [NTOK, D])
    wbf_in = dint("wbf_in", [D, 7440], BF16)
    wbf_out = dint("wbf_out", [D, D], BF16)
    wbf_br = [dint("wbf_br%d" % i, [r, D], BF16) for i, r in enumerate((256, 512, 512))]
    wbf_mem = dint("wbf_mem", [D, 1024], BF16)
    xsc = dint("xsc", [NEXP * CAP, D], BF16)
    ysc = dint("ysc", [NEXP * CAP, D])

    es = contextlib.ExitStack()
    with es:
        def sb(name, shape, dt):
            return es.enter_context(nc.sbuf_tensor(name, list(shape), dt))

        def pst(name, shape, dt):
            return es.enter_context(nc.psum_tensor(name, list(shape), dt))

        NG = 4
        psg = [pst("psg%d" % i, [128, 512], F32) for i in range(NG)]
        pstr = [pst("pstr%d" % i, [128, 1024], BF16) for i in range(2)]
        psN = pst("psN", [128, 512], F32)
        psL = pst("psL", [128, 512], F32)
        gpool = SlotPool(NG, "psg")
        trpool = SlotPool(2, "pstr")

        NF = 12
        NB = 10
        NX = 3
        NW = 3
        WCOLS = 512
        f32p = sb("f32p", [128, NF, 512], F32)
        bfp = sb("bfp", [128, NB, 1024], BF16)
        xpl = sb("xpl", [128, NX, 1024], F32)
        wbuf = sb("wbuf", [128, NW, 8, WCOLS], BF16)
        smallp = sb("smallp", [128, 96, 8], F32)
        fpool, bpool, xpool, wpool, spool = (SlotPool(NF, "f"), SlotPool(NB, "b"), SlotPool(NX, "x"),
                                             SlotPool(NW, "w"), SlotPool(96, "s"))
        xnT = sb("xnT", [128, 8, 512], BF16)
        kAT = sb("kAT", [128, 6, SEQ], BF16)
        vA = sb("vA", [128, 16, 768], BF16)
        qAT = sb("qAT", [128, 6, 512], BF16)
        big1 = sb("big1", [128, 8, 512], BF16)
        vb_bf = big1[:, 0:4, :]
        rbT = big1[:, 4:8, :]
        mergedT = big1[:, :, :]
        oaT = sb("oaT", [64, 4, 512], BF16)
        ob2T = sb("ob2T", [128, 4, 512], BF16)
        ocT = sb("ocT", [128, 4, 512], BF16)
        wbr_sb = [sb("wbr_a", [64, 2, 4, 128], BF16), sb("wbr_b", [128, 2, 4, 128], BF16),
                  sb("wbr_c", [128, 2, 4, 128], BF16)]
        wab = sb("wab", [128, 8, 16], BF16)
        S32 = sb("S32", [128, 2, 2, 128], F32)
        Sbf = sb("Sbf", [128, 2, 2, 128], BF16)
        kcT = sb("kcT", [128, 2, 4, 256], BF16)
        vc = sb("vc", [128, 2, 2, 512], BF16)
        kcache = kAT[:, 0:2, 128:1152].rearrange("p s (kt c) -> p s kt c", kt=4)
        vcache = kAT[:, 2:4, 128:1152].rearrange("p s (kt c) -> p s kt c", kt=4)
        kcTA = kAT[:, 4:6, 128:1152].rearrange("p s (kt pr c) -> p s kt pr c", kt=4, pr=2)
        wts = sb("wts", [128, NTILE, 2], F32)
        dsti = sb("dsti", [128, NTILE, 2], I32)
        rbase = sb("rbase", [128, 32], F32)
        ident = sb("ident", [128, 128], BF16)
        cf32 = sb("cf32", [128, 128], F32)
        ones_bf = sb("ones_bf", [128, 128], BF16)
        zeros_bf = sb("zeros_bf", [128, 128], BF16)
        stri_bf = sb("stri_bf", [128, 128], BF16)
        onesdiv = sb("onesdiv", [128, 128], F32)
        tri_p = sb("tri_p", [128, 128], F32)
        tri_s = sb("tri_s", [128, 128], F32)
        blk_p = sb("blk_p", [128, 16], F32)
        blk_s = sb("blk_s", [128, 16], F32)
        E_r = {4: sb("E4", [4, 32, 4], F32), 16: sb("E16", [16, 8, 16], F32)}
        masks = sb("masks", [128, 9, 128], BF16)
        maskS = sb("maskS", [128, 3, 128], BF16)
        maskC = sb("maskC", [128, 6, 4, 8], BF16)
        nmix_rep = sb("nmix_rep", [128, D], F32)
        nffn_rep = sb("nffn_rep", [128, D], F32)
        qna_rep = sb("qna_rep", [128, 64], F32)
        kna_rep = sb("kna_rep", [128, 64], F32)
        qnc_rep = sb("qnc_rep", [128, 128], F32)
        knc_rep = sb("knc_rep", [128, 128], F32)
        gbias_rep = sb("gbias_rep", [128, 256], F32)
        rbias_rep = sb("rbias_rep", [128, 36], F32)
        ecap = sb("ecap", [128, 32], F32)
        gnorm_c = sb("gnorm_c", [128, 1], F32)
        eps_c = sb("eps_c", [128, 1], F32)
        gup_bf = sb("gup_bf", [16, 256], BF16)
        wr_bf = sb("wr_bf", [128, 8, 36], BF16)

        def fk(i): return "f%d" % i
        def bk(i): return "b%d" % i
        def xk(i): return "x%d" % i
        def wk(i): return "w%d" % i
        def sk(i): return "s%d" % i
        def gk(i): return "psg%d" % i
        def tk(i): return "pstr%d" % i

        def mm(out, lhsT, rhs, start, stop, reads, writes):
            S.op("pe", lambda e: e.matmul(out, lhsT=lhsT, rhs=rhs, start=start, stop=stop, skip_group_check=True),
                 reads, writes)

        def tr(out, in_, reads, writes):
            S.op("pe", lambda e: e.transpose(out=out, in_=in_, identity=ident[:]), list(reads) + ["ident"], writes)

        def act(out, in_, func, reads, writes, bias=None, scale=None, accum_out=None):
            kw = {}
            if bias is not None:
                kw["bias"] = bias
            if scale is not None:
                kw["scale"] = scale
            if accum_out is not None:
                kw["accum_out"] = accum_out
            S.op("act", lambda e: e.activation(out=out, in_=in_, func=func, **kw), reads, writes)

        def tt(eng, out, in0, in1, op, reads, writes):
            S.op(eng, lambda e: e.tensor_tensor(out=out, in0=in0, in1=in1, op=op), reads, writes)

        def ts(out, in0, s1, s2, op0, op1, reads, writes):
            if s2 is None:
                S.op("dve", lambda e: e.tensor_scalar(out=out, in0=in0, scalar1=s1, scalar2=None, op0=op0),
                     reads, writes)
            else:
                S.op("dve", lambda e: e.tensor_scalar(out=out, in0=in0, scalar1=s1, scalar2=s2, op0=op0, op1=op1),
                     reads, writes)

        def stt(out, in0, scalar, in1, op0, op1, reads, writes):
            S.op("dve", lambda e: e.scalar_tensor_tensor(out=out, in0=in0, scalar=scalar, in1=in1, op0=op0, op1=op1),
                 reads, writes)

        def cp(eng, out, in_, reads, writes):
            if eng == "act":
                S.op("act", lambda e: e.activation(out=out, in_=in_, func=AF.Copy), reads, writes)
            else:
                S.op(eng, lambda e: e.tensor_copy(out=out, in_=in_), reads, writes)

        def recip(out, in_, reads, writes):
            S.op("dve", lambda e: e.reciprocal(out=out, in_=in_), reads, writes)

        def red(out, in_, op, reads, writes):
            S.op("dve", lambda e: e.tensor_reduce(out=out, in_=in_, axis=AX.X, op=op), reads, writes)

        def mset(eng, ap, val, writes):
            S.op(eng, lambda e: e.memset(ap, val), (), writes)

        def dma(q, out, in_, reads, writes):
            S.op(q, lambda e: e.dma_start(out=out, in_=in_), reads, writes)

        def asel(out, in_, pattern, cmp_op, fill, base, cm, reads, writes):
            S.op("pool", lambda e: e.affine_select(out=out, in_=in_, pattern=pattern, compare_op=cmp_op, fill=fill,
                                                   base=base, channel_multiplier=cm), reads, writes)

        mset("pool", eps_c[:], EPS, ["eps_c"])
        mset("pool", cf32[:], 1.0, ["cf32"])
        asel(cf32[:], cf32[:], [[-1, 128]], ALU.is_equal, 0.0, 0, 1, ["cf32"], ["cf32"])
        cp("dve", ident[:], cf32[:], ["cf32"], ["ident"])
        mset("pool", ones_bf[:], 1.0, ["ones_bf"])
        mset("pool", zeros_bf[:], 0.0, ["zeros_bf"])
        mset("pool", onesdiv[:], 1.0 / 128, ["onesdiv"])
        mset("pool", tri_p[:], 1.0, ["tri_p"])
        asel(tri_p[:], tri_p[:], [[1, 128]], ALU.is_ge, 0.0, 0, -1, ["tri_p"], ["tri_p"])
        mset("pool", cf32[:], 1.0, ["cf32"])
        asel(cf32[:], cf32[:], [[1, 128]], ALU.is_gt, 0.0, 0, -1, ["cf32"], ["cf32"])
        cp("dve", stri_bf[:], cf32[:], ["cf32"], ["stri_bf"])
        cp("pool", tri_s[:], tri_p[:], ["tri_p"], ["tri_s"])
        asel(tri_s[:].rearrange("p (b l) -> p b l", l=8), tri_s[:].rearrange("p (b l) -> p b l", l=8),
             [[-8, 16], [0, 8]], ALU.is_ge, 0.0, 0, 1, ["tri_s"], ["tri_s"])
        mset("pool", blk_p[:], 0.0, ["blk_p"])
        mset("pool", blk_p[:, 0:1], 1.0, ["blk_p"])
        mset("pool", blk_s[:], 1.0, ["blk_s"])
        asel(blk_s[:], blk_s[:], [[-8, 16]], ALU.is_ge, 0.0, 0, 1, ["blk_s"], ["blk_s"])
        asel(blk_s[:], blk_s[:], [[8, 16]], ALU.is_ge, 0.0, 7, -1, ["blk_s"], ["blk_s"])
        ecap_i = sb("ecap_i", [128, 32], I32)
        S.op("pool", lambda e: e.iota(ecap_i[:], pattern=[[CAP, 32]], base=0, channel_multiplier=0), (), ["ecap_i"])
        cp("dve", ecap[:], ecap_i[:], ["ecap_i"], ["ecap"])
        mset("pool", rbase[:], 0.0, ["rbase"])
        dma("q_sp", nmix_rep[:], norm_mix.partition_broadcast(128), (), ["nmix_rep"])
        dma("q_sp", nffn_rep[:], norm_ffn.partition_broadcast(128), (), ["nffn_rep"])
        dma("q_sp", qna_rep[:], qn_a.partition_broadcast(128), (), ["qna_rep"])
        dma("q_sp", kna_rep[:], kn_a.partition_broadcast(128), (), ["kna_rep"])
        dma("q_sp", qnc_rep[:], qn_c.partition_broadcast(128), (), ["qnc_rep"])
        dma("q_sp", knc_rep[:], kn_c.partition_broadcast(128), (), ["knc_rep"])
        dma("q_sp", gbias_rep[:], gbias.partition_broadcast(128), (), ["gbias_rep"])
        dma("q_sp", rbias_rep[:, 0:4], b_rg.partition_broadcast(128), (), ["rbias_rep"])
        dma("q_sp", rbias_rep[:, 4:36], b_re.partition_broadcast(128), ["rbias_rep"], ["rbias_rep"])
        dma("q_sp", gnorm_c[:], gla_norm.rearrange("(p o) -> p o", o=1), (), ["gnorm_c"])
        ts(qna_rep[:], qna_rep[:], 0.125, None, ALU.mult, None, ["qna_rep"], ["qna_rep"])
        ts(qnc_rep[:], qnc_rep[:], float(128 ** -0.5), None, ALU.mult, None, ["qnc_rep"], ["qnc_rep"])
        dma("q_pool", gup_bf[:], gup, (), ["gup_bf"])
        dma("q_pool", wr_bf[:, :, 0:4], w_rg.rearrange("(k p) c -> p k c", p=128), (), ["wr_bf"])
        dma("q_pool", wr_bf[:, :, 4:36], w_re.rearrange("(k p) c -> p k c", p=128), ["wr_bf"], ["wr_bf"])
        dma("q_pool", wab[:], w_in[:, C_AB:C_AB + 16].rearrange("(k p) c -> p k c", p=128), (), ["wab"])
        for gi, r in enumerate(DIL):
            g = gpool.get()
            if r == 1:
                mset("dve", psg[g][:, 0:128], 1.0, [gk(g)])
            else:
                er = E_r[r]
                mset("pool", er[:], 1.0, ["E%d" % r])
                asel(er[:], er[:], [[0, 128 // r], [1, r]], ALU.is_equal, 0.0, 0, -1, ["E%d" % r], ["E%d" % r])
                er2 = er[:].rearrange("p a b -> p (a b)")
                mm(psg[g][:, 0:128], er2, er2, True, True, ["E%d" % r], [gk(g)])
            f = fpool.get()
            ts(f32p[:, f, 0:128], psg[g][:, 0:128], -1.0, -NEG, ALU.add, ALU.mult, [gk(g)], [fk(f)])
            gpool.put(g)
            cp("dve", masks[:, 3 * gi + 1, :], f32p[:, f, 0:128], [fk(f)], ["masks"])
            f2 = fpool.get()
            asel(f32p[:, f2, 0:128], f32p[:, f, 0:128], [[1, 128]], ALU.is_ge, NEG, 0, -1, [fk(f)], [fk(f2)])
            cp("dve", masks[:, 3 * gi + 0, :], f32p[:, f2, 0:128], [fk(f2)], ["masks"])
            asel(f32p[:, f2, 0:128], f32p[:, f, 0:128], [[-1, 128]], ALU.is_ge, NEG, 0, 1, [fk(f)], [fk(f2)])
            cp("dve", masks[:, 3 * gi + 2, :], f32p[:, f2, 0:128], [fk(f2)], ["masks"])
            asel(f32p[:, f2, 0:128], f32p[:, f, 0:128], [[1, 128]], ALU.is_ge, NEG, 0, -1, [fk(f)], [fk(f2)])
            v3 = f32p[:, f2, 0:128].rearrange("p (b l) -> p b l", l=8)
            asel(v3, v3, [[-8, 16], [0, 8]], ALU.is_ge, NEG, 0, 1, [fk(f2)], [fk(f2)])
            cp("dve", maskS[:, gi, :], f32p[:, f2, 0:128], [fk(f2)], ["maskS"])
            fpool.put(f)
            fpool.put(f2)
            for hh in range(4):
                cp("pool", maskC[:, 2 * gi + 0, hh, :], masks[:, 3 * gi + 2, 0:8], ["masks"], ["maskC"])
                cp("pool", maskC[:, 2 * gi + 1, hh, :], masks[:, 3 * gi + 1, 0:8], ["masks"], ["maskC"])

        def rms_stats(src_ap, n, reads, tag):
            s = spool.get()
            jb = bpool.get()
            mset("pool", smallp[:, s, 0:1], 0.0, [sk(s)])
            act(bfp[:, jb, 0:n], src_ap, AF.Square, list(reads) + [sk(s)], [bk(jb), sk(s)],
                accum_out=smallp[:, s, 0:1])
            bpool.put(jb)
            act(smallp[:, s, 1:2], smallp[:, s, 0:1], AF.Sqrt, [sk(s), "eps_c"], [sk(s)], bias=eps_c[:], scale=1.0 / n)
            recip(smallp[:, s, 0:1], smallp[:, s, 1:2], [sk(s)], [sk(s)])
            return s

        def transposes_to(dst3, src2, nblk, reads, writes):
            t = trpool.get()
            for j in range(nblk):
                tr(pstr[t][:, j * 128:(j + 1) * 128], src2[:, j * 128:(j + 1) * 128], reads, [tk(t)])
            cp("dve", dst3, pstr[t][:, 0:nblk * 128].rearrange("p (k c) -> p k c", k=nblk), [tk(t)], writes)
            trpool.put(t)

        def front_a(x_src, grep, grep_key):
            x = xpool.get()
            dma("q_sp", xpl[:, x, :], x_src, (), [xk(x)])
            s = rms_stats(xpl[:, x, :], D, [xk(x)], "fr")
            b = bpool.get()
            stt(bfp[:, b, :], xpl[:, x, :], smallp[:, s, 0:1], grep, ALU.mult, ALU.mult,
                [xk(x), sk(s), grep_key], [bk(b)])
            spool.put(s)
            xpool.put(x)
            return b

        def front_b(b, dstT, dst_key):
            transposes_to(dstT, bfp[:, b, :], 8, [bk(b)], [dst_key])
            bpool.put(b)

        def front(x_src, grep, grep_key, dstT, dst_key, keep_x=False):
            front_b(front_a(x_src, grep, grep_key), dstT, dst_key)

        def wload(src3, kk, cols):
            src3, skey = src3
            w = wpool.get()
            dma("q_pool", wbuf[:, w, 0:kk, 0:cols], src3, [skey], [wk(w)])
            return w

        conv_blocks = ([(C_QA + i * 256, 256) for i in range(9)] + [(C_VB, 512), (C_RB, 512), (C_QC, 512), (C_QB, 512)]
                       + [(C_GL + i * 512, 512) for i in range(6)])
        for (c0, cols) in conv_blocks:
            dma("q_pool", wbf_in[:, c0:c0 + cols], w_in[:, c0:c0 + cols], (), ["wbfin%d" % c0])
        for b_ in range(3):
            dma("q_pool", wbf_br[b_][:, :], w_br[b_][:, :], (), ["wbfbr%d" % b_])
        for hf in range(2):
            dma("q_pool", wbf_out[:, hf * 512:(hf + 1) * 512], w_out[:, hf * 512:(hf + 1) * 512], (),
                ["wbfout%d" % hf])

        def w_in_blk(c0, cols):
            return (wbf_in[:, c0:c0 + cols].rearrange("(k p) c -> p k c", p=128), "wbfin%d" % c0)

        def proj_tok(actT, act_key, t0, w, cols, ps_ap, ps_key, wc0=0):
            for k in range(8):
                mm(ps_ap, actT[:, k, t0:t0 + 128], wbuf[:, w, k, wc0:wc0 + cols], k == 0, k == 7,
                   [act_key, wk(w)], [ps_key])

        def headnorm(ps_ap, ps_key, nh, hd, grep, grep_key, outs):
            f = fpool.get()
            s = spool.get()
            act(f32p[:, f, 0:nh * hd], ps_ap, AF.Square, [ps_key], [fk(f)])
            red(smallp[:, s, 0:nh], f32p[:, f, 0:nh * hd].rearrange("p (h d) -> p h d", h=nh), ALU.add,
                [fk(f)], [sk(s)])
            fpool.put(f)
            s2 = spool.get()
            act(smallp[:, s2, 0:nh], smallp[:, s, 0:nh], AF.Sqrt, [sk(s), "eps_c"], [sk(s2)], bias=eps_c[:],
                scale=1.0 / hd)
            recip(smallp[:, s, 0:nh], smallp[:, s2, 0:nh], [sk(s2)], [sk(s)])
            spool.put(s2)
            for (dst3, dkey) in outs:
                for h in range(nh):
                    stt(dst3[:, h, :], ps_ap[:, h * hd:(h + 1) * hd], smallp[:, s, h:h + 1], grep[:, 0:hd],
                        ALU.mult, ALU.mult, [ps_key, sk(s), grep_key], [dkey])
            spool.put(s)

        state = {"tile_no": 0}

        def attn_blocks(blocks, hsel=None):
            for _ in attn_blocks_gen(blocks):
                pass

        def attn_blocks_gen(blocks):
            groups = []
            i = 0
            while i < len(blocks):
                grp = []
                tot = 0
                while i < len(blocks) and tot + blocks[i]["ncol"] <= 512:
                    grp.append((blocks[i], tot))
                    tot += blocks[i]["ncol"]
                    i += 1
                groups.append((grp, tot))

            def pv_stage(grp, pb):
                for (b, c0) in grp:
                    for (v, cc, cn, oN, oL) in b["pv"]:
                        mm(oN, v, bfp[:, pb, c0 + cc:c0 + cc + cn], False, False, [b["vkey"], bk(pb)], ["psN"])
                        mm(oL, ones_bf[:, 0:64], bfp[:, pb, c0 + cc:c0 + cc + cn], False, False,
                           ["ones_bf", bk(pb)], ["psL"])
                bpool.put(pb)

            prev = None
            for (grp, tot) in groups:
                g = gpool.get()
                for (b, c0) in grp:
                    n = b["ncol"]
                    if len(b["sub"]) == 1:
                        mm(psg[g][:, c0:c0 + n], ident[:], b["mask"], True, False, ["ident", b["mkey"]], [gk(g)])
                        (kT, q, cc, cn) = b["sub"][0]
                        mm(psg[g][:, c0 + cc:c0 + cc + cn], kT, q, False, True, [b["kkey"], b["qkey"]], [gk(g)])
                    else:
                        hn = n // 2
                        first = True
                        for half in range(2):
                            mm(psg[g][:, c0 + half * hn:c0 + (half + 1) * hn], ident[:],
                               b["mask"][:, half * hn:(half + 1) * hn], first, False, ["ident", b["mkey"]], [gk(g)])
                            first = False
                            for (kT, q, cc, cn) in b["sub"][half * 2:half * 2 + 2]:
                                mm(psg[g][:, c0 + cc:c0 + cc + cn], kT, q, False, True, [b["kkey"], b["qkey"]],
                                   [gk(g)])
                pb = bpool.get()
                act(bfp[:, pb, 0:tot], psg[g][:, 0:tot], AF.Exp, [gk(g)], [bk(pb)])
                gpool.put(g)
                if prev is not None:
                    pv_stage(*prev)
                prev = (grp, pb)
                yield
            if prev is not None:
                pv_stage(*prev)
            yield

        def attn_finish(t0):
            f = fpool.get()
            recip(f32p[0:64, f, :], psL[0:64, :], ["psL"], [fk(f)])
            tt("dve", oaT[:, :, t0:t0 + 128], psN[0:64, :].rearrange("p (h t) -> p h t", h=4),
               f32p[0:64, f, :].rearrange("p (h t) -> p h t", h=4), ALU.mult, ["psN", fk(f)], ["oaT"])
            fpool.put(f)

        def attn_prompt_tile(qt, t0):
            mset("dve", psN[0:64, :], 0.0, ["psN"])
            mset("dve", psL[0:64, :], 0.0, ["psL"])
            blocks = []
            for h in range(4):
                hp, pr = (h % 2) * 64, h // 2
                for gi in range(3):
                    nd = WIN[gi] // 128
                    ch = gi * 2 + pr
                    for kt in range(max(0, qt - nd), qt + 1):
                        dlt = qt - kt
                        typ = 0 if dlt == 0 else (2 if dlt == nd else 1)
                        blocks.append(dict(
                            mask=masks[:, 3 * gi + typ, :], mkey="masks", kkey="kAT", qkey="qAT", vkey="vA", ncol=128,
                            sub=[(kAT[hp:hp + 64, ch, kt * 128:(kt + 1) * 128], qAT[hp:hp + 64, ch, t0:t0 + 128], 0, 128)],
                            pv=[(vA[:, kt, (gi * 4 + h) * 64:(gi * 4 + h + 1) * 64], 0, 128,
                                 psN[0:64, h * 128:(h + 1) * 128], psL[0:64, h * 128:(h + 1) * 128])]))
            yield from attn_blocks_gen(blocks)
            attn_finish(t0)
            yield

        def run_threads(gens):
            gens = list(gens)
            while gens:
                for gtor in list(gens):
                    try:
                        next(gtor)
                    except StopIteration:
                        gens.remove(gtor)

        def chain(makers):
            for mk in makers:
                yield from mk()

        def gla_tile(ti, t0, tri, tri_key, blk, blk_key, nblk, s_loader, s_store):
            bw = 128 // nblk
            g = gpool.get()
            for k in range(8):
                mm(psg[g][:, 0:16], xnT[:, k, t0:t0 + 128], wab[:, k, :], k == 0, k == 7, ["xnT", "wab"], [gk(g)])
            b1 = bpool.get()
            cp("act", bfp[:, b1, 0:16], psg[g][:, 0:16], [gk(g)], [bk(b1)])
            gpool.put(g)
            yield
            t = trpool.get()
            tr(pstr[t][0:16, 0:128], bfp[:, b1, 0:16], [bk(b1)], [tk(t)])
            cp("dve", bfp[0:16, b1, 128:256], pstr[t][0:16, 0:128], [tk(t)], [bk(b1)])
            trpool.put(t)
            yield
            g = gpool.get()
            mm(psg[g][:, 0:256], bfp[0:16, b1, 128:256], gup_bf[:], True, True, [bk(b1), "gup_bf"], [gk(g)])
            bpool.put(b1)
            fz = fpool.get()
            tt("dve", f32p[:, fz, 0:256], psg[g][:, 0:256], gbias_rep[:], ALU.add, [gk(g), "gbias_rep"], [fk(fz)])
            gpool.put(g)
            act(f32p[:, fz, 256:512], f32p[:, fz, 0:256], AF.Exp, [fk(fz)], [fk(fz)], scale=-1.0)
            act(f32p[:, fz, 0:256], f32p[:, fz, 256:512], AF.Ln, [fk(fz)], [fk(fz)], bias=1.0)
            yield
            g = gpool.get()
            mm(psg[g][:, 0:256], tri, f32p[:, fz, 0:256], True, True, [tri_key, fk(fz)], [gk(g)])
            for pr in range(2):
                mm(psg[g][:, 256 + pr * 16:256 + pr * 16 + nblk], f32p[:, fz, pr * 128:(pr + 1) * 128], blk[:, 0:nblk],
                   True, True, [fk(fz), blk_key], [gk(g)])
            fe = fpool.get()
            act(f32p[:, fe, 0:256], psg[g][:, 0:256], AF.Exp, [gk(g)], [fk(fe)], scale=-1.0 / 16)
            act(f32p[:, fe, 256:512], psg[g][:, 0:256], AF.Exp, [gk(g)], [fk(fe)], scale=1.0 / 16)
            ebl = f32p[:, fz, 256:256 + 32]
            act(ebl, psg[g][:, 256:288], AF.Exp, [gk(g), fk(fz)], [fk(fz)], scale=-1.0 / 16)
            gpool.put(g)
            yield
            w = state["w_qkb"]
            g = gpool.get()
            proj_tok(xnT, "xnT", t0, w, 512, psg[g][:, :], gk(g))
            bq = bpool.get()
            stt(bfp[:, bq, 0:256], psg[g][:, 0:256], 0.125, f32p[:, fe, 0:256], ALU.mult, ALU.mult,
                [gk(g), fk(fe)], [bk(bq)])
            tt("dve", bfp[:, bq, 256:512], psg[g][:, 256:512], f32p[:, fe, 256:512], ALU.mult, [gk(g), fk(fe)],
               [bk(bq)])
            gpool.put(g)
            fpool.put(fe)
            yield
            bT = bpool.get()
            transposes_to(bfp[:, bT, 0:512].rearrange("p (k c) -> p k c", k=4), bfp[:, bq, 0:512], 4, [bk(bq)],
                          [bk(bT)])
            DBG.setdefault("gla_slots", dict(bq=bq, bT=bT, fz=fz))
            yield
            gAB = [gpool.get(), gpool.get()]
            for h in (0, 2, 1, 3):
                hp, pr = (h % 2) * 64, h // 2
                g = gAB[h % 2]
                mm(psg[g][:, pr * 128:(pr + 1) * 128], bfp[hp:hp + 64, bT, 256 + pr * 128:256 + (pr + 1) * 128],
                   bfp[hp:hp + 64, bT, pr * 128:(pr + 1) * 128], True, True, [bk(bT)], [gk(g)])
            ba = bpool.get()
            for h in range(4):
                g = gAB[h % 2]
                pr = h // 2
                tt("dve", bfp[:, ba, h * 128:(h + 1) * 128], psg[g][:, pr * 128:(pr + 1) * 128], tri, ALU.mult,
                   [gk(g), tri_key], [bk(ba)])
            gpool.put(gAB[0])
            gpool.put(gAB[1])
            yield
            go = gpool.get()
            if nblk == 1:
                slot, skey = s_loader(0, "o")
                for h in range(4):
                    hp, pr = (h % 2) * 64, h // 2
                    mm(psg[go][:, h * 128:(h + 1) * 128], vb_bf[:, ti, h * 128:(h + 1) * 128],
                       bfp[:, ba, h * 128:(h + 1) * 128], True, False, ["big1", bk(ba)], [gk(go)])
                    mm(psg[go][:, h * 128:(h + 1) * 128], Sbf[hp:hp + 64, slot, pr, :],
                       bfp[hp:hp + 64, bT, pr * 128:(pr + 1) * 128], False, True, [skey + "bf", bk(bT)], [gk(go)])
            else:
                mset("dve", psg[go][:, :], 0.0, [gk(go)])
                for h in range(4):
                    mm(psg[go][:, h * 128:(h + 1) * 128], vb_bf[:, ti, h * 128:(h + 1) * 128],
                       bfp[:, ba, h * 128:(h + 1) * 128], False, False, ["big1", bk(ba)], [gk(go)])
                for bi in range(nblk):
                    slot, skey = s_loader(bi, "o")
                    for h in (0, 2, 1, 3):
                        hp, pr = (h % 2) * 64, h // 2
                        if h in (0, 1):
                            mm(psg[go][:, 0:8], zeros_bf[:, :], ones_bf[:, 0:8], False, False,
                               ["zeros_bf", "ones_bf"], [gk(go)])
                        mm(psg[go][:, h * 128 + bi * bw:h * 128 + (bi + 1) * bw], Sbf[hp:hp + 64, slot, pr, :],
                           bfp[hp:hp + 64, bT, pr * 128 + bi * bw:pr * 128 + (bi + 1) * bw], False, False,
                           [skey + "bf", bk(bT)], [gk(go)])
            bpool.put(ba)
            yield
            for bi in range(nblk):
                slot, skey = s_loader(bi, "u")
                if nblk == 1:
                    kesrc, kkey, kb_ = bfp[:, bq, 256:512], bk(bq), None
                else:
                    kb_ = bpool.get()
                    ts(bfp[:, kb_, 0:256], bfp[:, bq, 256:512], blk[:, bi:bi + 1], None, ALU.mult, None,
                       [bk(bq), blk_key], [bk(kb_)])
                    kesrc, kkey = bfp[:, kb_, 0:256], bk(kb_)
                g = gpool.get()
                for h in range(4):
                    pr = h // 2
                    mm(psg[g][:, h * 128:(h + 1) * 128], kesrc[:, pr * 128:(pr + 1) * 128],
                       vb_bf[:, ti, h * 128:(h + 1) * 128], True, True, [kkey, "big1"], [gk(g)])
                if kb_ is not None:
                    bpool.put(kb_)
                for h in range(4):
                    hp, pr = (h % 2) * 64, h // 2
                    tt("dve", S32[hp:hp + 64, slot, pr, :], psg[g][hp:hp + 64, h * 128:(h + 1) * 128],
                       S32[hp:hp + 64, slot, pr, :], ALU.add, [gk(g), skey], [skey])
                    ts(S32[hp:hp + 64, slot, pr, :], S32[hp:hp + 64, slot, pr, :],
                       f32p[hp:hp + 64, fz, 256 + pr * 16 + bi:256 + pr * 16 + bi + 1], None, ALU.mult, None,
                       [skey, fk(fz)], [skey])
                gpool.put(g)
                s_store(bi, slot, skey)
            bpool.put(bq)
            bpool.put(bT)
            fpool.put(fz)
            yield
            fo = fpool.get()
            fq = fpool.get()
            cp("dve", f32p[:, fo, :], psg[go][:, :], [gk(go)], [fk(fo)])
            act(f32p[:, fq, :], psg[go][:, :], AF.Square, [gk(go)], [fk(fq)])
            gpool.put(go)
            yield
            gm = gpool.get()
            gs = gpool.get()
            mm(psg[gm][:, :], onesdiv[:], f32p[:, fo, :], True, True, ["onesdiv", fk(fo)], [gk(gm)])
            mm(psg[gs][:, :], onesdiv[:], f32p[:, fq, :], True, True, ["onesdiv", fk(fq)], [gk(gs)])
            act(f32p[:, fq, :], psg[gm][:, :], AF.Square, [gk(gm), fk(fq)], [fk(fq)])
            tt("dve", f32p[:, fq, :], psg[gs][:, :], f32p[:, fq, :], ALU.subtract, [gk(gs), fk(fq)], [fk(fq)])
            gpool.put(gs)
            ts(f32p[:, fq, :], f32p[:, fq, :], 0.0, None, ALU.max, None, [fk(fq)], [fk(fq)])
            act(f32p[:, fq, :], f32p[:, fq, :], AF.Sqrt, [fk(fq), "eps_c"], [fk(fq)], bias=eps_c[:])
            recip(f32p[:, fq, :], f32p[:, fq, :], [fk(fq)], [fk(fq)])
            tt("dve", f32p[:, fo, :], f32p[:, fo, :], psg[gm][:, :], ALU.subtract, [fk(fo), gk(gm)], [fk(fo)])
            gpool.put(gm)
            tt("dve", f32p[:, fo, :], f32p[:, fo, :], f32p[:, fq, :], ALU.mult, [fk(fo), fk(fq)], [fk(fo)])
            fpool.put(fq)
            stt(ob2T[:, :, t0:t0 + 128], f32p[:, fo, :].rearrange("p (h t) -> p h t", h=4), gnorm_c[:, 0:1],
                rbT[:, :, t0:t0 + 128], ALU.mult, ALU.mult, [fk(fo), "gnorm_c", "big1"], ["ob2T"])
            fpool.put(fo)
            yield

        def cross_tile(t0, qc_b, groups):
            goc = gpool.get()
            gl = gpool.get()
            for pair in range(2):
                g = gpool.get()
                for hh in range(2):
                    h = pair * 2 + hh
                    for blk_i in range(2):
                        for (slot, key, c0, cn) in groups:
                            col = (hh * 2 + blk_i) * 128 + c0
                            mm(psg[g][:, col:col + cn], kcT[:, slot, h, blk_i * 128:(blk_i + 1) * 128],
                               bfp[:, qc_b, h * 128 + c0:h * 128 + c0 + cn], True, True, [key + "k", bk(qc_b)],
                               [gk(g)])
                pb = bpool.get()
                act(bfp[:, pb, 0:512], psg[g][:, :], AF.Exp, [gk(g)], [bk(pb)])
                gpool.put(g)
                for hh in range(2):
                    h = pair * 2 + hh
                    for (slot, key, c0, cn) in groups:
                        for blk_i in range(2):
                            col = (hh * 2 + blk_i) * 128 + c0
                            mm(psg[goc][:, h * 128 + c0:h * 128 + c0 + cn],
                               vc[:, slot, blk_i, h * 128:(h + 1) * 128], bfp[:, pb, col:col + cn],
                               blk_i == 0, blk_i == 1, [key + "v", bk(pb)], [gk(goc)])
                        for blk_i in range(2):
                            col = (hh * 2 + blk_i) * 128 + c0
                            mm(psg[gl][:, h * 128 + c0:h * 128 + c0 + cn], ones_bf[:, :], bfp[:, pb, col:col + cn],
                               blk_i == 0, blk_i == 1, ["ones_bf", bk(pb)], [gk(gl)])
                bpool.put(pb)
            f = fpool.get()
            recip(f32p[:, f, :], psg[gl][:, :], [gk(gl)], [fk(f)])
            gpool.put(gl)
            tt("dve", ocT[:, :, t0:t0 + 128], psg[goc][:, :].rearrange("p (h t) -> p h t", h=4),
               f32p[:, f, :].rearrange("p (h t) -> p h t", h=4), ALU.mult, [gk(goc), fk(f)], ["ocT"])
            gpool.put(goc)
            fpool.put(f)

        def route_tile(x, tile_no):
            s = rms_stats(xpl[:, x, :], D, [xk(x)], "ffn")
            bx = bpool.get()
            stt(bfp[:, bx, :], xpl[:, x, :], smallp[:, s, 0:1], nffn_rep[:], ALU.mult, ALU.mult,
                [xk(x), sk(s), "nffn_rep"], [bk(bx)])
            spool.put(s)
            bT = bpool.get()
            transposes_to(bfp[:, bT, :].rearrange("p (k c) -> p k c", k=8), bfp[:, bx, :], 8, [bk(bx)], [bk(bT)])
            g = gpool.get()
            for k in range(8):
                mm(psg[g][:, 0:36], bfp[:, bT, k * 128:(k + 1) * 128], wr_bf[:, k, :], k == 0, k == 7,
                   [bk(bT), "wr_bf"], [gk(g)])
            bpool.put(bT)
            f = fpool.get()
            F_ = f32p[:, f, :]
            tt("dve", F_[:, 0:36], psg[g][:, 0:36], rbias_rep[:], ALU.add, [gk(g), "rbias_rep"], [fk(f)])
            gpool.put(g)
            s = spool.get()
            sm = smallp[:, s, :]
            red(sm[:, 0:1], F_[:, 0:4], ALU.max, [fk(f)], [sk(s)])
            ts(F_[:, 40:44], F_[:, 0:4], sm[:, 0:1], None, ALU.is_ge, None, [fk(f), sk(s)], [fk(f)])
            ts(sm[:, 1:2], sm[:, 0:1], -1.0, None, ALU.mult, None, [sk(s)], [sk(s)])
            mset("pool", sm[:, 2:3], 0.0, [sk(s)])
            act(F_[:, 44:48], F_[:, 0:4], AF.Exp, [fk(f), sk(s)], [fk(f), sk(s)], bias=sm[:, 1:2], accum_out=sm[:, 2:3])
            recip(sm[:, 3:4], sm[:, 2:3], [sk(s)], [sk(s)])
            ts(F_[:, 44:48], F_[:, 40:44], -NEG, NEG, ALU.mult, ALU.add, [fk(f)], [fk(f)])
            tt("dve", F_[:, 64:96].rearrange("p (g e) -> p g e", g=4),
               F_[:, 4:36].rearrange("p (g e) -> p g e", g=4),
               F_[:, 44:48].unsqueeze(2).to_broadcast([128, 4, 8]), ALU.add, [fk(f)], [fk(f)])
            S.op("dve", lambda e: e.max(out=F_[:, 96:104], in_=F_[:, 64:96]), [fk(f)], [fk(f)])
            ts(F_[:, 128:160], F_[:, 64:96], F_[:, 96:97], None, ALU.is_ge, None, [fk(f)], [fk(f)])
            ts(F_[:, 160:192], F_[:, 64:96], F_[:, 97:98], None, ALU.is_ge, None, [fk(f)], [fk(f)])
            tt("dve", F_[:, 160:192], F_[:, 160:192], F_[:, 128:160], ALU.subtract, [fk(f)], [fk(f)])
            ts(sm[:, 4:5], F_[:, 96:97], -1.0, None, ALU.mult, None, [fk(f)], [sk(s)])
            act(sm[:, 5:6], F_[:, 97:98], AF.Exp, [fk(f), sk(s)], [sk(s)], bias=sm[:, 4:5])
            ts(sm[:, 6:7], sm[:, 5:6], 1.0, None, ALU.add, None, [sk(s)], [sk(s)])
            recip(sm[:, 6:7], sm[:, 6:7], [sk(s)], [sk(s)])
            tt("dve", wts[:, tile_no, 0:1], sm[:, 6:7], sm[:, 3:4], ALU.mult, [sk(s)], ["wts"])
            tt("dve", wts[:, tile_no, 1:2], wts[:, tile_no, 0:1], sm[:, 5:6], ALU.mult, [sk(s), "wts"], ["wts"])
            ba = bpool.get()
            cp("dve", bfp[:, ba, 0:64], F_[:, 128:192], [fk(f)], [bk(ba)])
            g = gpool.get()
            mm(psg[g][:, 0:64], stri_bf[:], bfp[:, ba, 0:64], True, True, ["stri_bf", bk(ba)], [gk(g)])
            mm(psg[g][:, 64:128], ones_bf[:], bfp[:, ba, 0:64], True, True, ["ones_bf", bk(ba)], [gk(g)])
            bpool.put(ba)
            tt("dve", F_[:, 256:288], rbase[:], ecap[:], ALU.add, ["rbase", "ecap"], [fk(f)])
            tt("dve", F_[:, 288:320], F_[:, 256:288], psg[g][:, 64:96], ALU.add, [fk(f), gk(g)], [fk(f)])
            tt("dve", F_[:, 256:288], F_[:, 256:288], psg[g][:, 0:32], ALU.add, [fk(f), gk(g)], [fk(f)])
            tt("dve", F_[:, 288:320], F_[:, 288:320], psg[g][:, 32:64], ALU.add, [fk(f), gk(g)], [fk(f)])
            tt("dve", F_[:, 256:288], F_[:, 256:288], F_[:, 128:160], ALU.mult, [fk(f)], [fk(f)])
            tt("dve", F_[:, 288:320], F_[:, 288:320], F_[:, 160:192], ALU.mult, [fk(f)], [fk(f)])
            red(sm[:, 0:2], F_[:, 256:320].rearrange("p (a e) -> p a e", a=2), ALU.add, [fk(f)], [sk(s)])
            tt("dve", rbase[:], rbase[:], psg[g][:, 64:96], ALU.add, ["rbase", gk(g)], ["rbase"])
            tt("dve", rbase[:], rbase[:], psg[g][:, 96:128], ALU.add, ["rbase", gk(g)], ["rbase"])
            gpool.put(g)
            cp("dve", dsti[:, tile_no, :], sm[:, 0:2], [sk(s)], ["dsti"])
            spool.put(s)
            fpool.put(f)
            for j in range(2):
                S.op("q_pool", lambda e, j=j: e.indirect_dma_start(
                    out=xsc[:, :], out_offset=bass.IndirectOffsetOnAxis(ap=dsti[:, tile_no, j:j + 1], axis=0),
                    in_=bfp[:, bx, :], in_offset=None),
                    [bk(bx), "dsti"] + ["xsc_z%d" % r for r in range(NEXP * CAP // 512)], ["xsc"])
            bpool.put(bx)

        def merge_supertile(ntile, x_src_of, tok0, prefetch=None):
            T = ntile * 128
            if prefetch is not None:
                prefetch()
            srcs = [(oaT, "oaT", 64, 4), (ob2T, "ob2T", 128, 4), (ocT, "ocT", 128, 4)]
            order = [("g", half, b) for half in range(2) for b in range(3)] + [("o", hf, 0) for hf in range(2)]
            wq = {}

            def ensure(n):
                if n < len(order) and n not in wq:
                    kind, p0, p1 = order[n]
                    if kind == "g":
                        wq[n] = wload(w_in_blk(C_GL + p1 * 1024 + p0 * 512, 512), 8, 512)
                    else:
                        wq[n] = wload((wbf_out[:, p0 * 512:(p0 + 1) * 512].rearrange("(k p) c -> p k c", p=128),
                                       "wbfout%d" % p0), 8, 512)
            ensure(0)
            acc = None
            for n in range(6):
                ensure(n + 1)
                _, half, b = order[n]
                w = wq[n]
                if b == 0:
                    acc = [fpool.get() for _ in range(4)]
                src, skey, np_, nj = srcs[b]
                for cc in range(4):
                    c = half * 4 + cc
                    par = cc % 2
                    wkey = "wbr%d_%d" % (b, par)
                    dma("q_pool", wbr_sb[b][:, par, :, :],
                        wbf_br[b][:, c * 128:(c + 1) * 128].rearrange("(j p) c -> p j c", p=np_), ["wbfbr%d" % b],
                        [wkey])
                    g = gpool.get()
                    for k in range(8):
                        mm(psg[g][:, 0:T], wbuf[:, w, k, cc * 128:(cc + 1) * 128], xnT[:, k, 0:T],
                           k == 0, k == 7, [wk(w), "xnT"], [gk(g)])
                    sgb = bpool.get()
                    act(bfp[:, sgb, 0:T], psg[g][:, 0:T], AF.Sigmoid, [gk(g)], [bk(sgb)])
                    gpool.put(g)
                    g = gpool.get()
                    for j in range(nj):
                        mm(psg[g][:, 0:T], wbr_sb[b][0:np_, par, j, :], src[0:np_, j, 0:T], j == 0, j == nj - 1,
                           [wkey, skey], [gk(g)])
                    if b == 0:
                        tt("dve", f32p[:, acc[cc], 0:T], psg[g][:, 0:T], bfp[:, sgb, 0:T], ALU.mult,
                           [gk(g), bk(sgb)], [fk(acc[cc])])
                    else:
                        ft = fpool.get()
                        tt("dve", f32p[:, ft, 0:T], psg[g][:, 0:T], bfp[:, sgb, 0:T], ALU.mult, [gk(g), bk(sgb)],
                           [fk(ft)])
                        if b == 1:
                            tt("dve", f32p[:, acc[cc], 0:T], f32p[:, acc[cc], 0:T], f32p[:, ft, 0:T], ALU.add,
                               [fk(acc[cc]), fk(ft)], [fk(acc[cc])])
                        else:
                            tt("dve", mergedT[:, c, 0:T], f32p[:, acc[cc], 0:T], f32p[:, ft, 0:T], ALU.add,
                               [fk(acc[cc]), fk(ft)], ["big1"])
                        fpool.put(ft)
                    gpool.put(g)
                    bpool.put(sgb)
                wpool.put(w)
                if b == 2:
                    for a_ in acc:
                        fpool.put(a_)
            ensure(7)
            wo = [wq[6], wq[7]]
            prev = None
            for i in range(ntile):
                x = xpool.get()
                dma("q_sp", xpl[:, x, :], x_src_of(i), (), [xk(x)])
                for hf in range(2):
                    g = gpool.get()
                    for c in range(8):
                        mm(psg[g][:, :], mergedT[:, c, i * 128:(i + 1) * 128], wbuf[:, wo[hf], c, :], c == 0, c == 7,
                           ["big1", wk(wo[hf])], [gk(g)])
                    tt("dve", xpl[:, x, hf * 512:(hf + 1) * 512], xpl[:, x, hf * 512:(hf + 1) * 512], psg[g][:, :],
                       ALU.add, [xk(x), gk(g)], [xk(x)])
                    gpool.put(g)
                tn = state["tile_no"]
                state["tile_no"] += 1
                dma("q_sp", h2s[tn * 128:(tn + 1) * 128, :], xpl[:, x, :], [xk(x)], ["h2s"])
                if prev is not None:
                    route_tile(*prev)
                    xpool.put(prev[0])
                prev = (x, tn)
            route_tile(*prev)
            xpool.put(prev[0])
            for w in wo:
                wpool.put(w)

        def run_blocks(blocks, depth=2):
            pend = []
            wq = {}

            def ensure_w(n):
                if n < len(blocks) and n not in wq:
                    wq[n] = wload(*blocks[n]["wsrc"])
            ensure_w(0)
            ensure_w(1)
            for n, blk in enumerate(blocks):
                ensure_w(n + 2)
                w = wq[n]
                for (fa, fb) in blk["items"]:
                    ctx = fa(w)
                    pend.append((fb, ctx))
                    if len(pend) > depth:
                        fb_, ctx_ = pend.pop(0)
                        fb_(ctx_)
                wpool.put(w)
            while pend:
                fb_, ctx_ = pend.pop(0)
                fb_(ctx_)

        def project_supertile(ntile, seq_tile0, is_prompt, out_kv, cross_fn):
            T = ntile * 128
            blocks = []

            def A_tok(i, cols):
                def fa(w):
                    g = gpool.get()
                    proj_tok(xnT, "xnT", i * 128, w, cols, psg[g][:, 0:cols], gk(g))
                    return g
                return fa

            for gi in range(3):
                items = []
                for i in range(ntile):
                    def fb(g, gi=gi, i=i):
                        bq = bpool.get()
                        headnorm(psg[g][:, 0:256], gk(g), 4, 64, qna_rep, "qna_rep",
                                 [(bfp[:, bq, 0:256].rearrange("p (h d) -> p h d", h=4), bk(bq))])
                        gpool.put(g)
                        transposes_to(qAT[:, gi * 2:gi * 2 + 2, i * 128:(i + 1) * 128], bfp[:, bq, 0:256], 2,
                                      [bk(bq)], ["qAT"])
                        bpool.put(bq)
                    items.append((A_tok(i, 256), fb))
                blocks.append(dict(wsrc=(w_in_blk(C_QA + gi * 256, 256), 8, 256), items=items))
            for gi in range(3):
                items = []
                for i in range(ntile):
                    def fb(g, gi=gi, i=i):
                        bq = bpool.get()
                        f = fpool.get()
                        headnorm(psg[g][:, 0:256], gk(g), 4, 64, kna_rep, "kna_rep",
                                 [(f32p[:, f, 0:256].rearrange("p (h d) -> p h d", h=4), fk(f))])
                        gpool.put(g)
                        cp("act", bfp[:, bq, 0:256], f32p[:, f, 0:256], [fk(f)], [bk(bq)])
                        out_kv(0, gi, i, f32p[:, f, 0:256], fk(f))
                        fpool.put(f)
                        kt = seq_tile0 + i
                        transposes_to(kAT[:, gi * 2:gi * 2 + 2, kt * 128:(kt + 1) * 128], bfp[:, bq, 0:256], 2,
                                      [bk(bq)], ["kAT"])
                        bpool.put(bq)
                    items.append((A_tok(i, 256), fb))
                blocks.append(dict(wsrc=(w_in_blk(C_KA + gi * 256, 256), 8, 256), items=items))
            for gi in range(3):
                items = []
                for i in range(ntile):
                    def fb(g, gi=gi, i=i):
                        f = fpool.get()
                        cp("dve", f32p[:, f, 0:256], psg[g][:, 0:256], [gk(g)], [fk(f)])
                        cp("act", vA[:, seq_tile0 + i, gi * 256:(gi + 1) * 256], psg[g][:, 0:256], [gk(g)], ["vA"])
                        gpool.put(g)
                        out_kv(1, gi, i, f32p[:, f, 0:256], fk(f))
                        fpool.put(f)
                    items.append((A_tok(i, 256), fb))
                blocks.append(dict(wsrc=(w_in_blk(C_VA + gi * 256, 256), 8, 256), items=items))
            items = []
            for i in range(ntile):
                def fb(g, i=i):
                    cp("act", vb_bf[:, i, :], psg[g][:, :], [gk(g)], ["big1"])
                    gpool.put(g)
                items.append((A_tok(i, 512), fb))
            blocks.append(dict(wsrc=(w_in_blk(C_VB, 512), 8, 512), items=items))
            items = []
            for j in range(4):
                def fa(w, j=j):
                    g = gpool.get()
                    for k in range(8):
                        mm(psg[g][:, 0:T], wbuf[:, w, k, j * 128:(j + 1) * 128], xnT[:, k, 0:T], k == 0, k == 7,
                           [wk(w), "xnT"], [gk(g)])
                    return g

                def fb(g, j=j):
                    act(rbT[:, j, 0:T], psg[g][:, 0:T], AF.Silu, [gk(g)], ["big1"])
                    gpool.put(g)
                items.append((fa, fb))
            blocks.append(dict(wsrc=(w_in_blk(C_RB, 512), 8, 512), items=items))
            run_blocks(blocks)
            w = wload(w_in_blk(C_QC, 512), 8, 512)
            for i in range(ntile):
                g = gpool.get()
                proj_tok(xnT, "xnT", i * 128, w, 512, psg[g][:, :], gk(g))
                bq = bpool.get()
                headnorm(psg[g][:, :], gk(g), 4, 128, qnc_rep, "qnc_rep",
                         [(bfp[:, bq, 0:512].rearrange("p (h d) -> p h d", h=4), bk(bq))])
                gpool.put(g)
                bT = bpool.get()
                transposes_to(bfp[:, bT, 0:512].rearrange("p (k c) -> p k c", k=4), bfp[:, bq, 0:512], 4, [bk(bq)],
                              [bk(bT)])
                bpool.put(bq)
                cross_fn(i, bT)
                bpool.put(bT)
            wpool.put(w)
            state["w_qkb"] = wload(w_in_blk(C_QB, 512), 8, 512)

        def mem_kv_prompt(sq):
            mT = [bpool.get(), bpool.get()]
            x = xpool.get()
            dma("q_sp", xpl[:, x, :], mem_norm.partition_broadcast(128), (), [xk(x)])
            for t in range(2):
                front(memp[sq, t * 128:(t + 1) * 128, :], xpl[:, x, :], xk(x),
                      bfp[:, mT[t], :].rearrange("p (k c) -> p k c", k=8), bk(mT[t]))
            xpool.put(x)
            for part in range(2):
                w = wload((w_mem_kv[:, part * 512:(part + 1) * 512].rearrange("(k p) c -> p k c", p=128),
                           "nokey"), 8, 512)
                for t in range(2):
                    g = gpool.get()
                    for k in range(8):
                        mm(psg[g][:, :], bfp[:, mT[t], k * 128:(k + 1) * 128], wbuf[:, w, k, :], k == 0, k == 7,
                           [bk(mT[t]), wk(w)], [gk(g)])
                    f = fpool.get()
                    if part == 0:
                        headnorm(psg[g][:, :], gk(g), 4, 128, knc_rep, "knc_rep",
                                 [(f32p[:, f, :].rearrange("p (h d) -> p h d", h=4), fk(f))])
                        gpool.put(g)
                        bq = bpool.get()
                        cp("act", bfp[:, bq, 0:512], f32p[:, f, :], [fk(f)], [bk(bq)])
                        transposes_to(kcT[:, 0, :, t * 128:(t + 1) * 128], bfp[:, bq, 0:512], 4, [bk(bq)], ["kv0k"])
                        bpool.put(bq)
                    else:
                        cp("dve", f32p[:, f, :], psg[g][:, :], [gk(g)], [fk(f)])
                        cp("act", vc[:, 0, t, :], psg[g][:, :], [gk(g)], ["kv0v"])
                        gpool.put(g)
                    dma("q_sp", memkv[sq, t * 128:(t + 1) * 128, part, :, :],
                        f32p[:, f, :].rearrange("p (h d) -> p h d", h=4), [fk(f)], ["memkv"])
                    fpool.put(f)
                wpool.put(w)
            for m in mT:
                bpool.put(m)

        pending_copies = [(gi, b) for b in range(NSB) for gi in range(3)]

        def issue_copies(n):
            for _ in range(n):
                if not pending_copies:
                    return
                gi, b = pending_copies.pop(0)
                Wb = WIN[gi]
                dma("q_act", ws[gi][b, 0:Wb - LS], cw[gi][b, LS:Wb], (), ["wsc%d_%d" % (gi, b)])

        def prompt_seq(sq):
            mem_kv_prompt(sq)
            mset("pool", S32[:, 0, :, :], 0.0, ["S0"])
            mset("pool", Sbf[:, 0, :, :], 0.0, ["S0bf"])
            for st in range(DBG["nst"]):
                tile0 = st * 4
                pre = state.pop("pre", None)
                if pre is None:
                    pre = [front_a(xp[sq, (tile0 + i) * 128:(tile0 + i + 1) * 128, :], nmix_rep[:], "nmix_rep")
                           for i in range(4)]
                for r in state.pop("zero_rest", []):
                    dma("q_sp", xsc[r * 512:(r + 1) * 512, :], xsc[0:512, :], ["xsc_z0"], ["xsc_z%d" % r])
                for i in range(4):
                    front_b(pre[i], xnT[:, :, i * 128:(i + 1) * 128], "xnT")

                def prefetch(sq=sq, st=st):
                    if st + 1 < DBG["nst"]:
                        nsq, nt0 = sq, (st + 1) * 4
                    elif sq + 1 < DBG["nseq"]:
                        nsq, nt0 = sq + 1, 0
                    else:
                        if DBG["sample"]:
                            state["pre"] = [front_a(xsm[:, :], nmix_rep[:], "nmix_rep")]
                        return
                    state["pre"] = [front_a(xp[nsq, (nt0 + i) * 128:(nt0 + i + 1) * 128, :], nmix_rep[:],
                                            "nmix_rep") for i in range(4)]

                def out_kv(kind, gi, i, f32view, key, tile0=tile0):
                    pos0 = (tile0 + i) * 128
                    lo = SEQ - WIN[gi]
                    if pos0 >= lo:
                        dma("q_sp", wp[gi][sq, pos0 - lo:pos0 - lo + 128, kind, :, :],
                            f32view.rearrange("p (h d) -> p h d", h=4), [key], ["wp%d" % gi])

                project_supertile(4, tile0, True, out_kv,
                                  (lambda i, bT: cross_tile(i * 128, bT, [(0, "kv0", 0, 128)])) if "cross" in DBG["mix"]
                                  else (lambda i, bT: None))
                issue_copies(6)

                def s_loader(bi, phase):
                    return 0, "S0"

                def s_store(bi, slot, skey):
                    cp("act", Sbf[:, 0, :, :], S32[:, 0, :, :], ["S0"], ["S0bf"])

                threads = []
                if "attn" in DBG["mix"]:
                    threads.append(chain([(lambda i=i: attn_prompt_tile(tile0 + i, i * 128)) for i in range(4)]))
                if "gla" in DBG["mix"]:
                    threads.append(chain([(lambda i=i: gla_tile(i, i * 128, tri_p[:], "tri_p", blk_p, "blk_p", 1,
                                                                s_loader, s_store)) for i in range(4)]))
                run_threads(threads)
                wpool.put(state["w_qkb"])
                if DBG["merge"]:
                    merge_supertile(4, lambda i, tile0=tile0: xp[sq, (tile0 + i) * 128:(tile0 + i + 1) * 128, :], 0,
                                    prefetch)
            dma("q_sp", glap[sq].rearrange("(pr hh) dk dv -> (hh dk) pr dv", hh=2), S32[:, 0, :, :], ["S0"], ["glap"])

        def sample_tile():
            issue_copies(1000)
            pre = state.pop("pre", None)
            if pre is None:
                pre = [front_a(xsm[:, :], nmix_rep[:], "nmix_rep")]
            front_b(pre[0], xnT[:, :, 0:128], "xnT")

            def out_kv(kind, gi, i, f32view, key):
                Wb = WIN[gi]
                for b in range(NSB):
                    dma("q_sp", ws[gi][b, Wb - LS:Wb, kind, :, :],
                        f32view[b * LS:(b + 1) * LS, :].rearrange("p (h d) -> p h d", h=4), [key],
                        ["wsn%d_%d_%d" % (gi, b, kind)])

            project_supertile(1, 0, False, out_kv, lambda i, bT: cross_sample(bT))
            mset("dve", psN[0:64, :], 0.0, ["psN"])
            mset("dve", psL[0:64, :], 0.0, ["psL"])
            blocks = []
            for h in range(4):
                hp, pr = (h % 2) * 64, h // 2
                for gi in range(3):
                    ch = gi * 2 + pr
                    blocks.append(dict(
                        mask=maskS[:, gi, :], mkey="maskS", kkey="kAT", qkey="qAT", vkey="vA", ncol=128,
                        sub=[(kAT[hp:hp + 64, ch, 0:128], qAT[hp:hp + 64, ch, 0:128], 0, 128)],
                        pv=[(vA[:, 0, (gi * 4 + h) * 64:(gi * 4 + h + 1) * 64], 0, 128,
                             psN[0:64, h * 128:(h + 1) * 128], psL[0:64, h * 128:(h + 1) * 128])]))
            attn_blocks(blocks, None)
            ld = 0
            for b in range(NSB):
                for gi in range(3):
                    nkt_all = WIN[gi] // 128
                    for k0 in range(0, nkt_all, 4):
                        nkt = min(4, nkt_all - k0)
                        slot = ld % 2
                        ld += 1
                        ckey = "cache%d" % slot
                        dma("q_pool", kcache[:, slot, 0:nkt, :],
                            cw[gi][b, k0 * 128:(k0 + nkt) * 128, 0].rearrange("(kt p) h d -> p kt (h d)", p=128),
                            (), [ckey + "k"])
                        dma("q_pool", vcache[:, slot, 0:nkt, :],
                            cw[gi][b, k0 * 128:(k0 + nkt) * 128, 1].rearrange("(kt p) h d -> p kt (h d)", p=128),
                            (), [ckey + "v"])
                        t = trpool.get()
                        for kt in range(nkt):
                            for pr in range(2):
                                tr(pstr[t][:, (kt * 2 + pr) * 128:(kt * 2 + pr + 1) * 128],
                                   kcache[:, slot, kt, pr * 128:(pr + 1) * 128], [ckey + "k"], [tk(t)])
                        cp("dve", kcTA[:, slot, 0:nkt, :, :],
                           pstr[t][:, 0:nkt * 256].rearrange("p (kt pr c) -> p kt pr c", kt=nkt, pr=2),
                           [tk(t)], [ckey + "kT"])
                        trpool.put(t)
                        blocks = []
                        for kt in range(nkt):
                            typ = 0 if (k0 + kt) == 0 else 1
                            sub, pv = [], []
                            for ci, h in enumerate((0, 2, 1, 3)):
                                hp, pr = (h % 2) * 64, h // 2
                                sub.append((kcTA[hp:hp + 64, slot, kt, pr, :],
                                            qAT[hp:hp + 64, gi * 2 + pr, b * LS:(b + 1) * LS], ci * 8, 8))
                                pv.append((vcache[:, slot, kt, h * 64:(h + 1) * 64], ci * 8, 8,
                                           psN[0:64, h * 128 + b * LS:h * 128 + (b + 1) * LS],
                                           psL[0:64, h * 128 + b * LS:h * 128 + (b + 1) * LS]))
                            blocks.append(dict(mask=maskC[:, 2 * gi + typ, :, :].rearrange("p h l -> p (h l)"),
                                               mkey="maskC", kkey=ckey + "kT", qkey="qAT", vkey=ckey + "v", ncol=32,
                                               sub=sub, pv=pv))
                        attn_blocks(blocks, None)
            attn_finish(0)
            def s_loader(bi, phase):
                slot = bi % 2
                key = "S%d" % slot
                src = sgla[bi].rearrange("(pr hh) dk dv -> (hh dk) pr dv", hh=2)
                if phase == "o":
                    dma("q_pool", Sbf[:, slot, :, :], src, (), [key + "bf"])
                else:
                    dma("q_sp", S32[:, slot, :, :], src, (), [key])
                return slot, key

            def s_store(bi, slot, skey):
                dma("q_sp", glas[bi].rearrange("(pr hh) dk dv -> (hh dk) pr dv", hh=2), S32[:, slot, :, :], [skey],
                    ["glas"])

            for _ in gla_tile(0, 0, tri_s[:], "tri_s", blk_s, "blk_s", NSB, s_loader, s_store):
                pass
            wpool.put(state["w_qkb"])
            merge_supertile(1, lambda i: xsm[:, :], 0)

        def cross_sample(qc_b):
            goc = gpool.get()
            gl = gpool.get()
            for b in range(NSB):
                slot = b % 2
                key = "kvs%d" % slot
                bk_ = bpool.get()
                dma("q_pool", bfp[:, bk_, :].rearrange("p (blk c) -> p blk c", blk=2),
                    cmem[b, :, 0].rearrange("(blk p) h d -> p blk (h d)", p=128), (), [bk(bk_)])
                dma("q_pool", vc[:, slot, :, :], cmem[b, :, 1].rearrange("(blk p) h d -> p blk (h d)", p=128), (),
                    [key + "v"])
                t = trpool.get()
                for blk_i in range(2):
                    for h in range(4):
                        tr(pstr[t][:, (h * 2 + blk_i) * 128:(h * 2 + blk_i + 1) * 128],
                           bfp[:, bk_, blk_i * 512 + h * 128:blk_i * 512 + (h + 1) * 128], [bk(bk_)], [tk(t)])
                cp("dve", kcT[:, slot, :, :], pstr[t][:, :].rearrange("p (h n) -> p h n", h=4), [tk(t)], [key + "k"])
                trpool.put(t)
                bpool.put(bk_)
                c0, cn = b * LS, LS
                g = gpool.get()
                for h in range(4):
                    for blk_i in range(2):
                        col = (h * 2 + blk_i) * 8
                        mm(psg[g][:, col:col + 8], kcT[:, slot, h, blk_i * 128:(blk_i + 1) * 128],
                           bfp[:, qc_b, h * 128 + c0:h * 128 + c0 + cn], True, True, [key + "k", bk(qc_b)], [gk(g)])
                pb = bpool.get()
                act(bfp[:, pb, 0:64], psg[g][:, 0:64], AF.Exp, [gk(g)], [bk(pb)])
                gpool.put(g)
                for h in range(4):
                    for blk_i in range(2):
                        col = (h * 2 + blk_i) * 8
                        mm(psg[goc][:, h * 128 + c0:h * 128 + c0 + cn], vc[:, slot, blk_i, h * 128:(h + 1) * 128],
                           bfp[:, pb, col:col + 8], blk_i == 0, blk_i == 1,
                           [key + "v", bk(pb)], [gk(goc)])
                        mm(psg[gl][:, h * 128 + c0:h * 128 + c0 + cn], ones_bf[:, :], bfp[:, pb, col:col + 8],
                           blk_i == 0, blk_i == 1, ["ones_bf", bk(pb)], [gk(gl)])
                bpool.put(pb)
            f = fpool.get()
            recip(f32p[:, f, :], psg[gl][:, :], [gk(gl)], [fk(f)])
            gpool.put(gl)
            tt("dve", ocT[:, :, 0:128], psg[goc][:, :].rearrange("p (h t) -> p h t", h=4),
               f32p[:, f, :].rearrange("p (h t) -> p h t", h=4), ALU.mult, [gk(goc), fk(f)], ["ocT"])
            gpool.put(goc)
            fpool.put(f)

        def experts_phase():
            XeT = kAT[:, 0:4, :].rearrange("p a (k c) -> p (a k) c", c=512)
            Wv = vA[:, :, :].rearrange("p a c -> p (a c)")

            def views(e_):
                sl = e_ % 2
                base = sl * 6144
                return (sl, Wv[:, base:base + 2048].rearrange("p (k f) -> p k f", k=8),
                        Wv[:, base + 2048:base + 4096].rearrange("p (k f) -> p k f", k=8),
                        Wv[:, base + 4096:base + 6144].rearrange("p (k c) -> p k c", k=2))

            def loads_w(e_):
                sl, wg_v, wu_v, wd_v = views(e_)
                wkey = "ew%d" % sl
                dma("q_pool", wg_v, w_eg[e_].rearrange("(k p) f -> p k f", p=128), (), [wkey + "g"])
                dma("q_pool", wu_v, w_eu[e_].rearrange("(k p) f -> p k f", p=128), (), [wkey + "u"])
                dma("q_pool", wd_v, w_ed[e_].rearrange("(k p) c -> p k c", p=128), (), [wkey + "d"])

            def loads_x(e_):
                xs_slots = []
                for sb_ in range(4):
                    bx = bpool.get()
                    dma("q_sp", bfp[:, bx, :], xsc[e_ * CAP + sb_ * 128:e_ * CAP + (sb_ + 1) * 128, :], ["xsc"],
                        [bk(bx)])
                    xs_slots.append(bx)
                return xs_slots

            def tstage(e_, xs_slots):
                sl, wg_v, wu_v, wd_v = views(e_)
                xkey = "xe%d" % sl
                for sb_, bx in enumerate(xs_slots):
                    t = trpool.get()
                    for k in range(8):
                        tr(pstr[t][:, k * 128:(k + 1) * 128], bfp[:, bx, k * 128:(k + 1) * 128], [bk(bx)], [tk(t)])
                    cp("dve" if sb_ % 2 == 0 else "act", XeT[:, sl * 8:(sl + 1) * 8, sb_ * 128:(sb_ + 1) * 128],
                       pstr[t][:, :].rearrange("p (k c) -> p k c", k=8), [tk(t)], [xkey])
                    trpool.put(t)
                    bpool.put(bx)

            def compute(e_):
                sl, wg_v, wu_v, wd_v = views(e_)
                wkey = "ew%d" % sl
                xkey = "xe%d" % sl
                hb = bpool.get()
                for fc in range(2):
                    gg = gpool.get()
                    gu = gpool.get()
                    for k in range(8):
                        mm(psg[gg][:, :], wg_v[:, k, fc * 128:(fc + 1) * 128], XeT[:, sl * 8 + k, :], k == 0, k == 7,
                           [wkey + "g", xkey], [gk(gg)])
                    for k in range(8):
                        mm(psg[gu][:, :], wu_v[:, k, fc * 128:(fc + 1) * 128], XeT[:, sl * 8 + k, :], k == 0, k == 7,
                           [wkey + "u", xkey], [gk(gu)])
                    f = fpool.get()
                    act(f32p[:, f, :], psg[gg][:, :], AF.Silu, [gk(gg)], [fk(f)])
                    gpool.put(gg)
                    tt("dve", bfp[:, hb, fc * 512:(fc + 1) * 512], psg[gu][:, :], f32p[:, f, :], ALU.mult,
                       [gk(gu), fk(f)], [bk(hb)])
                    gpool.put(gu)
                    fpool.put(f)
                stores = []
                for sb_ in range(4):
                    x = xpool.get()
                    for hf in range(2):
                        g = gpool.get()
                        for fc in range(2):
                            mm(psg[g][:, :], bfp[:, hb, fc * 512 + sb_ * 128:fc * 512 + (sb_ + 1) * 128],
                               wd_v[:, fc, hf * 512:(hf + 1) * 512], fc == 0, fc == 1, [bk(hb), wkey + "d"], [gk(g)])
                        if hf == 0:
                            cp("act", xpl[:, x, 0:512], psg[g][:, :], [gk(g)], [xk(x)])
                        else:
                            cp("dve", xpl[:, x, 512:1024], psg[g][:, :], [gk(g)], [xk(x)])
                        gpool.put(g)
                    dma("q_sp", ysc[e_ * CAP + sb_ * 128:e_ * CAP + (sb_ + 1) * 128, :], xpl[:, x, :], [xk(x)],
                        ["ysc"])
                    xpool.put(x)
                bpool.put(hb)

            loads_w(0)
            tstage(0, loads_x(0))
            nxt = loads_x(1)
            for e_ in range(NEXP):
                if e_ + 1 < NEXP:
                    loads_w(e_ + 1)
                    tstage(e_ + 1, nxt)
                if e_ + 2 < NEXP:
                    nxt = loads_x(e_ + 2)
                compute(e_)

        def combine_phase():
            npair = NF // 2
            ntl = state["tile_no"]

            def loads(tn):
                x = xpool.get()
                dma("q_sp", xpl[:, x, :], h2s[tn * 128:(tn + 1) * 128, :], ["h2s"], [xk(x)])
                ys = []
                for j in range(2):
                    pi = (tn * 2 + j) % npair
                    yv = f32p[:, 2 * pi:2 * pi + 2, :].rearrange("p a c -> p (a c)")
                    keys = [fk(2 * pi), fk(2 * pi + 1)]
                    S.op("q_pool", lambda e, tn=tn, j=j, yv=yv: e.indirect_dma_start(
                        out=yv, out_offset=None, in_=ysc[:, :],
                        in_offset=bass.IndirectOffsetOnAxis(ap=dsti[:, tn, j:j + 1], axis=0)),
                        ["ysc", "dsti"], keys)
                    ys.append((yv, keys))
                return x, ys

            def finish(tn, x, ys):
                for j, (yv, keys) in enumerate(ys):
                    stt(xpl[:, x, :], yv, wts[:, tn, j:j + 1], xpl[:, x, :], ALU.mult, ALU.add,
                        keys + ["wts", xk(x)], [xk(x)])
                if tn < NPS * 16:
                    sq, tt_ = tn // 16, tn % 16
                    dst = yp[sq, tt_ * 128:(tt_ + 1) * 128, :]
                else:
                    dst = ysm[:, :]
                dma("q_sp", dst, xpl[:, x, :], [xk(x)], ["yout%d" % tn])
                xpool.put(x)

            if ntl == 0:
                return
            nxt = loads(0)
            for tn in range(ntl):
                cur = nxt
                if tn + 1 < ntl:
                    nxt = loads(tn + 1)
                finish(tn, *cur)

        mset("pool", big1[:], 0.0, ["big1"])
        dma("q_act", xsc[0:512, :].rearrange("(p j) d -> p (j d)", j=4), big1[:].rearrange("p a c -> p (a c)"),
            ["big1"], ["xsc_z0"])
        nz = NEXP * CAP // 512
        for r in range(1, 8):
            dma("q_act", xsc[r * 512:(r + 1) * 512, :], xsc[0:512, :], ["xsc_z0"], ["xsc_z%d" % r])
        state["zero_rest"] = list(range(8, nz))
        for sq in range(DBG["nseq"]):
            prompt_seq(sq)
        if DBG["sample"]:
            S.barrier(lambda e: e.memset(eps_c[:], EPS))
            sample_tile()
        if DBG["moe"]:
            S.barrier(lambda e: e.memset(eps_c[:], EPS))
            experts_phase()
            S.barrier(lambda e: e.memset(eps_c[:], EPS))
            combine_phase()
        if DBG.get("dump"):
            DBG["_in_dump"] = True
            DBG["dump"](locals())
        S.emit(nc)
    return nc


_PROG = {}


def _get_prog():
    if "nc" not in _PROG:
        _PROG["nc"] = build_program()
    return _PROG["nc"]


def kernel(x_prompt, x_sample, mem_prompt, cache_win1, cache_win2, cache_win3, state_gla, cache_mem,
           norm_mix, w_in, qn_a, kn_a, qn_c, kn_c, gla_gate_up, gla_gate_bias, gla_norm, mem_norm,
           w_mem_kv, w_branch_a, w_branch_b, w_branch_c, w_out, norm_ffn, w_router_group, b_router_group,
           w_router_expert, b_router_expert, w_exp_gate, w_exp_up, w_exp_down):
    nc = _get_prog()
    f = lambda a: np.ascontiguousarray(np.asarray(a, dtype=np.float32))
    shared = {
        "norm_mix": f(norm_mix), "w_in": f(w_in), "qn_a": f(qn_a), "kn_a": f(kn_a), "qn_c": f(qn_c), "kn_c": f(kn_c),
        "gla_gate_up": f(gla_gate_up), "gla_gate_bias": f(gla_gate_bias), "gla_norm": f(gla_norm),
        "mem_norm": f(mem_norm), "w_mem_kv": f(w_mem_kv), "w_branch_a": f(w_branch_a), "w_branch_b": f(w_branch_b),
        "w_branch_c": f(w_branch_c), "w_out": f(w_out), "norm_ffn": f(norm_ffn), "w_router_group": f(w_router_group),
        "b_router_group": f(b_router_group), "w_router_expert": f(w_router_expert),
        "b_router_expert": f(b_router_expert), "w_exp_gate": f(w_exp_gate), "w_exp_up": f(w_exp_up),
        "w_exp_down": f(w_exp_down),
    }
    x_prompt, x_sample, mem_prompt = f(x_prompt), f(x_sample), f(mem_prompt)
    cache_win1, cache_win2, cache_win3 = f(cache_win1), f(cache_win2), f(cache_win3)
    state_gla, cache_mem = f(state_gla), f(cache_mem)
    in_maps = []
    for c in range(NCORES):
        m = dict(shared)
        m["xp"] = x_prompt[c * NPS:(c + 1) * NPS]
        m["xs"] = x_sample[c * NSB:(c + 1) * NSB].reshape(128, D)
        m["memp"] = mem_prompt[c * NPS:(c + 1) * NPS]
        m["cw1"] = cache_win1[c * NSB:(c + 1) * NSB]
        m["cw2"] = cache_win2[c * NSB:(c + 1) * NSB]
        m["cw3"] = cache_win3[c * NSB:(c + 1) * NSB]
        m["sgla"] = state_gla[c * NSB:(c + 1) * NSB]
        m["cmem"] = cache_mem[c * NSB:(c + 1) * NSB]
        in_maps.append(m)
    ncr = DBG["ncores"]
    res = run_bass_kernel_spmd(nc, in_maps[:ncr], core_ids=list(range(ncr)))
    R = res.results
    cat = lambda k: np.concatenate([np.asarray(r[k]) for r in R], axis=0)
    y_prompt = cat("yp")
    y_sample = cat("ys").reshape(NCORES * NSB, LS, D)
    return (y_prompt, y_sample, cat("w1p"), cat("w2p"), cat("w3p"), cat("glap"), cat("memkv"),
            cat("w1s"), cat("w2s"), cat("w3s"), cat("glas"))
```

```python
import contextlib
import numpy as np
import concourse.bass as bass
import concourse.mybir as mybir
from concourse.bass_utils import run_bass_kernel_spmd

F32 = mybir.dt.float32
BF16 = mybir.dt.bfloat16
I32 = mybir.dt.int32
AF = mybir.ActivationFunctionType
ALU = mybir.AluOpType
AX = mybir.AxisListType

NCORES = 8
D = 1024
SEQ = 2048
NPS = 2
NSB = 16
LS = 8
NTOK = NPS * SEQ + NSB * LS
NTILE = NTOK // 128
CAP = 512
NEXP = 32
EPS = 1e-6
NEG = -30000.0
WIN = (128, 512, 2048)
DIL = (1, 4, 16)
C_QA, C_KA, C_VA, C_QB, C_KB, C_VB, C_RB, C_AB, C_QC, C_GL = 0, 768, 1536, 2304, 2560, 2816, 3328, 3840, 3856, 4368

COMPUTE = ("pe", "act", "dve", "pool")
QUEUES = ("q_sp", "q_act", "q_pool")
Q_HOST = {"q_sp": "sp", "q_act": "act", "q_pool": "pool"}
RING = {"q_sp": 16, "q_act": 8, "q_pool": 12}


class Op:
    __slots__ = ("eng", "fn", "deps", "idx", "signal", "is_dma", "dma_no", "stream", "where")


class Sched:
    def __init__(self):
        self.ops = []
        self.last_w = {}
        self.readers = {}
        self.streams = {"pe": [], "act": [], "dve": [], "pool": [], "sp": []}
        self.dma_count = {q: 0 for q in QUEUES}
        self.barrier_dep = None

    def op(self, eng, fn, reads=(), writes=()):
        mx = DBG.get("maxops")
        if mx is not None and len(self.ops) >= mx and not DBG.get("_in_dump"):
            return None
        is_dma = eng in QUEUES
        psr = [k for k in reads if k.startswith("ps")]
        if psr:
            reads = [k for k in reads if not k.startswith("ps")]
            writes = list(writes) + [k for k in psr if k not in writes]
        deps = set()
        if self.barrier_dep is not None:
            deps.add(self.barrier_dep)
        for k in reads:
            w = self.last_w.get(k)
            if w is not None:
                deps.add(w)
        for k in writes:
            w = self.last_w.get(k)
            if w is not None:
                deps.add(w)
            for r in self.readers.get(k, ()):
                deps.add(r)
        o = Op()
        o.eng, o.fn, o.deps, o.is_dma, o.signal, o.dma_no = eng, fn, deps, is_dma, False, None
        o.idx = len(self.ops)
        o.where = None
        if DBG.get("trace"):
            import sys as _sys
            fr = _sys._getframe(1)
            w = []
            while fr is not None and len(w) < 4:
                w.append("%s:%d" % (fr.f_code.co_name, fr.f_lineno))
                fr = fr.f_back
            o.where = " < ".join(w)
        self.ops.append(o)
        o.stream = Q_HOST[eng] if is_dma else eng
        if is_dma:
            o.dma_no = self.dma_count[eng]
            self.dma_count[eng] += 1
        self.streams[o.stream].append(o)
        for k in writes:
            self.last_w[k] = o.idx
            self.readers[k] = []
        for k in reads:
            if k not in writes:
                self.readers.setdefault(k, []).append(o.idx)
        return o

    def barrier(self, nop_fn):
        deps = set()
        for st in self.streams.values():
            if st:
                deps.add(st[-1].idx)
        for q in QUEUES:
            n = self.dma_count[q]
            cnt = 0
            for o in reversed(self.ops):
                if o.is_dma and o.eng == q:
                    deps.add(o.idx)
                    cnt += 1
                    if cnt >= RING[q]:
                        break
        o = self.op("dve", nop_fn)
        if o is None:
            return
        o.deps |= deps
        o.deps.discard(o.idx)
        self.barrier_dep = o.idx
        self.last_w = {}
        self.readers = {}

    def emit(self, nc):
        ops = self.ops
        for o in ops:
            for d in o.deps:
                p = ops[d]
                if not p.is_dma:
                    p.signal = True
        sig_count = {}
        cnt = {e: 0 for e in COMPUTE}
        for o in ops:
            if not o.is_dma:
                if o.signal:
                    cnt[o.eng] += 1
                sig_count[o.idx] = cnt[o.eng]
        with contextlib.ExitStack() as es:
            sems = {e: es.enter_context(nc.semaphore("s_" + e)) for e in COMPUTE}
            rings = {q: [es.enter_context(nc.semaphore("r_%s_%d" % (q, i))) for i in range(RING[q])]
                     for q in QUEUES}
            block = es.enter_context(nc.Block())
            handles = {"pe": "tensor", "act": "scalar", "dve": "vector", "pool": "gpsimd", "sp": "sync"}

            def run_stream(stream, e):
                waited = {}
                for o in self.streams[stream]:
                    need = {}
                    for d in o.deps:
                        p = ops[d]
                        if p.is_dma:
                            R = RING[p.eng]
                            key = (p.eng, p.dma_no % R)
                            val = 16 * (p.dma_no // R + 1)
                        else:
                            if p.eng == "pe" and stream == "pe":
                                continue
                            key = (p.eng, -1)
                            val = sig_count[p.idx]
                        if need.get(key, 0) < val:
                            need[key] = val
                    if o.is_dma:
                        R = RING[o.eng]
                        if o.dma_no >= R:
                            key = (o.eng, o.dma_no % R)
                            val = 16 * (o.dma_no // R)
                            if need.get(key, 0) < val:
                                need[key] = val
                    for key, val in need.items():
                        if waited.get(key, 0) >= val:
                            continue
                        waited[key] = val
                        sem = sems[key[0]] if key[1] < 0 else rings[key[0]][key[1]]
                        e.wait_ge(sem, val)
                    ins = o.fn(e)
                    if o.is_dma:
                        ins.then_inc(rings[o.eng][o.dma_no % RING[o.eng]], 16)
                    elif o.signal:
                        ins.then_inc(sems[o.eng], 1)
                for q in QUEUES:
                    if Q_HOST[q] != stream:
                        continue
                    n = self.dma_count[q]
                    R = RING[q]
                    for r in range(R):
                        k = (n - r + R - 1) // R
                        if k > 0 and waited.get((q, r), 0) < 16 * k:
                            e.wait_ge(rings[q][r], 16 * k)

            for stream, attr in handles.items():
                if not self.streams[stream]:
                    continue

                def body(e, stream=stream):
                    run_stream(stream, e)
                getattr(block, attr)(body)


class SlotPool:
    def __init__(self, n, key):
        self.free = list(range(n))
        self.key = key

    def get(self):
        assert self.free, "pool %s exhausted" % self.key
        return self.free.pop(0)

    def put(self, i):
        self.free.append(i)


DBG = {"nseq": NPS, "nst": 4, "sample": True, "moe": True, "mix": ("attn", "gla", "cross"), "merge": True,
       "ncores": NCORES}


def build_program():
    nc = bass.Bass("TRN2", target_bir_lowering=False)
    S = Sched()

    def din(name, shape, dt=F32):
        return nc.dram_tensor(name, list(shape), dt, kind="ExternalInput").ap()

    def dout(name, shape, dt=F32):
        return nc.dram_tensor(name, list(shape), dt, kind="ExternalOutput").ap()

    def dint(name, shape, dt=F32):
        return nc.dram_tensor(name, list(shape), dt, kind="Internal").ap()

    xp = din("xp", [NPS, SEQ, D])
    xsm = din("xs", [128, D])
    memp = din("memp", [NPS, 256, D])
    cw = [din("cw1", [NSB, 128, 2, 4, 64]), din("cw2", [NSB, 512, 2, 4, 64]), din("cw3", [NSB, 2048, 2, 4, 64])]
    sgla = din("sgla", [NSB, 4, 64, 128])
    cmem = din("cmem", [NSB, 256, 2, 4, 128])
    norm_mix = din("norm_mix", [D])
    w_in = din("w_in", [D, 7440])
    qn_a = din("qn_a", [64])
    kn_a = din("kn_a", [64])
    qn_c = din("qn_c", [128])
    kn_c = din("kn_c", [128])
    gup = din("gla_gate_up", [16, 256])
    gbias = din("gla_gate_bias", [256])
    gla_norm = din("gla_norm", [128])
    mem_norm = din("mem_norm", [D])
    w_mem_kv = din("w_mem_kv", [D, 1024])
    w_br = [din("w_branch_a", [256, D]), din("w_branch_b", [512, D]), din("w_branch_c", [512, D])]
    w_out = din("w_out", [D, D])
    norm_ffn = din("norm_ffn", [D])
    w_rg = din("w_router_group", [D, 4])
    b_rg = din("b_router_group", [4])
    w_re = din("w_router_expert", [D, 32])
    b_re = din("b_router_expert", [32])
    w_eg = din("w_exp_gate", [NEXP, D, 256])
    w_eu = din("w_exp_up", [NEXP, D, 256])
    w_ed = din("w_exp_down", [NEXP, 256, D])

    yp = dout("yp", [NPS, SEQ, D])
    ysm = dout("ys", [128, D])
    wp = [dout("w1p", [NPS, 128, 2, 4, 64]), dout("w2p", [NPS, 512, 2, 4, 64]), dout("w3p", [NPS, 2048, 2, 4, 64])]
    glap = dout("glap", [NPS, 4, 64, 128])
    memkv = dout("memkv", [NPS, 256, 2, 4, 128])
    ws = [dout("w1s", [NSB, 128, 2, 4, 64]), dout("w2s", [NSB, 512, 2, 4, 64]), dout("w3s", [NSB, 2048, 2, 4, 64])]
    glas = dout("glas", [NSB, 4, 64, 128])

    h2s = dint("h2s", [NTOK, D])
    wbf_in = dint("wbf_in", [D, 7440], BF16)
    wbf_out = dint("wbf_out", [D, D], BF16)
    wbf_br = [dint("wbf_br%d" % i, [r, D], BF16) for i, r in enumerate((256, 512, 512))]
    wbf_mem = dint("wbf_mem", [D, 1024], BF16)
    xsc = dint("xsc", [NEXP * CAP, D], BF16)
    ysc = dint("ysc", [NEXP * CAP, D])

    es = contextlib.ExitStack()
    with es:
        def sb(name, shape, dt):
            return es.enter_context(nc.sbuf_tensor(name, list(shape), dt))

        def pst(name, shape, dt):
            return es.enter_context(nc.psum_tensor(name, list(shape), dt))

        NG = 4
        psg = [pst("psg%d" % i, [128, 512], F32) for i in range(NG)]
        pstr = [pst("pstr%d" % i, [128, 1024], BF16) for i in range(2)]
        psN = pst("psN", [128, 512], F32)
        psL = pst("psL", [128, 512], F32)
        gpool = SlotPool(NG, "psg")
        trpool = SlotPool(2, "pstr")

        NF = 12
        NB = 10
        NX = 3
        NW = 3
        WCOLS = 512
        f32p = sb("f32p", [128, NF, 512], F32)
        bfp = sb("bfp", [128, NB, 1024], BF16)
        xpl = sb("xpl", [128, NX, 1024], F32)
        wbuf = sb("wbuf", [128, NW, 8, WCOLS], BF16)
        smallp = sb("smallp", [128, 96, 8], F32)
        fpool, bpool, xpool, wpool, spool = (SlotPool(NF, "f"), SlotPool(NB, "b"), SlotPool(NX, "x"),
                                             SlotPool(NW, "w"), SlotPool(96, "s"))
        xnT = sb("xnT", [128, 8, 512], BF16)
        kAT = sb("kAT", [128, 6, SEQ], BF16)
        vA = sb("vA", [128, 16, 768], BF16)
        qAT = sb("qAT", [128, 6, 512], BF16)
        big1 = sb("big1", [128, 8, 512], BF16)
        vb_bf = big1[:, 0:4, :]
        rbT = big1[:, 4:8, :]
        mergedT = big1[:, :, :]
        oaT = sb("oaT", [64, 4, 512], BF16)
        ob2T = sb("ob2T", [128, 4, 512], BF16)
        ocT = sb("ocT", [128, 4, 512], BF16)
        wbr_sb = [sb("wbr_a", [64, 2, 4, 128], BF16), sb("wbr_b", [128, 2, 4, 128], BF16),
                  sb("wbr_c", [128, 2, 4, 128], BF16)]
        wab = sb("wab", [128, 8, 16], BF16)
        S32 = sb("S32", [128, 2, 2, 128], F32)
        Sbf = sb("Sbf", [128, 2, 2, 128], BF16)
        kcT = sb("kcT", [128, 2, 4, 256], BF16)
        vc = sb("vc", [128, 2, 2, 512], BF16)
        kcache = kAT[:, 0:2, 128:1152].rearrange("p s (kt c) -> p s kt c", kt=4)
        vcache = kAT[:, 2:4, 128:1152].rearrange("p s (kt c) -> p s kt c", kt=4)
        kcTA = kAT[:, 4:6, 128:1152].rearrange("p s (kt pr c) -> p s kt pr c", kt=4, pr=2)
        wts = sb("wts", [128, NTILE, 2], F32)
        dsti = sb("dsti", [128, NTILE, 2], I32)
        rbase = sb("rbase", [128, 32], F32)
        ident = sb("ident", [128, 128], BF16)
        cf32 = sb("cf32", [128, 128], F32)
        ones_bf = sb("ones_bf", [128, 128], BF16)
        zeros_bf = sb("zeros_bf", [128, 128], BF16)
        stri_bf = sb("stri_bf", [128, 128], BF16)
        onesdiv = sb("onesdiv", [128, 128], F32)
        tri_p = sb("tri_p", [128, 128], F32)
        tri_s = sb("tri_s", [128, 128], F32)
        blk_p = sb("blk_p", [128, 16], F32)
        blk_s = sb("blk_s", [128, 16], F32)
        E_r = {4: sb("E4", [4, 32, 4], F32), 16: sb("E16", [16, 8, 16], F32)}
        masks = sb("masks", [128, 9, 128], BF16)
        maskS = sb("maskS", [128, 3, 128], BF16)
        maskC = sb("maskC", [128, 6, 4, 8], BF16)
        nmix_rep = sb("nmix_rep", [128, D], F32)
        nffn_rep = sb("nffn_rep", [128, D], F32)
        qna_rep = sb("qna_rep", [128, 64], F32)
        kna_rep = sb("kna_rep", [128, 64], F32)
        qnc_rep = sb("qnc_rep", [128, 128], F32)
        knc_rep = sb("knc_rep", [128, 128], F32)
        gbias_rep = sb("gbias_rep", [128, 256], F32)
        rbias_rep = sb("rbias_rep", [128, 36], F32)
        ecap = sb("ecap", [128, 32], F32)
        gnorm_c = sb("gnorm_c", [128, 1], F32)
        eps_c = sb("eps_c", [128, 1], F32)
        gup_bf = sb("gup_bf", [16, 256], BF16)
        wr_bf = sb("wr_bf", [128, 8, 36], BF16)

        def fk(i): return "f%d" % i
        def bk(i): return "b%d" % i
        def xk(i): return "x%d" % i
        def wk(i): return "w%d" % i
        def sk(i): return "s%d" % i
        def gk(i): return "psg%d" % i
        def tk(i): return "pstr%d" % i

        def mm(out, lhsT, rhs, start, stop, reads, writes):
            S.op("pe", lambda e: e.matmul(out, lhsT=lhsT, rhs=rhs, start=start, stop=stop, skip_group_check=True),
                 reads, writes)

        def tr(out, in_, reads, writes):
            S.op("pe", lambda e: e.transpose(out=out, in_=in_, identity=ident[:]), list(reads) + ["ident"], writes)

        def act(out, in_, func, reads, writes, bias=None, scale=None, accum_out=None):
            kw = {}
            if bias is not None:
                kw["bias"] = bias
            if scale is not None:
                kw["scale"] = scale
            if accum_out is not None:
                kw["accum_out"] = accum_out
            S.op("act", lambda e: e.activation(out=out, in_=in_, func=func, **kw), reads, writes)

        def tt(eng, out, in0, in1, op, reads, writes):
            S.op(eng, lambda e: e.tensor_tensor(out=out, in0=in0, in1=in1, op=op), reads, writes)

        def ts(out, in0, s1, s2, op0, op1, reads, writes):
            if s2 is None:
                S.op("dve", lambda e: e.tensor_scalar(out=out, in0=in0, scalar1=s1, scalar2=None, op0=op0),
                     reads, writes)
            else:
                S.op("dve", lambda e: e.tensor_scalar(out=out, in0=in0, scalar1=s1, scalar2=s2, op0=op0, op1=op1),
                     reads, writes)

        def stt(out, in0, scalar, in1, op0, op1, reads, writes):
            S.op("dve", lambda e: e.scalar_tensor_tensor(out=out, in0=in0, scalar=scalar, in1=in1, op0=op0, op1=op1),
                 reads, writes)

        def cp(eng, out, in_, reads, writes):
            if eng == "act":
                S.op("act", lambda e: e.activation(out=out, in_=in_, func=AF.Copy), reads, writes)
            else:
                S.op(eng, lambda e: e.tensor_copy(out=out, in_=in_), reads, writes)

        def recip(out, in_, reads, writes):
            S.op("dve", lambda e: e.reciprocal(out=out, in_=in_), reads, writes)

        def red(out, in_, op, reads, writes):
            S.op("dve", lambda e: e.tensor_reduce(out=out, in_=in_, axis=AX.X, op=op), reads, writes)

        def mset(eng, ap, val, writes):
            S.op(eng, lambda e: e.memset(ap, val), (), writes)

        def dma(q, out, in_, reads, writes):
            S.op(q, lambda e: e.dma_start(out=out, in_=in_), reads, writes)

        def asel(out, in_, pattern, cmp_op, fill, base, cm, reads, writes):
            S.op("pool", lambda e: e.affine_select(out=out, in_=in_, pattern=pattern, compare_op=cmp_op, fill=fill,
                                                   base=base, channel_multiplier=cm), reads, writes)

        mset("pool", eps_c[:], EPS, ["eps_c"])
        mset("pool", cf32[:], 1.0, ["cf32"])
        asel(cf32[:], cf32[:], [[-1, 128]], ALU.is_equal, 0.0, 0, 1, ["cf32"], ["cf32"])
        cp("dve", ident[:], cf32[:], ["cf32"], ["ident"])
        mset("pool", ones_bf[:], 1.0, ["ones_bf"])
        mset("pool", zeros_bf[:], 0.0, ["zeros_bf"])
        mset("pool", onesdiv[:], 1.0 / 128, ["onesdiv"])
        mset("pool", tri_p[:], 1.0, ["tri_p"])
        asel(tri_p[:], tri_p[:], [[1, 128]], ALU.is_ge, 0.0, 0, -1, ["tri_p"], ["tri_p"])
        mset("pool", cf32[:], 1.0, ["cf32"])
        asel(cf32[:], cf32[:], [[1, 128]], ALU.is_gt, 0.0, 0, -1, ["cf32"], ["cf32"])
        cp("dve", stri_bf[:], cf32[:], ["cf32"], ["stri_bf"])
        cp("pool", tri_s[:], tri_p[:], ["tri_p"], ["tri_s"])
        asel(tri_s[:].rearrange("p (b l) -> p b l", l=8), tri_s[:].rearrange("p (b l) -> p b l", l=8),
             [[-8, 16], [0, 8]], ALU.is_ge, 0.0, 0, 1, ["tri_s"], ["tri_s"])
        mset("pool", blk_p[:], 0.0, ["blk_p"])
        mset("pool", blk_p[:, 0:1], 1.0, ["blk_p"])
        mset("pool", blk_s[:], 1.0, ["blk_s"])
        asel(blk_s[:], blk_s[:], [[-8, 16]], ALU.is_ge, 0.0, 0, 1, ["blk_s"], ["blk_s"])
        asel(blk_s[:], blk_s[:], [[8, 16]], ALU.is_ge, 0.0, 7, -1, ["blk_s"], ["blk_s"])
        ecap_i = sb("ecap_i", [128, 32], I32)
        S.op("pool", lambda e: e.iota(ecap_i[:], pattern=[[CAP, 32]], base=0, channel_multiplier=0), (), ["ecap_i"])
        cp("dve", ecap[:], ecap_i[:], ["ecap_i"], ["ecap"])
        mset("pool", rbase[:], 0.0, ["rbase"])
        dma("q_sp", nmix_rep[:], norm_mix.partition_broadcast(128), (), ["nmix_rep"])
        dma("q_sp", nffn_rep[:], norm_ffn.partition_broadcast(128), (), ["nffn_rep"])
        dma("q_sp", qna_rep[:], qn_a.partition_broadcast(128), (), ["qna_rep"])
        dma("q_sp", kna_rep[:], kn_a.partition_broadcast(128), (), ["kna_rep"])
        dma("q_sp", qnc_rep[:], qn_c.partition_broadcast(128), (), ["qnc_rep"])
        dma("q_sp", knc_rep[:], kn_c.partition_broadcast(128), (), ["knc_rep"])
        dma("q_sp", gbias_rep[:], gbias.partition_broadcast(128), (), ["gbias_rep"])
        dma("q_sp", rbias_rep[:, 0:4], b_rg.partition_broadcast(128), (), ["rbias_rep"])
        dma("q_sp", rbias_rep[:, 4:36], b_re.partition_broadcast(128), ["rbias_rep"], ["rbias_rep"])
        dma("q_sp", gnorm_c[:], gla_norm.rearrange("(p o) -> p o", o=1), (), ["gnorm_c"])
        ts(qna_rep[:], qna_rep[:], 0.125, None, ALU.mult, None, ["qna_rep"], ["qna_rep"])
        ts(qnc_rep[:], qnc_rep[:], float(128 ** -0.5), None, ALU.mult, None, ["qnc_rep"], ["qnc_rep"])
        dma("q_pool", gup_bf[:], gup, (), ["gup_bf"])
        dma("q_pool", wr_bf[:, :, 0:4], w_rg.rearrange("(k p) c -> p k c", p=128), (), ["wr_bf"])
        dma("q_pool", wr_bf[:, :, 4:36], w_re.rearrange("(k p) c -> p k c", p=128), ["wr_bf"], ["wr_bf"])
        dma("q_pool", wab[:], w_in[:, C_AB:C_AB + 16].rearrange("(k p) c -> p k c", p=128), (), ["wab"])
        for gi, r in enumerate(DIL):
            g = gpool.get()
            if r == 1:
                mset("dve", psg[g][:, 0:128], 1.0, [gk(g)])
            else:
                er = E_r[r]
                mset("pool", er[:], 1.0, ["E%d" % r])
                asel(er[:], er[:], [[0, 128 // r], [1, r]], ALU.is_equal, 0.0, 0, -1, ["E%d" % r], ["E%d" % r])
                er2 = er[:].rearrange("p a b -> p (a b)")
                mm(psg[g][:, 0:128], er2, er2, True, True, ["E%d" % r], [gk(g)])
            f = fpool.get()
            ts(f32p[:, f, 0:128], psg[g][:, 0:128], -1.0, -NEG, ALU.add, ALU.mult, [gk(g)], [fk(f)])
            gpool.put(g)
            cp("dve", masks[:, 3 * gi + 1, :], f32p[:, f, 0:128], [fk(f)], ["masks"])
            f2 = fpool.get()
            asel(f32p[:, f2, 0:128], f32p[:, f, 0:128], [[1, 128]], ALU.is_ge, NEG, 0, -1, [fk(f)], [fk(f2)])
            cp("dve", masks[:, 3 * gi + 0, :], f32p[:, f2, 0:128], [fk(f2)], ["masks"])
            asel(f32p[:, f2, 0:128], f32p[:, f, 0:128], [[-1, 128]], ALU.is_ge, NEG, 0, 1, [fk(f)], [fk(f2)])
            cp("dve", masks[:, 3 * gi + 2, :], f32p[:, f2, 0:128], [fk(f2)], ["masks"])
            asel(f32p[:, f2, 0:128], f32p[:, f, 0:128], [[1, 128]], ALU.is_ge, NEG, 0, -1, [fk(f)], [fk(f2)])
            v3 = f32p[:, f2, 0:128].rearrange("p (b l) -> p b l", l=8)
            asel(v3, v3, [[-8, 16], [0, 8]], ALU.is_ge, NEG, 0, 1, [fk(f2)], [fk(f2)])
            cp("dve", maskS[:, gi, :], f32p[:, f2, 0:128], [fk(f2)], ["maskS"])
            fpool.put(f)
            fpool.put(f2)
            for hh in range(4):
                cp("pool", maskC[:, 2 * gi + 0, hh, :], masks[:, 3 * gi + 2, 0:8], ["masks"], ["maskC"])
                cp("pool", maskC[:, 2 * gi + 1, hh, :], masks[:, 3 * gi + 1, 0:8], ["masks"], ["maskC"])

        def rms_stats(src_ap, n, reads, tag):
            s = spool.get()
            jb = bpool.get()
            mset("pool", smallp[:, s, 0:1], 0.0, [sk(s)])
            act(bfp[:, jb, 0:n], src_ap, AF.Square, list(reads) + [sk(s)], [bk(jb), sk(s)],
                accum_out=smallp[:, s, 0:1])
            bpool.put(jb)
            act(smallp[:, s, 1:2], smallp[:, s, 0:1], AF.Ln, [sk(s), "eps_c"], [sk(s)], bias=eps_c[:], scale=1.0 / n)
            act(smallp[:, s, 0:1], smallp[:, s, 1:2], AF.Exp, [sk(s)], [sk(s)], scale=-0.5)
            return s

        def transposes_to(dst3, src2, nblk, reads, writes):
            t = trpool.get()
            for j in range(nblk):
                tr(pstr[t][:, j * 128:(j + 1) * 128], src2[:, j * 128:(j + 1) * 128], reads, [tk(t)])
            cp("dve", dst3, pstr[t][:, 0:nblk * 128].rearrange("p (k c) -> p k c", k=nblk), [tk(t)], writes)
            trpool.put(t)

        def front_a(x_src, grep, grep_key):
            x = xpool.get()
            dma("q_sp", xpl[:, x, :], x_src, (), [xk(x)])
            s = rms_stats(xpl[:, x, :], D, [xk(x)], "fr")
            b = bpool.get()
            stt(bfp[:, b, :], xpl[:, x, :], smallp[:, s, 0:1], grep, ALU.mult, ALU.mult,
                [xk(x), sk(s), grep_key], [bk(b)])
            spool.put(s)
            xpool.put(x)
            return b

        def front_b(b, dstT, dst_key):
            transposes_to(dstT, bfp[:, b, :], 8, [bk(b)], [dst_key])
            bpool.put(b)

        def front(x_src, grep, grep_key, dstT, dst_key, keep_x=False):
            front_b(front_a(x_src, grep, grep_key), dstT, dst_key)

        def wload(src3, kk, cols):
            src3, skey = src3
            w = wpool.get()
            dma("q_pool", wbuf[:, w, 0:kk, 0:cols], src3, [skey], [wk(w)])
            return w

        conv_blocks = ([(C_QA + i * 256, 256) for i in range(9)] + [(C_VB, 512), (C_RB, 512), (C_QC, 512), (C_QB, 512)]
                       + [(C_GL + i * 512, 512) for i in range(6)])
        for (c0, cols) in conv_blocks:
            dma("q_pool", wbf_in[:, c0:c0 + cols], w_in[:, c0:c0 + cols], (), ["wbfin%d" % c0])
        for b_ in range(3):
            dma("q_pool", wbf_br[b_][:, :], w_br[b_][:, :], (), ["wbfbr%d" % b_])
        for hf in range(2):
            dma("q_pool", wbf_out[:, hf * 512:(hf + 1) * 512], w_out[:, hf * 512:(hf + 1) * 512], (),
                ["wbfout%d" % hf])

        def w_in_blk(c0, cols):
            return (wbf_in[:, c0:c0 + cols].rearrange("(k p) c -> p k c", p=128), "wbfin%d" % c0)

        def proj_tok(actT, act_key, t0, w, cols, ps_ap, ps_key, wc0=0):
            for k in range(8):
                mm(ps_ap, actT[:, k, t0:t0 + 128], wbuf[:, w, k, wc0:wc0 + cols], k == 0, k == 7,
                   [act_key, wk(w)], [ps_key])

        def headnorm(ps_ap, ps_key, nh, hd, grep, grep_key, outs):
            f = fpool.get()
            s = spool.get()
            act(f32p[:, f, 0:nh * hd], ps_ap, AF.Square, [ps_key], [fk(f)])
            red(smallp[:, s, 0:nh], f32p[:, f, 0:nh * hd].rearrange("p (h d) -> p h d", h=nh), ALU.add,
                [fk(f)], [sk(s)])
            fpool.put(f)
            s2 = spool.get()
            act(smallp[:, s2, 0:nh], smallp[:, s, 0:nh], AF.Ln, [sk(s), "eps_c"], [sk(s2)], bias=eps_c[:],
                scale=1.0 / hd)
            act(smallp[:, s, 0:nh], smallp[:, s2, 0:nh], AF.Exp, [sk(s2)], [sk(s)], scale=-0.5)
            spool.put(s2)
            for (dst3, dkey) in outs:
                for h in range(nh):
                    stt(dst3[:, h, :], ps_ap[:, h * hd:(h + 1) * hd], smallp[:, s, h:h + 1], grep[:, 0:hd],
                        ALU.mult, ALU.mult, [ps_key, sk(s), grep_key], [dkey])
            spool.put(s)

        state = {"tile_no": 0}

        def attn_blocks(blocks, hsel=None):
            for _ in attn_blocks_gen(blocks):
                pass

        def attn_blocks_gen(blocks):
            groups = []
            i = 0
            while i < len(blocks):
                grp = []
                tot = 0
                while i < len(blocks) and tot + blocks[i]["ncol"] <= 512:
                    grp.append((blocks[i], tot))
                    tot += blocks[i]["ncol"]
                    i += 1
                groups.append((grp, tot))

            def pv_stage(grp, pb):
                for (b, c0) in grp:
                    for (v, cc, cn, oN, oL) in b["pv"]:
                        mm(oN, v, bfp[:, pb, c0 + cc:c0 + cc + cn], False, False, [b["vkey"], bk(pb)], ["psN"])
                        mm(oL, ones_bf[:, 0:64], bfp[:, pb, c0 + cc:c0 + cc + cn], False, False,
                           ["ones_bf", bk(pb)], ["psL"])
                bpool.put(pb)

            prev = None
            for (grp, tot) in groups:
                g = gpool.get()
                for (b, c0) in grp:
                    n = b["ncol"]
                    if len(b["sub"]) == 1:
                        mm(psg[g][:, c0:c0 + n], ident[:], b["mask"], True, False, ["ident", b["mkey"]], [gk(g)])
                        (kT, q, cc, cn) = b["sub"][0]
                        mm(psg[g][:, c0 + cc:c0 + cc + cn], kT, q, False, True, [b["kkey"], b["qkey"]], [gk(g)])
                    else:
                        hn = n // 2
                        first = True
                        for half in range(2):
                            mm(psg[g][:, c0 + half * hn:c0 + (half + 1) * hn], ident[:],
                               b["mask"][:, half * hn:(half + 1) * hn], first, False, ["ident", b["mkey"]], [gk(g)])
                            first = False
                            for (kT, q, cc, cn) in b["sub"][half * 2:half * 2 + 2]:
                                mm(psg[g][:, c0 + cc:c0 + cc + cn], kT, q, False, True, [b["kkey"], b["qkey"]],
                                   [gk(g)])
                pb = bpool.get()
                act(bfp[:, pb, 0:tot], psg[g][:, 0:tot], AF.Exp, [gk(g)], [bk(pb)])
                gpool.put(g)
                if prev is not None:
                    pv_stage(*prev)
                prev = (grp, pb)
                yield
            if prev is not None:
                pv_stage(*prev)
            yield

        def attn_finish(t0):
            f = fpool.get()
            recip(f32p[0:64, f, :], psL[0:64, :], ["psL"], [fk(f)])
            tt("dve", oaT[:, :, t0:t0 + 128], psN[0:64, :].rearrange("p (h t) -> p h t", h=4),
               f32p[0:64, f, :].rearrange("p (h t) -> p h t", h=4), ALU.mult, ["psN", fk(f)], ["oaT"])
            fpool.put(f)

        def attn_prompt_tile(qt, t0):
            mset("dve", psN[0:64, :], 0.0, ["psN"])
            mset("dve", psL[0:64, :], 0.0, ["psL"])
            blocks = []
            for h in range(4):
                hp, pr = (h % 2) * 64, h // 2
                for gi in range(3):
                    nd = WIN[gi] // 128
                    ch = gi * 2 + pr
                    for kt in range(max(0, qt - nd), qt + 1):
                        dlt = qt - kt
                        typ = 0 if dlt == 0 else (2 if dlt == nd else 1)
                        blocks.append(dict(
                            mask=masks[:, 3 * gi + typ, :], mkey="masks", kkey="kAT", qkey="qAT", vkey="vA", ncol=128,
                            sub=[(kAT[hp:hp + 64, ch, kt * 128:(kt + 1) * 128], qAT[hp:hp + 64, ch, t0:t0 + 128], 0, 128)],
                            pv=[(vA[:, kt, (gi * 4 + h) * 64:(gi * 4 + h + 1) * 64], 0, 128,
                                 psN[0:64, h * 128:(h + 1) * 128], psL[0:64, h * 128:(h + 1) * 128])]))
            yield from attn_blocks_gen(blocks)
            attn_finish(t0)
            yield

        def run_threads(gens):
            gens = list(gens)
            while gens:
                for gtor in list(gens):
                    try:
                        next(gtor)
                    except StopIteration:
                        gens.remove(gtor)

        def chain(makers):
            for mk in makers:
                yield from mk()

        def gla_tile(ti, t0, tri, tri_key, blk, blk_key, nblk, s_loader, s_store):
            bw = 128 // nblk
            g = gpool.get()
            for k in range(8):
                mm(psg[g][:, 0:16], xnT[:, k, t0:t0 + 128], wab[:, k, :], k == 0, k == 7, ["xnT", "wab"], [gk(g)])
            b1 = bpool.get()
            cp("act", bfp[:, b1, 0:16], psg[g][:, 0:16], [gk(g)], [bk(b1)])
            gpool.put(g)
            yield
            t = trpool.get()
            tr(pstr[t][0:16, 0:128], bfp[:, b1, 0:16], [bk(b1)], [tk(t)])
            cp("dve", bfp[0:16, b1, 128:256], pstr[t][0:16, 0:128], [tk(t)], [bk(b1)])
            trpool.put(t)
            yield
            g = gpool.get()
            mm(psg[g][:, 0:256], bfp[0:16, b1, 128:256], gup_bf[:], True, True, [bk(b1), "gup_bf"], [gk(g)])
            bpool.put(b1)
            fz = fpool.get()
            tt("dve", f32p[:, fz, 0:256], psg[g][:, 0:256], gbias_rep[:], ALU.add, [gk(g), "gbias_rep"], [fk(fz)])
            gpool.put(g)
            act(f32p[:, fz, 256:512], f32p[:, fz, 0:256], AF.Exp, [fk(fz)], [fk(fz)], scale=-1.0)
            act(f32p[:, fz, 0:256], f32p[:, fz, 256:512], AF.Ln, [fk(fz)], [fk(fz)], bias=1.0)
            yield
            g = gpool.get()
            mm(psg[g][:, 0:256], tri, f32p[:, fz, 0:256], True, True, [tri_key, fk(fz)], [gk(g)])
            for pr in range(2):
                mm(psg[g][:, 256 + pr * 16:256 + pr * 16 + nblk], f32p[:, fz, pr * 128:(pr + 1) * 128], blk[:, 0:nblk],
                   True, True, [fk(fz), blk_key], [gk(g)])
            fe = fpool.get()
            act(f32p[:, fe, 0:256], psg[g][:, 0:256], AF.Exp, [gk(g)], [fk(fe)], scale=-1.0 / 16)
            act(f32p[:, fe, 256:512], psg[g][:, 0:256], AF.Exp, [gk(g)], [fk(fe)], scale=1.0 / 16)
            ebl = f32p[:, fz, 256:256 + 32]
            act(ebl, psg[g][:, 256:288], AF.Exp, [gk(g), fk(fz)], [fk(fz)], scale=-1.0 / 16)
            gpool.put(g)
            yield
            w = state["w_qkb"]
            g = gpool.get()
            proj_tok(xnT, "xnT", t0, w, 512, psg[g][:, :], gk(g))
            bq = bpool.get()
            stt(bfp[:, bq, 0:256], psg[g][:, 0:256], 0.125, f32p[:, fe, 0:256], ALU.mult, ALU.mult,
                [gk(g), fk(fe)], [bk(bq)])
            tt("dve", bfp[:, bq, 256:512], psg[g][:, 256:512], f32p[:, fe, 256:512], ALU.mult, [gk(g), fk(fe)],
               [bk(bq)])
            gpool.put(g)
            fpool.put(fe)
            yield
            bT = bpool.get()
            transposes_to(bfp[:, bT, 0:512].rearrange("p (k c) -> p k c", k=4), bfp[:, bq, 0:512], 4, [bk(bq)],
                          [bk(bT)])
            DBG.setdefault("gla_slots", dict(bq=bq, bT=bT, fz=fz))
            yield
            gAB = [gpool.get(), gpool.get()]
            for h in (0, 2, 1, 3):
                hp, pr = (h % 2) * 64, h // 2
                g = gAB[h % 2]
                mm(psg[g][:, pr * 128:(pr + 1) * 128], bfp[hp:hp + 64, bT, 256 + pr * 128:256 + (pr + 1) * 128],
                   bfp[hp:hp + 64, bT, pr * 128:(pr + 1) * 128], True, True, [bk(bT)], [gk(g)])
            ba = bpool.get()
            for h in range(4):
                g = gAB[h % 2]
                pr = h // 2
                tt("dve", bfp[:, ba, h * 128:(h + 1) * 128], psg[g][:, pr * 128:(pr + 1) * 128], tri, ALU.mult,
                   [gk(g), tri_key], [bk(ba)])
            gpool.put(gAB[0])
            gpool.put(gAB[1])
            yield
            go = gpool.get()
            if nblk == 1:
                slot, skey = s_loader(0, "o")
                for h in range(4):
                    hp, pr = (h % 2) * 64, h // 2
                    mm(psg[go][:, h * 128:(h + 1) * 128], vb_bf[:, ti, h * 128:(h + 1) * 128],
                       bfp[:, ba, h * 128:(h + 1) * 128], True, False, ["big1", bk(ba)], [gk(go)])
                    mm(psg[go][:, h * 128:(h + 1) * 128], Sbf[hp:hp + 64, slot, pr, :],
                       bfp[hp:hp + 64, bT, pr * 128:(pr + 1) * 128], False, True, [skey + "bf", bk(bT)], [gk(go)])
            else:
                mset("dve", psg[go][:, :], 0.0, [gk(go)])
                for h in range(4):
                    mm(psg[go][:, h * 128:(h + 1) * 128], vb_bf[:, ti, h * 128:(h + 1) * 128],
                       bfp[:, ba, h * 128:(h + 1) * 128], False, False, ["big1", bk(ba)], [gk(go)])
                for bi in range(nblk):
                    slot, skey = s_loader(bi, "o")
                    for h in (0, 2, 1, 3):
                        hp, pr = (h % 2) * 64, h // 2
                        if h in (0, 1):
                            mm(psg[go][:, 0:8], zeros_bf[:, :], ones_bf[:, 0:8], False, False,
                               ["zeros_bf", "ones_bf"], [gk(go)])
                        mm(psg[go][:, h * 128 + bi * bw:h * 128 + (bi + 1) * bw], Sbf[hp:hp + 64, slot, pr, :],
                           bfp[hp:hp + 64, bT, pr * 128 + bi * bw:pr * 128 + (bi + 1) * bw], False, False,
                           [skey + "bf", bk(bT)], [gk(go)])
            bpool.put(ba)
            yield
            for bi in range(nblk):
                slot, skey = s_loader(bi, "u")
                if nblk == 1:
                    kesrc, kkey, kb_ = bfp[:, bq, 256:512], bk(bq), None
                else:
                    kb_ = bpool.get()
                    ts(bfp[:, kb_, 0:256], bfp[:, bq, 256:512], blk[:, bi:bi + 1], None, ALU.mult, None,
                       [bk(bq), blk_key], [bk(kb_)])
                    kesrc, kkey = bfp[:, kb_, 0:256], bk(kb_)
                g = gpool.get()
                for h in range(4):
                    pr = h // 2
                    mm(psg[g][:, h * 128:(h + 1) * 128], kesrc[:, pr * 128:(pr + 1) * 128],
                       vb_bf[:, ti, h * 128:(h + 1) * 128], True, True, [kkey, "big1"], [gk(g)])
                if kb_ is not None:
                    bpool.put(kb_)
                for h in range(4):
                    hp, pr = (h % 2) * 64, h // 2
                    tt("dve", S32[hp:hp + 64, slot, pr, :], psg[g][hp:hp + 64, h * 128:(h + 1) * 128],
                       S32[hp:hp + 64, slot, pr, :], ALU.add, [gk(g), skey], [skey])
                    ts(S32[hp:hp + 64, slot, pr, :], S32[hp:hp + 64, slot, pr, :],
                       f32p[hp:hp + 64, fz, 256 + pr * 16 + bi:256 + pr * 16 + bi + 1], None, ALU.mult, None,
                       [skey, fk(fz)], [skey])
                gpool.put(g)
                s_store(bi, slot, skey)
            bpool.put(bq)
            bpool.put(bT)
            fpool.put(fz)
            yield
            fo = fpool.get()
            fq = fpool.get()
            cp("dve", f32p[:, fo, :], psg[go][:, :], [gk(go)], [fk(fo)])
            act(f32p[:, fq, :], psg[go][:, :], AF.Square, [gk(go)], [fk(fq)])
            gpool.put(go)
            yield
            gm = gpool.get()
            gs = gpool.get()
            mm(psg[gm][:, :], onesdiv[:], f32p[:, fo, :], True, True, ["onesdiv", fk(fo)], [gk(gm)])
            mm(psg[gs][:, :], onesdiv[:], f32p[:, fq, :], True, True, ["onesdiv", fk(fq)], [gk(gs)])
            act(f32p[:, fq, :], psg[gm][:, :], AF.Square, [gk(gm), fk(fq)], [fk(fq)])
            tt("dve", f32p[:, fq, :], psg[gs][:, :], f32p[:, fq, :], ALU.subtract, [gk(gs), fk(fq)], [fk(fq)])
            gpool.put(gs)
            ts(f32p[:, fq, :], f32p[:, fq, :], 0.0, None, ALU.max, None, [fk(fq)], [fk(fq)])
            act(f32p[:, fq, :], f32p[:, fq, :], AF.Ln, [fk(fq), "eps_c"], [fk(fq)], bias=eps_c[:])
            act(f32p[:, fq, :], f32p[:, fq, :], AF.Exp, [fk(fq)], [fk(fq)], scale=-0.5)
            tt("dve", f32p[:, fo, :], f32p[:, fo, :], psg[gm][:, :], ALU.subtract, [fk(fo), gk(gm)], [fk(fo)])
            gpool.put(gm)
            tt("dve", f32p[:, fo, :], f32p[:, fo, :], f32p[:, fq, :], ALU.mult, [fk(fo), fk(fq)], [fk(fo)])
            fpool.put(fq)
            stt(ob2T[:, :, t0:t0 + 128], f32p[:, fo, :].rearrange("p (h t) -> p h t", h=4), gnorm_c[:, 0:1],
                rbT[:, :, t0:t0 + 128], ALU.mult, ALU.mult, [fk(fo), "gnorm_c", "big1"], ["ob2T"])
            fpool.put(fo)
            yield

        def cross_tile(t0, qc_b, groups):
            goc = gpool.get()
            gl = gpool.get()
            for pair in range(2):
                g = gpool.get()
                for hh in range(2):
                    h = pair * 2 + hh
                    for blk_i in range(2):
                        for (slot, key, c0, cn) in groups:
                            col = (hh * 2 + blk_i) * 128 + c0
                            mm(psg[g][:, col:col + cn], kcT[:, slot, h, blk_i * 128:(blk_i + 1) * 128],
                               bfp[:, qc_b, h * 128 + c0:h * 128 + c0 + cn], True, True, [key + "k", bk(qc_b)],
                               [gk(g)])
                pb = bpool.get()
                act(bfp[:, pb, 0:512], psg[g][:, :], AF.Exp, [gk(g)], [bk(pb)])
                gpool.put(g)
                for hh in range(2):
                    h = pair * 2 + hh
                    for (slot, key, c0, cn) in groups:
                        for blk_i in range(2):
                            col = (hh * 2 + blk_i) * 128 + c0
                            mm(psg[goc][:, h * 128 + c0:h * 128 + c0 + cn],
                               vc[:, slot, blk_i, h * 128:(h + 1) * 128], bfp[:, pb, col:col + cn],
                               blk_i == 0, blk_i == 1, [key + "v", bk(pb)], [gk(goc)])
                        for blk_i in range(2):
                            col = (hh * 2 + blk_i) * 128 + c0
                            mm(psg[gl][:, h * 128 + c0:h * 128 + c0 + cn], ones_bf[:, :], bfp[:, pb, col:col + cn],
                               blk_i == 0, blk_i == 1, ["ones_bf", bk(pb)], [gk(gl)])
                bpool.put(pb)
            f = fpool.get()
            recip(f32p[:, f, :], psg[gl][:, :], [gk(gl)], [fk(f)])
            gpool.put(gl)
            tt("dve", ocT[:, :, t0:t0 + 128], psg[goc][:, :].rearrange("p (h t) -> p h t", h=4),
               f32p[:, f, :].rearrange("p (h t) -> p h t", h=4), ALU.mult, [gk(goc), fk(f)], ["ocT"])
            gpool.put(goc)
            fpool.put(f)

        def route_tile(x, tile_no):
            s = rms_stats(xpl[:, x, :], D, [xk(x)], "ffn")
            bx = bpool.get()
            stt(bfp[:, bx, :], xpl[:, x, :], smallp[:, s, 0:1], nffn_rep[:], ALU.mult, ALU.mult,
                [xk(x), sk(s), "nffn_rep"], [bk(bx)])
            spool.put(s)
            bT = bpool.get()
            transposes_to(bfp[:, bT, :].rearrange("p (k c) -> p k c", k=8), bfp[:, bx, :], 8, [bk(bx)], [bk(bT)])
            g = gpool.get()
            for k in range(8):
                mm(psg[g][:, 0:36], bfp[:, bT, k * 128:(k + 1) * 128], wr_bf[:, k, :], k == 0, k == 7,
                   [bk(bT), "wr_bf"], [gk(g)])
            bpool.put(bT)
            f = fpool.get()
            F_ = f32p[:, f, :]
            tt("dve", F_[:, 0:36], psg[g][:, 0:36], rbias_rep[:], ALU.add, [gk(g), "rbias_rep"], [fk(f)])
            gpool.put(g)
            s = spool.get()
            sm = smallp[:, s, :]
            red(sm[:, 0:1], F_[:, 0:4], ALU.max, [fk(f)], [sk(s)])
            ts(F_[:, 40:44], F_[:, 0:4], sm[:, 0:1], None, ALU.is_ge, None, [fk(f), sk(s)], [fk(f)])
            ts(sm[:, 1:2], sm[:, 0:1], -1.0, None, ALU.mult, None, [sk(s)], [sk(s)])
            mset("pool", sm[:, 2:3], 0.0, [sk(s)])
            act(F_[:, 44:48], F_[:, 0:4], AF.Exp, [fk(f), sk(s)], [fk(f), sk(s)], bias=sm[:, 1:2], accum_out=sm[:, 2:3])
            recip(sm[:, 3:4], sm[:, 2:3], [sk(s)], [sk(s)])
            ts(F_[:, 44:48], F_[:, 40:44], -NEG, NEG, ALU.mult, ALU.add, [fk(f)], [fk(f)])
            tt("dve", F_[:, 64:96].rearrange("p (g e) -> p g e", g=4),
               F_[:, 4:36].rearrange("p (g e) -> p g e", g=4),
               F_[:, 44:48].unsqueeze(2).to_broadcast([128, 4, 8]), ALU.add, [fk(f)], [fk(f)])
            S.op("dve", lambda e: e.max(out=F_[:, 96:104], in_=F_[:, 64:96]), [fk(f)], [fk(f)])
            ts(F_[:, 128:160], F_[:, 64:96], F_[:, 96:97], None, ALU.is_ge, None, [fk(f)], [fk(f)])
            ts(F_[:, 160:192], F_[:, 64:96], F_[:, 97:98], None, ALU.is_ge, None, [fk(f)], [fk(f)])
            tt("dve", F_[:, 160:192], F_[:, 160:192], F_[:, 128:160], ALU.subtract, [fk(f)], [fk(f)])
            ts(sm[:, 4:5], F_[:, 96:97], -1.0, None, ALU.mult, None, [fk(f)], [sk(s)])
            act(sm[:, 5:6], F_[:, 97:98], AF.Exp, [fk(f), sk(s)], [sk(s)], bias=sm[:, 4:5])
            ts(sm[:, 6:7], sm[:, 5:6], 1.0, None, ALU.add, None, [sk(s)], [sk(s)])
            recip(sm[:, 6:7], sm[:, 6:7], [sk(s)], [sk(s)])
            tt("dve", wts[:, tile_no, 0:1], sm[:, 6:7], sm[:, 3:4], ALU.mult, [sk(s)], ["wts"])
            tt("dve", wts[:, tile_no, 1:2], wts[:, tile_no, 0:1], sm[:, 5:6], ALU.mult, [sk(s), "wts"], ["wts"])
            ba = bpool.get()
            cp("dve", bfp[:, ba, 0:64], F_[:, 128:192], [fk(f)], [bk(ba)])
            g = gpool.get()
            mm(psg[g][:, 0:64], stri_bf[:], bfp[:, ba, 0:64], True, True, ["stri_bf", bk(ba)], [gk(g)])
            mm(psg[g][:, 64:128], ones_bf[:], bfp[:, ba, 0:64], True, True, ["ones_bf", bk(ba)], [gk(g)])
            bpool.put(ba)
            tt("dve", F_[:, 256:288], rbase[:], ecap[:], ALU.add, ["rbase", "ecap"], [fk(f)])
            tt("dve", F_[:, 288:320], F_[:, 256:288], psg[g][:, 64:96], ALU.add, [fk(f), gk(g)], [fk(f)])
            tt("dve", F_[:, 256:288], F_[:, 256:288], psg[g][:, 0:32], ALU.add, [fk(f), gk(g)], [fk(f)])
            tt("dve", F_[:, 288:320], F_[:, 288:320], psg[g][:, 32:64], ALU.add, [fk(f), gk(g)], [fk(f)])
            tt("dve", F_[:, 256:288], F_[:, 256:288], F_[:, 128:160], ALU.mult, [fk(f)], [fk(f)])
            tt("dve", F_[:, 288:320], F_[:, 288:320], F_[:, 160:192], ALU.mult, [fk(f)], [fk(f)])
            red(sm[:, 0:2], F_[:, 256:320].rearrange("p (a e) -> p a e", a=2), ALU.add, [fk(f)], [sk(s)])
            tt("dve", rbase[:], rbase[:], psg[g][:, 64:96], ALU.add, ["rbase", gk(g)], ["rbase"])
            tt("dve", rbase[:], rbase[:], psg[g][:, 96:128], ALU.add, ["rbase", gk(g)], ["rbase"])
            gpool.put(g)
            cp("dve", dsti[:, tile_no, :], sm[:, 0:2], [sk(s)], ["dsti"])
            spool.put(s)
            fpool.put(f)
            for j in range(2):
                S.op("q_pool", lambda e, j=j: e.indirect_dma_start(
                    out=xsc[:, :], out_offset=bass.IndirectOffsetOnAxis(ap=dsti[:, tile_no, j:j + 1], axis=0),
                    in_=bfp[:, bx, :], in_offset=None),
                    [bk(bx), "dsti"] + ["xsc_z%d" % r for r in range(NEXP * CAP // 512)], ["xsc"])
            bpool.put(bx)

        def merge_supertile(ntile, x_src_of, tok0, prefetch=None):
            T = ntile * 128
            if prefetch is not None:
                prefetch()
            srcs = [(oaT, "oaT", 64, 4), (ob2T, "ob2T", 128, 4), (ocT, "ocT", 128, 4)]
            order = [("g", half, b) for half in range(2) for b in range(3)] + [("o", hf, 0) for hf in range(2)]
            wq = {}

            def ensure(n):
                if n < len(order) and n not in wq:
                    kind, p0, p1 = order[n]
                    if kind == "g":
                        wq[n] = wload(w_in_blk(C_GL + p1 * 1024 + p0 * 512, 512), 8, 512)
                    else:
                        wq[n] = wload((wbf_out[:, p0 * 512:(p0 + 1) * 512].rearrange("(k p) c -> p k c", p=128),
                                       "wbfout%d" % p0), 8, 512)
            ensure(0)
            acc = None
            for n in range(6):
                ensure(n + 1)
                _, half, b = order[n]
                w = wq[n]
                if b == 0:
                    acc = [fpool.get() for _ in range(4)]
                src, skey, np_, nj = srcs[b]
                for cc in range(4):
                    c = half * 4 + cc
                    par = cc % 2
                    wkey = "wbr%d_%d" % (b, par)
                    dma("q_pool", wbr_sb[b][:, par, :, :],
                        wbf_br[b][:, c * 128:(c + 1) * 128].rearrange("(j p) c -> p j c", p=np_), ["wbfbr%d" % b],
                        [wkey])
                    g = gpool.get()
                    for k in range(8):
                        mm(psg[g][:, 0:T], wbuf[:, w, k, cc * 128:(cc + 1) * 128], xnT[:, k, 0:T],
                           k == 0, k == 7, [wk(w), "xnT"], [gk(g)])
                    sgb = bpool.get()
                    act(bfp[:, sgb, 0:T], psg[g][:, 0:T], AF.Sigmoid, [gk(g)], [bk(sgb)])
                    gpool.put(g)
                    g = gpool.get()
                    for j in range(nj):
                        mm(psg[g][:, 0:T], wbr_sb[b][0:np_, par, j, :], src[0:np_, j, 0:T], j == 0, j == nj - 1,
                           [wkey, skey], [gk(g)])
                    if b == 0:
                        tt("dve", f32p[:, acc[cc], 0:T], psg[g][:, 0:T], bfp[:, sgb, 0:T], ALU.mult,
                           [gk(g), bk(sgb)], [fk(acc[cc])])
                    else:
                        ft = fpool.get()
                        tt("dve", f32p[:, ft, 0:T], psg[g][:, 0:T], bfp[:, sgb, 0:T], ALU.mult, [gk(g), bk(sgb)],
                           [fk(ft)])
                        if b == 1:
                            tt("dve", f32p[:, acc[cc], 0:T], f32p[:, acc[cc], 0:T], f32p[:, ft, 0:T], ALU.add,
                               [fk(acc[cc]), fk(ft)], [fk(acc[cc])])
                        else:
                            tt("dve", mergedT[:, c, 0:T], f32p[:, acc[cc], 0:T], f32p[:, ft, 0:T], ALU.add,
                               [fk(acc[cc]), fk(ft)], ["big1"])
                        fpool.put(ft)
                    gpool.put(g)
                    bpool.put(sgb)
                wpool.put(w)
                if b == 2:
                    for a_ in acc:
                        fpool.put(a_)
            ensure(7)
            wo = [wq[6], wq[7]]
            if prefetch is not None and state.get("more_tiles"):
                state["w_pre"] = wload(w_in_blk(C_QA, 256), 8, 256)
            prev = None
            for i in range(ntile):
                x = xpool.get()
                dma("q_sp", xpl[:, x, :], x_src_of(i), (), [xk(x)])
                for hf in range(2):
                    g = gpool.get()
                    for c in range(8):
                        mm(psg[g][:, :], mergedT[:, c, i * 128:(i + 1) * 128], wbuf[:, wo[hf], c, :], c == 0, c == 7,
                           ["big1", wk(wo[hf])], [gk(g)])
                    tt("dve", xpl[:, x, hf * 512:(hf + 1) * 512], xpl[:, x, hf * 512:(hf + 1) * 512], psg[g][:, :],
                       ALU.add, [xk(x), gk(g)], [xk(x)])
                    gpool.put(g)
                tn = state["tile_no"]
                state["tile_no"] += 1
                dma("q_sp", h2s[tn * 128:(tn + 1) * 128, :], xpl[:, x, :], [xk(x)], ["h2s"])
                if prev is not None:
                    route_tile(*prev)
                    xpool.put(prev[0])
                prev = (x, tn)
            route_tile(*prev)
            xpool.put(prev[0])
            for w in wo:
                wpool.put(w)

        def run_blocks(blocks, depth=2):
            pend = []
            wq = {}

            def ensure_w(n):
                if n < len(blocks) and n not in wq:
                    wq[n] = wload(*blocks[n]["wsrc"])
            if state.get("w_pre") is not None:
                wq[0] = state.pop("w_pre")
            ensure_w(0)
            ensure_w(1)
            for n, blk in enumerate(blocks):
                ensure_w(n + 2)
                w = wq[n]
                for (fa, fb) in blk["items"]:
                    ctx = fa(w)
                    pend.append((fb, ctx))
                    if len(pend) > depth:
                        fb_, ctx_ = pend.pop(0)
                        fb_(ctx_)
                wpool.put(w)
            while pend:
                fb_, ctx_ = pend.pop(0)
                fb_(ctx_)

        def project_supertile(ntile, seq_tile0, is_prompt, out_kv, cross_fn):
            T = ntile * 128
            blocks = []

            def A_tok(i, cols):
                def fa(w):
                    g = gpool.get()
                    proj_tok(xnT, "xnT", i * 128, w, cols, psg[g][:, 0:cols], gk(g))
                    return g
                return fa

            for gi in range(3):
                items = []
                for i in range(ntile):
                    def fb(g, gi=gi, i=i):
                        bq = bpool.get()
                        headnorm(psg[g][:, 0:256], gk(g), 4, 64, qna_rep, "qna_rep",
                                 [(bfp[:, bq, 0:256].rearrange("p (h d) -> p h d", h=4), bk(bq))])
                        gpool.put(g)
                        transposes_to(qAT[:, gi * 2:gi * 2 + 2, i * 128:(i + 1) * 128], bfp[:, bq, 0:256], 2,
                                      [bk(bq)], ["qAT"])
                        bpool.put(bq)
                    items.append((A_tok(i, 256), fb))
                blocks.append(dict(wsrc=(w_in_blk(C_QA + gi * 256, 256), 8, 256), items=items))
            for gi in range(3):
                items = []
                for i in range(ntile):
                    def fb(g, gi=gi, i=i):
                        bq = bpool.get()
                        f = fpool.get()
                        headnorm(psg[g][:, 0:256], gk(g), 4, 64, kna_rep, "kna_rep",
                                 [(f32p[:, f, 0:256].rearrange("p (h d) -> p h d", h=4), fk(f))])
                        gpool.put(g)
                        cp("act", bfp[:, bq, 0:256], f32p[:, f, 0:256], [fk(f)], [bk(bq)])
                        out_kv(0, gi, i, f32p[:, f, 0:256], fk(f))
                        fpool.put(f)
                        kt = seq_tile0 + i
                        transposes_to(kAT[:, gi * 2:gi * 2 + 2, kt * 128:(kt + 1) * 128], bfp[:, bq, 0:256], 2,
                                      [bk(bq)], ["kAT"])
                        bpool.put(bq)
                    items.append((A_tok(i, 256), fb))
                blocks.append(dict(wsrc=(w_in_blk(C_KA + gi * 256, 256), 8, 256), items=items))
            for gi in range(3):
                items = []
                for i in range(ntile):
                    def fb(g, gi=gi, i=i):
                        f = fpool.get()
                        cp("dve", f32p[:, f, 0:256], psg[g][:, 0:256], [gk(g)], [fk(f)])
                        cp("act", vA[:, seq_tile0 + i, gi * 256:(gi + 1) * 256], psg[g][:, 0:256], [gk(g)], ["vA"])
                        gpool.put(g)
                        out_kv(1, gi, i, f32p[:, f, 0:256], fk(f))
                        fpool.put(f)
                    items.append((A_tok(i, 256), fb))
                blocks.append(dict(wsrc=(w_in_blk(C_VA + gi * 256, 256), 8, 256), items=items))
            items = []
            for i in range(ntile):
                def fb(g, i=i):
                    cp("act", vb_bf[:, i, :], psg[g][:, :], [gk(g)], ["big1"])
                    gpool.put(g)
                items.append((A_tok(i, 512), fb))
            blocks.append(dict(wsrc=(w_in_blk(C_VB, 512), 8, 512), items=items))
            items = []
            for j in range(4):
                def fa(w, j=j):
                    g = gpool.get()
                    for k in range(8):
                        mm(psg[g][:, 0:T], wbuf[:, w, k, j * 128:(j + 1) * 128], xnT[:, k, 0:T], k == 0, k == 7,
                           [wk(w), "xnT"], [gk(g)])
                    return g

                def fb(g, j=j):
                    act(rbT[:, j, 0:T], psg[g][:, 0:T], AF.Silu, [gk(g)], ["big1"])
                    gpool.put(g)
                items.append((fa, fb))
            blocks.append(dict(wsrc=(w_in_blk(C_RB, 512), 8, 512), items=items))
            run_blocks(blocks)
            w = wload(w_in_blk(C_QC, 512), 8, 512)
            for i in range(ntile):
                g = gpool.get()
                proj_tok(xnT, "xnT", i * 128, w, 512, psg[g][:, :], gk(g))
                bq = bpool.get()
                headnorm(psg[g][:, :], gk(g), 4, 128, qnc_rep, "qnc_rep",
                         [(bfp[:, bq, 0:512].rearrange("p (h d) -> p h d", h=4), bk(bq))])
                gpool.put(g)
                bT = bpool.get()
                transposes_to(bfp[:, bT, 0:512].rearrange("p (k c) -> p k c", k=4), bfp[:, bq, 0:512], 4, [bk(bq)],
                              [bk(bT)])
                bpool.put(bq)
                cross_fn(i, bT)
                bpool.put(bT)
            wpool.put(w)
            state["w_qkb"] = wload(w_in_blk(C_QB, 512), 8, 512)

        def mem_kv_prompt(sq):
            mT = [bpool.get(), bpool.get()]
            x = xpool.get()
            dma("q_sp", xpl[:, x, :], mem_norm.partition_broadcast(128), (), [xk(x)])
            for t in range(2):
                front(memp[sq, t * 128:(t + 1) * 128, :], xpl[:, x, :], xk(x),
                      bfp[:, mT[t], :].rearrange("p (k c) -> p k c", k=8), bk(mT[t]))
            xpool.put(x)
            for part in range(2):
                w = wload((w_mem_kv[:, part * 512:(part + 1) * 512].rearrange("(k p) c -> p k c", p=128),
                           "nokey"), 8, 512)
                for t in range(2):
                    g = gpool.get()
                    for k in range(8):
                        mm(psg[g][:, :], bfp[:, mT[t], k * 128:(k + 1) * 128], wbuf[:, w, k, :], k == 0, k == 7,
                           [bk(mT[t]), wk(w)], [gk(g)])
                    f = fpool.get()
                    if part == 0:
                        headnorm(psg[g][:, :], gk(g), 4, 128, knc_rep, "knc_rep",
                                 [(f32p[:, f, :].rearrange("p (h d) -> p h d", h=4), fk(f))])
                        gpool.put(g)
                        bq = bpool.get()
                        cp("act", bfp[:, bq, 0:512], f32p[:, f, :], [fk(f)], [bk(bq)])
                        transposes_to(kcT[:, 0, :, t * 128:(t + 1) * 128], bfp[:, bq, 0:512], 4, [bk(bq)], ["kv0k"])
                        bpool.put(bq)
                    else:
                        cp("dve", f32p[:, f, :], psg[g][:, :], [gk(g)], [fk(f)])
                        cp("act", vc[:, 0, t, :], psg[g][:, :], [gk(g)], ["kv0v"])
                        gpool.put(g)
                    dma("q_sp", memkv[sq, t * 128:(t + 1) * 128, part, :, :],
                        f32p[:, f, :].rearrange("p (h d) -> p h d", h=4), [fk(f)], ["memkv"])
                    fpool.put(f)
                wpool.put(w)
            for m in mT:
                bpool.put(m)

        pending_copies = [(gi, b) for b in range(NSB) for gi in range(3)]

        def issue_copies(n):
            for _ in range(n):
                if not pending_copies:
                    return
                gi, b = pending_copies.pop(0)
                Wb = WIN[gi]
                dma("q_act", ws[gi][b, 0:Wb - LS], cw[gi][b, LS:Wb], (), ["wsc%d_%d" % (gi, b)])

        def prompt_seq(sq):
            mem_kv_prompt(sq)
            mset("pool", S32[:, 0, :, :], 0.0, ["S0"])
            mset("pool", Sbf[:, 0, :, :], 0.0, ["S0bf"])
            for st in range(DBG["nst"]):
                tile0 = st * 4
                pre = state.pop("pre", None)
                if pre is None:
                    pre = [front_a(xp[sq, (tile0 + i) * 128:(tile0 + i + 1) * 128, :], nmix_rep[:], "nmix_rep")
                           for i in range(4)]
                for r in state.pop("zero_rest", []):
                    dma("q_sp", xsc[r * 512:(r + 1) * 512, :], xsc[0:512, :], ["xsc_z0"], ["xsc_z%d" % r])
                for i in range(4):
                    front_b(pre[i], xnT[:, :, i * 128:(i + 1) * 128], "xnT")

                def prefetch(sq=sq, st=st):
                    state["more_tiles"] = (st + 1 < DBG["nst"]) or (sq + 1 < DBG["nseq"]) or DBG["sample"]
                    if st + 1 < DBG["nst"]:
                        nsq, nt0 = sq, (st + 1) * 4
                    elif sq + 1 < DBG["nseq"]:
                        nsq, nt0 = sq + 1, 0
                    else:
                        if DBG["sample"]:
                            state["pre"] = [front_a(xsm[:, :], nmix_rep[:], "nmix_rep")]
                        return
                    state["pre"] = [front_a(xp[nsq, (nt0 + i) * 128:(nt0 + i + 1) * 128, :], nmix_rep[:],
                                            "nmix_rep") for i in range(4)]

                def out_kv(kind, gi, i, f32view, key, tile0=tile0):
                    pos0 = (tile0 + i) * 128
                    lo = SEQ - WIN[gi]
                    if pos0 >= lo:
                        dma("q_sp", wp[gi][sq, pos0 - lo:pos0 - lo + 128, kind, :, :],
                            f32view.rearrange("p (h d) -> p h d", h=4), [key], ["wp%d" % gi])

                project_supertile(4, tile0, True, out_kv,
                                  (lambda i, bT: cross_tile(i * 128, bT, [(0, "kv0", 0, 128)])) if "cross" in DBG["mix"]
                                  else (lambda i, bT: None))
                issue_copies(6)

                def s_loader(bi, phase):
                    return 0, "S0"

                def s_store(bi, slot, skey):
                    cp("act", Sbf[:, 0, :, :], S32[:, 0, :, :], ["S0"], ["S0bf"])

                threads = []
                if "attn" in DBG["mix"]:
                    threads.append(chain([(lambda i=i: attn_prompt_tile(tile0 + i, i * 128)) for i in range(4)]))
                if "gla" in DBG["mix"]:
                    threads.append(chain([(lambda i=i: gla_tile(i, i * 128, tri_p[:], "tri_p", blk_p, "blk_p", 1,
                                                                s_loader, s_store)) for i in range(4)]))
                run_threads(threads)
                wpool.put(state["w_qkb"])
                if DBG["merge"]:
                    merge_supertile(4, lambda i, tile0=tile0: xp[sq, (tile0 + i) * 128:(tile0 + i + 1) * 128, :], 0,
                                    prefetch)
            dma("q_sp", glap[sq].rearrange("(pr hh) dk dv -> (hh dk) pr dv", hh=2), S32[:, 0, :, :], ["S0"], ["glap"])

        def sample_tile():
            issue_copies(1000)
            pre = state.pop("pre", None)
            if pre is None:
                pre = [front_a(xsm[:, :], nmix_rep[:], "nmix_rep")]
            front_b(pre[0], xnT[:, :, 0:128], "xnT")

            def out_kv(kind, gi, i, f32view, key):
                Wb = WIN[gi]
                for b in range(NSB):
                    dma("q_sp", ws[gi][b, Wb - LS:Wb, kind, :, :],
                        f32view[b * LS:(b + 1) * LS, :].rearrange("p (h d) -> p h d", h=4), [key],
                        ["wsn%d_%d_%d" % (gi, b, kind)])

            project_supertile(1, 0, False, out_kv, lambda i, bT: cross_sample(bT))
            mset("dve", psN[0:64, :], 0.0, ["psN"])
            mset("dve", psL[0:64, :], 0.0, ["psL"])
            blocks = []
            for h in range(4):
                hp, pr = (h % 2) * 64, h // 2
                for gi in range(3):
                    ch = gi * 2 + pr
                    blocks.append(dict(
                        mask=maskS[:, gi, :], mkey="maskS", kkey="kAT", qkey="qAT", vkey="vA", ncol=128,
                        sub=[(kAT[hp:hp + 64, ch, 0:128], qAT[hp:hp + 64, ch, 0:128], 0, 128)],
                        pv=[(vA[:, 0, (gi * 4 + h) * 64:(gi * 4 + h + 1) * 64], 0, 128,
                             psN[0:64, h * 128:(h + 1) * 128], psL[0:64, h * 128:(h + 1) * 128])]))
            attn_blocks(blocks, None)
            ld = 0
            for b in range(NSB):
                for gi in range(3):
                    nkt_all = WIN[gi] // 128
                    for k0 in range(0, nkt_all, 4):
                        nkt = min(4, nkt_all - k0)
                        slot = ld % 2
                        ld += 1
                        ckey = "cache%d" % slot
                        dma("q_pool", kcache[:, slot, 0:nkt, :],
                            cw[gi][b, k0 * 128:(k0 + nkt) * 128, 0].rearrange("(kt p) h d -> p kt (h d)", p=128),
                            (), [ckey + "k"])
                        dma("q_pool", vcache[:, slot, 0:nkt, :],
                            cw[gi][b, k0 * 128:(k0 + nkt) * 128, 1].rearrange("(kt p) h d -> p kt (h d)", p=128),
                            (), [ckey + "v"])
                        t = trpool.get()
                        for kt in range(nkt):
                            for pr in range(2):
                                tr(pstr[t][:, (kt * 2 + pr) * 128:(kt * 2 + pr + 1) * 128],
                                   kcache[:, slot, kt, pr * 128:(pr + 1) * 128], [ckey + "k"], [tk(t)])
                        cp("dve", kcTA[:, slot, 0:nkt, :, :],
                           pstr[t][:, 0:nkt * 256].rearrange("p (kt pr c) -> p kt pr c", kt=nkt, pr=2),
                           [tk(t)], [ckey + "kT"])
                        trpool.put(t)
                        blocks = []
                        for kt in range(nkt):
                            typ = 0 if (k0 + kt) == 0 else 1
                            sub, pv = [], []
                            for ci, h in enumerate((0, 2, 1, 3)):
                                hp, pr = (h % 2) * 64, h // 2
                                sub.append((kcTA[hp:hp + 64, slot, kt, pr, :],
                                            qAT[hp:hp + 64, gi * 2 + pr, b * LS:(b + 1) * LS], ci * 8, 8))
                                pv.append((vcache[:, slot, kt, h * 64:(h + 1) * 64], ci * 8, 8,
                                           psN[0:64, h * 128 + b * LS:h * 128 + (b + 1) * LS],
                                           psL[0:64, h * 128 + b * LS:h * 128 + (b + 1) * LS]))
                            blocks.append(dict(mask=maskC[:, 2 * gi + typ, :, :].rearrange("p h l -> p (h l)"),
                                               mkey="maskC", kkey=ckey + "kT", qkey="qAT", vkey=ckey + "v", ncol=32,
                                               sub=sub, pv=pv))
                        attn_blocks(blocks, None)
            attn_finish(0)
            def s_loader(bi, phase):
                slot = bi % 2
                key = "S%d" % slot
                src = sgla[bi].rearrange("(pr hh) dk dv -> (hh dk) pr dv", hh=2)
                if phase == "o":
                    dma("q_pool", Sbf[:, slot, :, :], src, (), [key + "bf"])
                else:
                    dma("q_sp", S32[:, slot, :, :], src, (), [key])
                return slot, key

            def s_store(bi, slot, skey):
                dma("q_sp", glas[bi].rearrange("(pr hh) dk dv -> (hh dk) pr dv", hh=2), S32[:, slot, :, :], [skey],
                    ["glas"])

            for _ in gla_tile(0, 0, tri_s[:], "tri_s", blk_s, "blk_s", NSB, s_loader, s_store):
                pass
            wpool.put(state["w_qkb"])
            merge_supertile(1, lambda i: xsm[:, :], 0)

        def cross_sample(qc_b):
            goc = gpool.get()
            gl = gpool.get()
            for b in range(NSB):
                slot = b % 2
                key = "kvs%d" % slot
                bk_ = bpool.get()
                dma("q_pool", bfp[:, bk_, :].rearrange("p (blk c) -> p blk c", blk=2),
                    cmem[b, :, 0].rearrange("(blk p) h d -> p blk (h d)", p=128), (), [bk(bk_)])
                dma("q_pool", vc[:, slot, :, :], cmem[b, :, 1].rearrange("(blk p) h d -> p blk (h d)", p=128), (),
                    [key + "v"])
                t = trpool.get()
                for blk_i in range(2):
                    for h in range(4):
                        tr(pstr[t][:, (h * 2 + blk_i) * 128:(h * 2 + blk_i + 1) * 128],
                           bfp[:, bk_, blk_i * 512 + h * 128:blk_i * 512 + (h + 1) * 128], [bk(bk_)], [tk(t)])
                cp("dve", kcT[:, slot, :, :], pstr[t][:, :].rearrange("p (h n) -> p h n", h=4), [tk(t)], [key + "k"])
                trpool.put(t)
                bpool.put(bk_)
                c0, cn = b * LS, LS
                g = gpool.get()
                for h in range(4):
                    for blk_i in range(2):
                        col = (h * 2 + blk_i) * 8
                        mm(psg[g][:, col:col + 8], kcT[:, slot, h, blk_i * 128:(blk_i + 1) * 128],
                           bfp[:, qc_b, h * 128 + c0:h * 128 + c0 + cn], True, True, [key + "k", bk(qc_b)], [gk(g)])
                pb = bpool.get()
                act(bfp[:, pb, 0:64], psg[g][:, 0:64], AF.Exp, [gk(g)], [bk(pb)])
                gpool.put(g)
                for h in range(4):
                    for blk_i in range(2):
                        col = (h * 2 + blk_i) * 8
                        mm(psg[goc][:, h * 128 + c0:h * 128 + c0 + cn], vc[:, slot, blk_i, h * 128:(h + 1) * 128],
                           bfp[:, pb, col:col + 8], blk_i == 0, blk_i == 1,
                           [key + "v", bk(pb)], [gk(goc)])
                        mm(psg[gl][:, h * 128 + c0:h * 128 + c0 + cn], ones_bf[:, :], bfp[:, pb, col:col + 8],
                           blk_i == 0, blk_i == 1, ["ones_bf", bk(pb)], [gk(gl)])
                bpool.put(pb)
            f = fpool.get()
            recip(f32p[:, f, :], psg[gl][:, :], [gk(gl)], [fk(f)])
            gpool.put(gl)
            tt("dve", ocT[:, :, 0:128], psg[goc][:, :].rearrange("p (h t) -> p h t", h=4),
               f32p[:, f, :].rearrange("p (h t) -> p h t", h=4), ALU.mult, [gk(goc), fk(f)], ["ocT"])
            gpool.put(goc)
            fpool.put(f)

        def experts_phase():
            XeT = kAT[:, 0:4, :].rearrange("p a (k c) -> p (a k) c", c=512)
            Wv = vA[:, :, :].rearrange("p a c -> p (a c)")

            def views(e_):
                sl = e_ % 2
                base = sl * 6144
                return (sl, Wv[:, base:base + 2048].rearrange("p (k f) -> p k f", k=8),
                        Wv[:, base + 2048:base + 4096].rearrange("p (k f) -> p k f", k=8),
                        Wv[:, base + 4096:base + 6144].rearrange("p (k c) -> p k c", k=2))

            def loads_w(e_):
                sl, wg_v, wu_v, wd_v = views(e_)
                wkey = "ew%d" % sl
                dma("q_pool", wg_v, w_eg[e_].rearrange("(k p) f -> p k f", p=128), (), [wkey + "g"])
                dma("q_pool", wu_v, w_eu[e_].rearrange("(k p) f -> p k f", p=128), (), [wkey + "u"])
                dma("q_pool", wd_v, w_ed[e_].rearrange("(k p) c -> p k c", p=128), (), [wkey + "d"])

            def loads_x(e_):
                xs_slots = []
                for sb_ in range(4):
                    bx = bpool.get()
                    dma("q_sp", bfp[:, bx, :], xsc[e_ * CAP + sb_ * 128:e_ * CAP + (sb_ + 1) * 128, :], ["xsc"],
                        [bk(bx)])
                    xs_slots.append(bx)
                return xs_slots

            def tstage(e_, xs_slots):
                sl, wg_v, wu_v, wd_v = views(e_)
                xkey = "xe%d" % sl
                for sb_, bx in enumerate(xs_slots):
                    t = trpool.get()
                    for k in range(8):
                        tr(pstr[t][:, k * 128:(k + 1) * 128], bfp[:, bx, k * 128:(k + 1) * 128], [bk(bx)], [tk(t)])
                    cp("dve" if sb_ % 2 == 0 else "act", XeT[:, sl * 8:(sl + 1) * 8, sb_ * 128:(sb_ + 1) * 128],
                       pstr[t][:, :].rearrange("p (k c) -> p k c", k=8), [tk(t)], [xkey])
                    trpool.put(t)
                    bpool.put(bx)

            def compute(e_):
                sl, wg_v, wu_v, wd_v = views(e_)
                wkey = "ew%d" % sl
                xkey = "xe%d" % sl
                hb = bpool.get()
                for fc in range(2):
                    gg = gpool.get()
                    gu = gpool.get()
                    for k in range(8):
                        mm(psg[gg][:, :], wg_v[:, k, fc * 128:(fc + 1) * 128], XeT[:, sl * 8 + k, :], k == 0, k == 7,
                           [wkey + "g", xkey], [gk(gg)])
                    for k in range(8):
                        mm(psg[gu][:, :], wu_v[:, k, fc * 128:(fc + 1) * 128], XeT[:, sl * 8 + k, :], k == 0, k == 7,
                           [wkey + "u", xkey], [gk(gu)])
                    f = fpool.get()
                    act(f32p[:, f, :], psg[gg][:, :], AF.Silu, [gk(gg)], [fk(f)])
                    gpool.put(gg)
                    tt("dve", bfp[:, hb, fc * 512:(fc + 1) * 512], psg[gu][:, :], f32p[:, f, :], ALU.mult,
                       [gk(gu), fk(f)], [bk(hb)])
                    gpool.put(gu)
                    fpool.put(f)
                stores = []
                for sb_ in range(4):
                    x = xpool.get()
                    for hf in range(2):
                        g = gpool.get()
                        for fc in range(2):
                            mm(psg[g][:, :], bfp[:, hb, fc * 512 + sb_ * 128:fc * 512 + (sb_ + 1) * 128],
                               wd_v[:, fc, hf * 512:(hf + 1) * 512], fc == 0, fc == 1, [bk(hb), wkey + "d"], [gk(g)])
                        if hf == 0:
                            cp("act", xpl[:, x, 0:512], psg[g][:, :], [gk(g)], [xk(x)])
                        else:
                            cp("dve", xpl[:, x, 512:1024], psg[g][:, :], [gk(g)], [xk(x)])
                        gpool.put(g)
                    dma("q_sp", ysc[e_ * CAP + sb_ * 128:e_ * CAP + (sb_ + 1) * 128, :], xpl[:, x, :], [xk(x)],
                        ["ysc"])
                    xpool.put(x)
                bpool.put(hb)

            loads_w(0)
            tstage(0, loads_x(0))
            nxt = loads_x(1)
            for e_ in range(NEXP):
                if e_ + 1 < NEXP:
                    loads_w(e_ + 1)
                    tstage(e_ + 1, nxt)
                if e_ + 2 < NEXP:
                    nxt = loads_x(e_ + 2)
                compute(e_)

        def combine_phase():
            npair = NF // 2
            ntl = state["tile_no"]

            def loads(tn):
                x = xpool.get()
                dma("q_sp", xpl[:, x, :], h2s[tn * 128:(tn + 1) * 128, :], ["h2s"], [xk(x)])
                ys = []
                for j in range(2):
                    pi = (tn * 2 + j) % npair
                    yv = f32p[:, 2 * pi:2 * pi + 2, :].rearrange("p a c -> p (a c)")
                    keys = [fk(2 * pi), fk(2 * pi + 1)]
                    S.op("q_pool", lambda e, tn=tn, j=j, yv=yv: e.indirect_dma_start(
                        out=yv, out_offset=None, in_=ysc[:, :],
                        in_offset=bass.IndirectOffsetOnAxis(ap=dsti[:, tn, j:j + 1], axis=0)),
                        ["ysc", "dsti"], keys)
                    ys.append((yv, keys))
                return x, ys

            def finish(tn, x, ys):
                for j, (yv, keys) in enumerate(ys):
                    stt(xpl[:, x, :], yv, wts[:, tn, j:j + 1], xpl[:, x, :], ALU.mult, ALU.add,
                        keys + ["wts", xk(x)], [xk(x)])
                if tn < NPS * 16:
                    sq, tt_ = tn // 16, tn % 16
                    dst = yp[sq, tt_ * 128:(tt_ + 1) * 128, :]
                else:
                    dst = ysm[:, :]
                dma("q_sp", dst, xpl[:, x, :], [xk(x)], ["yout%d" % tn])
                xpool.put(x)

            if ntl == 0:
                return
            nxt = loads(0)
            for tn in range(ntl):
                cur = nxt
                if tn + 1 < ntl:
                    nxt = loads(tn + 1)
                finish(tn, *cur)

        mset("pool", big1[:], 0.0, ["big1"])
        dma("q_act", xsc[0:512, :].rearrange("(p j) d -> p (j d)", j=4), big1[:].rearrange("p a c -> p (a c)"),
            ["big1"], ["xsc_z0"])
        nz = NEXP * CAP // 512
        for r in range(1, 8):
            dma("q_act", xsc[r * 512:(r + 1) * 512, :], xsc[0:512, :], ["xsc_z0"], ["xsc_z%d" % r])
        state["zero_rest"] = list(range(8, nz))
        for sq in range(DBG["nseq"]):
            prompt_seq(sq)
        if DBG["sample"]:
            S.barrier(lambda e: e.memset(eps_c[:], EPS))
            sample_tile()
        if DBG["moe"]:
            S.barrier(lambda e: e.memset(eps_c[:], EPS))
            experts_phase()
            S.barrier(lambda e: e.memset(eps_c[:], EPS))
            combine_phase()
        if DBG.get("dump"):
            DBG["_in_dump"] = True
            DBG["dump"](locals())
        S.emit(nc)
    return nc


_PROG = {}


def _get_prog():
    if "nc" not in _PROG:
        _PROG["nc"] = build_program()
    return _PROG["nc"]


def kernel(x_prompt, x_sample, mem_prompt, cache_win1, cache_win2, cache_win3, state_gla, cache_mem,
           norm_mix, w_in, qn_a, kn_a, qn_c, kn_c, gla_gate_up, gla_gate_bias, gla_norm, mem_norm,
           w_mem_kv, w_branch_a, w_branch_b, w_branch_c, w_out, norm_ffn, w_router_group, b_router_group,
           w_router_expert, b_router_expert, w_exp_gate, w_exp_up, w_exp_down):
    nc = _get_prog()
    f = lambda a: np.ascontiguousarray(np.asarray(a, dtype=np.float32))
    shared = {
        "norm_mix": f(norm_mix), "w_in": f(w_in), "qn_a": f(qn_a), "kn_a": f(kn_a), "qn_c": f(qn_c), "kn_c": f(kn_c),
        "gla_gate_up": f(gla_gate_up), "gla_gate_bias": f(gla_gate_bias), "gla_norm": f(gla_norm),
        "mem_norm": f(mem_norm), "w_mem_kv": f(w_mem_kv), "w_branch_a": f(w_branch_a), "w_branch_b": f(w_branch_b),
        "w_branch_c": f(w_branch_c), "w_out": f(w_out), "norm_ffn": f(norm_ffn), "w_router_group": f(w_router_group),
        "b_router_group": f(b_router_group), "w_router_expert": f(w_router_expert),
        "b_router_expert": f(b_router_expert), "w_exp_gate": f(w_exp_gate), "w_exp_up": f(w_exp_up),
        "w_exp_down": f(w_exp_down),
    }
    x_prompt, x_sample, mem_prompt = f(x_prompt), f(x_sample), f(mem_prompt)
    cache_win1, cache_win2, cache_win3 = f(cache_win1), f(cache_win2), f(cache_win3)
    state_gla, cache_mem = f(state_gla), f(cache_mem)
    in_maps = []
    for c in range(NCORES):
        m = dict(shared)
        m["xp"] = x_prompt[c * NPS:(c + 1) * NPS]
        m["xs"] = x_sample[c * NSB:(c + 1) * NSB].reshape(128, D)
        m["memp"] = mem_prompt[c * NPS:(c + 1) * NPS]
        m["cw1"] = cache_win1[c * NSB:(c + 1) * NSB]
        m["cw2"] = cache_win2[c * NSB:(c + 1) * NSB]
        m["cw3"] = cache_win3[c * NSB:(c + 1) * NSB]
        m["sgla"] = state_gla[c * NSB:(c + 1) * NSB]
        m["cmem"] = cache_mem[c * NSB:(c + 1) * NSB]
        in_maps.append(m)
    ncr = DBG["ncores"]
    res = run_bass_kernel_spmd(nc, in_maps[:ncr], core_ids=list(range(ncr)))
    R = res.results
    cat = lambda k: np.concatenate([np.asarray(r[k]) for r in R], axis=0)
    y_prompt = cat("yp")
    y_sample = cat("ys").reshape(NCORES * NSB, LS, D)
    return (y_prompt, y_sample, cat("w1p"), cat("w2p"), cat("w3p"), cat("glap"), cat("memkv"),
            cat("w1s"), cat("w2s"), cat("w3s"), cat("glas"))
```

```python
import contextlib
import numpy as np
import concourse.bass as bass
import concourse.mybir as mybir
from concourse.bass_utils import run_bass_kernel_spmd

F32 = mybir.dt.float32
BF16 = mybir.dt.bfloat16
I32 = mybir.dt.int32
AF = mybir.ActivationFunctionType
ALU = mybir.AluOpType
AX = mybir.AxisListType

NCORES = 8
D = 1024
SEQ = 2048
NPS = 2
NSB = 16
LS = 8
NTOK = NPS * SEQ + NSB * LS
NTILE = NTOK // 128
CAP = 512
NEXP = 32
EPS = 1e-6
NEG = -30000.0
WIN = (128, 512, 2048)
DIL = (1, 4, 16)
C_QA, C_KA, C_VA, C_QB, C_KB, C_VB, C_RB, C_AB, C_QC, C_GL = 0, 768, 1536, 2304, 2560, 2816, 3328, 3840, 3856, 4368

COMPUTE = ("pe", "act", "dve", "pool")
QUEUES = ("q_sp", "q_act", "q_pool")
Q_HOST = {"q_sp": "sp", "q_act": "act", "q_pool": "pool"}
RING = {"q_sp": 16, "q_act": 8, "q_pool": 12}


class Op:
    __slots__ = ("eng", "fn", "deps", "idx", "signal", "is_dma", "dma_no", "stream", "where")


class Sched:
    def __init__(self):
        self.ops = []
        self.last_w = {}
        self.readers = {}
        self.streams = {"pe": [], "act": [], "dve": [], "pool": [], "sp": []}
        self.dma_count = {q: 0 for q in QUEUES}
        self.barrier_dep = None

    def op(self, eng, fn, reads=(), writes=()):
        mx = DBG.get("maxops")
        if mx is not None and len(self.ops) >= mx and not DBG.get("_in_dump"):
            return None
        is_dma = eng in QUEUES
        psr = [k for k in reads if k.startswith("ps")]
        if psr:
            reads = [k for k in reads if not k.startswith("ps")]
            writes = list(writes) + [k for k in psr if k not in writes]
        deps = set()
        if self.barrier_dep is not None:
            deps.add(self.barrier_dep)
        for k in reads:
            w = self.last_w.get(k)
            if w is not None:
                deps.add(w)
        for k in writes:
            w = self.last_w.get(k)
            if w is not None:
                deps.add(w)
            for r in self.readers.get(k, ()):
                deps.add(r)
        o = Op()
        o.eng, o.fn, o.deps, o.is_dma, o.signal, o.dma_no = eng, fn, deps, is_dma, False, None
        o.idx = len(self.ops)
        o.where = None
        if DBG.get("trace"):
            import sys as _sys
            fr = _sys._getframe(1)
            w = []
            while fr is not None and len(w) < 4:
                w.append("%s:%d" % (fr.f_code.co_name, fr.f_lineno))
                fr = fr.f_back
            o.where = " < ".join(w)
        self.ops.append(o)
        o.stream = Q_HOST[eng] if is_dma else eng
        if is_dma:
            o.dma_no = self.dma_count[eng]
            self.dma_count[eng] += 1
        self.streams[o.stream].append(o)
        for k in writes:
            self.last_w[k] = o.idx
            self.readers[k] = []
        for k in reads:
            if k not in writes:
                self.readers.setdefault(k, []).append(o.idx)
        return o

    def barrier(self, nop_fn):
        deps = set()
        for st in self.streams.values():
            if st:
                deps.add(st[-1].idx)
        for q in QUEUES:
            n = self.dma_count[q]
            cnt = 0
            for o in reversed(self.ops):
                if o.is_dma and o.eng == q:
                    deps.add(o.idx)
                    cnt += 1
                    if cnt >= RING[q]:
                        break
        o = self.op("dve", nop_fn)
        if o is None:
            return
        o.deps |= deps
        o.deps.discard(o.idx)
        self.barrier_dep = o.idx
        self.last_w = {}
        self.readers = {}

    def emit(self, nc):
        ops = self.ops
        for o in ops:
            for d in o.deps:
                p = ops[d]
                if not p.is_dma:
                    p.signal = True
        sig_count = {}
        cnt = {e: 0 for e in COMPUTE}
        for o in ops:
            if not o.is_dma:
                if o.signal:
                    cnt[o.eng] += 1
                sig_count[o.idx] = cnt[o.eng]
        with contextlib.ExitStack() as es:
            sems = {e: es.enter_context(nc.semaphore("s_" + e)) for e in COMPUTE}
            rings = {q: [es.enter_context(nc.semaphore("r_%s_%d" % (q, i))) for i in range(RING[q])]
                     for q in QUEUES}
            block = es.enter_context(nc.Block())
            handles = {"pe": "tensor", "act": "scalar", "dve": "vector", "pool": "gpsimd", "sp": "sync"}

            def run_stream(stream, e):
                waited = {}
                for o in self.streams[stream]:
                    need = {}
                    for d in o.deps:
                        p = ops[d]
                        if p.is_dma:
                            R = RING[p.eng]
                            key = (p.eng, p.dma_no % R)
                            val = 16 * (p.dma_no // R + 1)
                        else:
                            if p.eng == "pe" and stream == "pe":
                                continue
                            key = (p.eng, -1)
                            val = sig_count[p.idx]
                        if need.get(key, 0) < val:
                            need[key] = val
                    if o.is_dma:
                        R = RING[o.eng]
                        if o.dma_no >= R:
                            key = (o.eng, o.dma_no % R)
                            val = 16 * (o.dma_no // R)
                            if need.get(key, 0) < val:
                                need[key] = val
                    for key, val in need.items():
                        if waited.get(key, 0) >= val:
                            continue
                        waited[key] = val
                        sem = sems[key[0]] if key[1] < 0 else rings[key[0]][key[1]]
                        e.wait_ge(sem, val)
                    ins = o.fn(e)
                    if o.is_dma:
                        ins.then_inc(rings[o.eng][o.dma_no % RING[o.eng]], 16)
                    elif o.signal:
                        ins.then_inc(sems[o.eng], 1)
                for q in QUEUES:
                    if Q_HOST[q] != stream:
                        continue
                    n = self.dma_count[q]
                    R = RING[q]
                    for r in range(R):
                        k = (n - r + R - 1) // R
                        if k > 0 and waited.get((q, r), 0) < 16 * k:
                            e.wait_ge(rings[q][r], 16 * k)

            for stream, attr in handles.items():
                if not self.streams[stream]:
                    continue

                def body(e, stream=stream):
                    run_stream(stream, e)
                getattr(block, attr)(body)


class SlotPool:
    def __init__(self, n, key):
        self.free = list(range(n))
        self.key = key

    def get(self):
        assert self.free, "pool %s exhausted" % self.key
        return self.free.pop(0)

    def put(self, i):
        self.free.append(i)


DBG = {"nseq": NPS, "nst": 4, "sample": True, "moe": True, "mix": ("attn", "gla", "cross"), "merge": True,
       "ncores": NCORES}


def build_program():
    nc = bass.Bass("TRN2", target_bir_lowering=False)
    S = Sched()

    def din(name, shape, dt=F32):
        return nc.dram_tensor(name, list(shape), dt, kind="ExternalInput").ap()

    def dout(name, shape, dt=F32):
        return nc.dram_tensor(name, list(shape), dt, kind="ExternalOutput").ap()

    def dint(name, shape, dt=F32):
        return nc.dram_tensor(name, list(shape), dt, kind="Internal").ap()

    xp = din("xp", [NPS, SEQ, D])
    xsm = din("xs", [128, D])
    memp = din("memp", [NPS, 256, D])
    cw = [din("cw1", [NSB, 128, 2, 4, 64]), din("cw2", [NSB, 512, 2, 4, 64]), din("cw3", [NSB, 2048, 2, 4, 64])]
    sgla = din("sgla", [NSB, 4, 64, 128])
    cmem = din("cmem", [NSB, 256, 2, 4, 128])
    norm_mix = din("norm_mix", [D])
    w_in = din("w_in", [D, 7440])
    qn_a = din("qn_a", [64])
    kn_a = din("kn_a", [64])
    qn_c = din("qn_c", [128])
    kn_c = din("kn_c", [128])
    gup = din("gla_gate_up", [16, 256])
    gbias = din("gla_gate_bias", [256])
    gla_norm = din("gla_norm", [128])
    mem_norm = din("mem_norm", [D])
    w_mem_kv = din("w_mem_kv", [D, 1024])
    w_br = [din("w_branch_a", [256, D]), din("w_branch_b", [512, D]), din("w_branch_c", [512, D])]
    w_out = din("w_out", [D, D])
    norm_ffn = din("norm_ffn", [D])
    w_rg = din("w_router_group", [D, 4])
    b_rg = din("b_router_group", [4])
    w_re = din("w_router_expert", [D, 32])
    b_re = din("b_router_expert", [32])
    w_eg = din("w_exp_gate", [NEXP, D, 256])
    w_eu = din("w_exp_up", [NEXP, D, 256])
    w_ed = din("w_exp_down", [NEXP, 256, D])

    yp = dout("yp", [NPS, SEQ, D])
    ysm = dout("ys", [128, D])
    wp = [dout("w1p", [NPS, 128, 2, 4, 64]), dout("w2p", [NPS, 512, 2, 4, 64]), dout("w3p", [NPS, 2048, 2, 4, 64])]
    glap = dout("glap", [NPS, 4, 64, 128])
    memkv = dout("memkv", [NPS, 256, 2, 4, 128])
    ws = [dout("w1s", [NSB, 128, 2, 4, 64]), dout("w2s", [NSB, 512, 2, 4, 64]), dout("w3s", [NSB, 2048, 2, 4, 64])]
    glas = dout("glas", [NSB, 4, 64, 128])

    h2s = dint("h2s", [NTOK, D])
    wbf_in = dint("wbf_in", [D, 7440], BF16)
    wbf_out = dint("wbf_out", [D, D], BF16)
    wbf_br = [dint("wbf_br%d" % i, [r, D], BF16) for i, r in enumerate((256, 512, 512))]
    wbf_mem = dint("wbf_mem", [D, 1024], BF16)
    xsc = dint("xsc", [NEXP * CAP, D], BF16)
    ysc = dint("ysc", [NEXP * CAP, D])

    es = contextlib.ExitStack()
    with es:
        def sb(name, shape, dt):
            return es.enter_context(nc.sbuf_tensor(name, list(shape), dt))

        def pst(name, shape, dt):
            return es.enter_context(nc.psum_tensor(name, list(shape), dt))

        NG = 4
        psg = [pst("psg%d" % i, [128, 512], F32) for i in range(NG)]
        pstr = [pst("pstr%d" % i, [128, 1024], BF16) for i in range(2)]
        psN = pst("psN", [128, 512], F32)
        psL = pst("psL", [128, 512], F32)
        gpool = SlotPool(NG, "psg")
        trpool = SlotPool(2, "pstr")

        NF = 12
        NB = 10
        NX = 3
        NW = 3
        WCOLS = 512
        f32p = sb("f32p", [128, NF, 512], F32)
        bfp = sb("bfp", [128, NB, 1024], BF16)
        xpl = sb("xpl", [128, NX, 1024], F32)
        wbuf = sb("wbuf", [128, NW, 8, WCOLS], BF16)
        smallp = sb("smallp", [128, 96, 8], F32)
        fpool, bpool, xpool, wpool, spool = (SlotPool(NF, "f"), SlotPool(NB, "b"), SlotPool(NX, "x"),
                                             SlotPool(NW, "w"), SlotPool(96, "s"))
        xnT = sb("xnT", [128, 8, 512], BF16)
        kAT = sb("kAT", [128, 6, SEQ], BF16)
        vA = sb("vA", [128, 16, 768], BF16)
        qAT = sb("qAT", [128, 6, 512], BF16)
        big1 = sb("big1", [128, 8, 512], BF16)
        vb_bf = big1[:, 0:4, :]
        rbT = big1[:, 4:8, :]
        mergedT = big1[:, :, :]
        oaT = sb("oaT", [64, 4, 512], BF16)
        ob2T = sb("ob2T", [128, 4, 512], BF16)
        ocT = sb("ocT", [128, 4, 512], BF16)
        wbr_sb = [sb("wbr_a", [64, 2, 4, 128], BF16), sb("wbr_b", [128, 2, 4, 128], BF16),
                  sb("wbr_c", [128, 2, 4, 128], BF16)]
        wab = sb("wab", [128, 8, 16], BF16)
        S32 = sb("S32", [128, 2, 2, 128], F32)
        Sbf = sb("Sbf", [128, 2, 2, 128], BF16)
        kcT = sb("kcT", [128, 2, 4, 256], BF16)
        vc = sb("vc", [128, 2, 2, 512], BF16)
        kcache = kAT[:, 0:2, 128:1152].rearrange("p s (kt c) -> p s kt c", kt=4)
        vcache = kAT[:, 2:4, 128:1152].rearrange("p s (kt c) -> p s kt c", kt=4)
        kcTA = kAT[:, 4:6, 128:1152].rearrange("p s (kt pr c) -> p s kt pr c", kt=4, pr=2)
        wts = sb("wts", [128, NTILE, 2], F32)
        dsti = sb("dsti", [128, NTILE, 2], I32)
        rbase = sb("rbase", [128, 32], F32)
        ident = sb("ident", [128, 128], BF16)
        cf32 = sb("cf32", [128, 128], F32)
        ones_bf = sb("ones_bf", [128, 128], BF16)
        zeros_bf = sb("zeros_bf", [128, 128], BF16)
        stri_bf = sb("stri_bf", [128, 128], BF16)
        onesdiv = sb("onesdiv", [128, 128], F32)
        tri_p = sb("tri_p", [128, 128], F32)
        tri_s = sb("tri_s", [128, 128], F32)
        blk_p = sb("blk_p", [128, 16], F32)
        blk_s = sb("blk_s", [128, 16], F32)
        E_r = {4: sb("E4", [4, 32, 4], F32), 16: sb("E16", [16, 8, 16], F32)}
        masks = sb("masks", [128, 9, 128], BF16)
        maskS = sb("maskS", [128, 3, 128], BF16)
        maskC = sb("maskC", [128, 6, 4, 8], BF16)
        nmix_rep = sb("nmix_rep", [128, D], F32)
        nffn_rep = sb("nffn_rep", [128, D], F32)
        qna_rep = sb("qna_rep", [128, 64], F32)
        kna_rep = sb("kna_rep", [128, 64], F32)
        qnc_rep = sb("qnc_rep", [128, 128], F32)
        knc_rep = sb("knc_rep", [128, 128], F32)
        gbias_rep = sb("gbias_rep", [128, 256], F32)
        rbias_rep = sb("rbias_rep", [128, 36], F32)
        ecap = sb("ecap", [128, 32], F32)
        gnorm_c = sb("gnorm_c", [128, 1], F32)
        eps_c = sb("eps_c", [128, 1], F32)
        gup_bf = sb("gup_bf", [16, 256], BF16)
        wr_bf = sb("wr_bf", [128, 8, 36], BF16)

        def fk(i): return "f%d" % i
        def bk(i): return "b%d" % i
        def xk(i): return "x%d" % i
        def wk(i): return "w%d" % i
        def sk(i): return "s%d" % i
        def gk(i): return "psg%d" % i
        def tk(i): return "pstr%d" % i

        def mm(out, lhsT, rhs, start, stop, reads, writes):
            S.op("pe", lambda e: e.matmul(out, lhsT=lhsT, rhs=rhs, start=start, stop=stop, skip_group_check=True),
                 reads, writes)

        def tr(out, in_, reads, writes):
            S.op("pe", lambda e: e.transpose(out=out, in_=in_, identity=ident[:]), list(reads) + ["ident"], writes)

        def act(out, in_, func, reads, writes, bias=None, scale=None, accum_out=None):
            kw = {}
            if bias is not None:
                kw["bias"] = bias
            if scale is not None:
                kw["scale"] = scale
            if accum_out is not None:
                kw["accum_out"] = accum_out
            S.op("act", lambda e: e.activation(out=out, in_=in_, func=func, **kw), reads, writes)

        def tt(eng, out, in0, in1, op, reads, writes):
            S.op(eng, lambda e: e.tensor_tensor(out=out, in0=in0, in1=in1, op=op), reads, writes)

        def ts(out, in0, s1, s2, op0, op1, reads, writes):
            if s2 is None:
                S.op("dve", lambda e: e.tensor_scalar(out=out, in0=in0, scalar1=s1, scalar2=None, op0=op0),
                     reads, writes)
            else:
                S.op("dve", lambda e: e.tensor_scalar(out=out, in0=in0, scalar1=s1, scalar2=s2, op0=op0, op1=op1),
                     reads, writes)

        def stt(out, in0, scalar, in1, op0, op1, reads, writes):
            S.op("dve", lambda e: e.scalar_tensor_tensor(out=out, in0=in0, scalar=scalar, in1=in1, op0=op0, op1=op1),
                 reads, writes)

        def cp(eng, out, in_, reads, writes):
            if eng == "act":
                S.op("act", lambda e: e.activation(out=out, in_=in_, func=AF.Copy), reads, writes)
            else:
                S.op(eng, lambda e: e.tensor_copy(out=out, in_=in_), reads, writes)

        def recip(out, in_, reads, writes):
            S.op("dve", lambda e: e.reciprocal(out=out, in_=in_), reads, writes)

        def red(out, in_, op, reads, writes):
            S.op("dve", lambda e: e.tensor_reduce(out=out, in_=in_, axis=AX.X, op=op), reads, writes)

        def mset(eng, ap, val, writes):
            S.op(eng, lambda e: e.memset(ap, val), (), writes)

        def dma(q, out, in_, reads, writes):
            S.op(q, lambda e: e.dma_start(out=out, in_=in_), reads, writes)

        def asel(out, in_, pattern, cmp_op, fill, base, cm, reads, writes):
            S.op("pool", lambda e: e.affine_select(out=out, in_=in_, pattern=pattern, compare_op=cmp_op, fill=fill,
                                                   base=base, channel_multiplier=cm), reads, writes)

        mset("pool", eps_c[:], EPS, ["eps_c"])
        mset("pool", cf32[:], 1.0, ["cf32"])
        asel(cf32[:], cf32[:], [[-1, 128]], ALU.is_equal, 0.0, 0, 1, ["cf32"], ["cf32"])
        cp("dve", ident[:], cf32[:], ["cf32"], ["ident"])
        mset("pool", ones_bf[:], 1.0, ["ones_bf"])
        mset("pool", zeros_bf[:], 0.0, ["zeros_bf"])
        mset("pool", onesdiv[:], 1.0 / 128, ["onesdiv"])
        mset("pool", tri_p[:], 1.0, ["tri_p"])
        asel(tri_p[:], tri_p[:], [[1, 128]], ALU.is_ge, 0.0, 0, -1, ["tri_p"], ["tri_p"])
        mset("pool", cf32[:], 1.0, ["cf32"])
        asel(cf32[:], cf32[:], [[1, 128]], ALU.is_gt, 0.0, 0, -1, ["cf32"], ["cf32"])
        cp("dve", stri_bf[:], cf32[:], ["cf32"], ["stri_bf"])
        cp("pool", tri_s[:], tri_p[:], ["tri_p"], ["tri_s"])
        asel(tri_s[:].rearrange("p (b l) -> p b l", l=8), tri_s[:].rearrange("p (b l) -> p b l", l=8),
             [[-8, 16], [0, 8]], ALU.is_ge, 0.0, 0, 1, ["tri_s"], ["tri_s"])
        mset("pool", blk_p[:], 0.0, ["blk_p"])
        mset("pool", blk_p[:, 0:1], 1.0, ["blk_p"])
        mset("pool", blk_s[:], 1.0, ["blk_s"])
        asel(blk_s[:], blk_s[:], [[-8, 16]], ALU.is_ge, 0.0, 0, 1, ["blk_s"], ["blk_s"])
        asel(blk_s[:], blk_s[:], [[8, 16]], ALU.is_ge, 0.0, 7, -1, ["blk_s"], ["blk_s"])
        ecap_i = sb("ecap_i", [128, 32], I32)
        S.op("pool", lambda e: e.iota(ecap_i[:], pattern=[[CAP, 32]], base=0, channel_multiplier=0), (), ["ecap_i"])
        cp("dve", ecap[:], ecap_i[:], ["ecap_i"], ["ecap"])
        mset("pool", rbase[:], 0.0, ["rbase"])
        dma("q_sp", nmix_rep[:], norm_mix.partition_broadcast(128), (), ["nmix_rep"])
        dma("q_sp", nffn_rep[:], norm_ffn.partition_broadcast(128), (), ["nffn_rep"])
        dma("q_sp", qna_rep[:], qn_a.partition_broadcast(128), (), ["qna_rep"])
        dma("q_sp", kna_rep[:], kn_a.partition_broadcast(128), (), ["kna_rep"])
        dma("q_sp", qnc_rep[:], qn_c.partition_broadcast(128), (), ["qnc_rep"])
        dma("q_sp", knc_rep[:], kn_c.partition_broadcast(128), (), ["knc_rep"])
        dma("q_sp", gbias_rep[:], gbias.partition_broadcast(128), (), ["gbias_rep"])
        dma("q_sp", rbias_rep[:, 0:4], b_rg.partition_broadcast(128), (), ["rbias_rep"])
        dma("q_sp", rbias_rep[:, 4:36], b_re.partition_broadcast(128), ["rbias_rep"], ["rbias_rep"])
        dma("q_sp", gnorm_c[:], gla_norm.rearrange("(p o) -> p o", o=1), (), ["gnorm_c"])
        ts(qna_rep[:], qna_rep[:], 0.125, None, ALU.mult, None, ["qna_rep"], ["qna_rep"])
        ts(qnc_rep[:], qnc_rep[:], float(128 ** -0.5), None, ALU.mult, None, ["qnc_rep"], ["qnc_rep"])
        dma("q_pool", gup_bf[:], gup, (), ["gup_bf"])
        dma("q_pool", wr_bf[:, :, 0:4], w_rg.rearrange("(k p) c -> p k c", p=128), (), ["wr_bf"])
        dma("q_pool", wr_bf[:, :, 4:36], w_re.rearrange("(k p) c -> p k c", p=128), ["wr_bf"], ["wr_bf"])
        dma("q_pool", wab[:], w_in[:, C_AB:C_AB + 16].rearrange("(k p) c -> p k c", p=128), (), ["wab"])
        for gi, r in enumerate(DIL):
            g = gpool.get()
            if r == 1:
                mset("dve", psg[g][:, 0:128], 1.0, [gk(g)])
            else:
                er = E_r[r]
                mset("pool", er[:], 1.0, ["E%d" % r])
                asel(er[:], er[:], [[0, 128 // r], [1, r]], ALU.is_equal, 0.0, 0, -1, ["E%d" % r], ["E%d" % r])
                er2 = er[:].rearrange("p a b -> p (a b)")
                mm(psg[g][:, 0:128], er2, er2, True, True, ["E%d" % r], [gk(g)])
            f = fpool.get()
            ts(f32p[:, f, 0:128], psg[g][:, 0:128], -1.0, -NEG, ALU.add, ALU.mult, [gk(g)], [fk(f)])
            gpool.put(g)
            cp("dve", masks[:, 3 * gi + 1, :], f32p[:, f, 0:128], [fk(f)], ["masks"])
            f2 = fpool.get()
            asel(f32p[:, f2, 0:128], f32p[:, f, 0:128], [[1, 128]], ALU.is_ge, NEG, 0, -1, [fk(f)], [fk(f2)])
            cp("dve", masks[:, 3 * gi + 0, :], f32p[:, f2, 0:128], [fk(f2)], ["masks"])
            asel(f32p[:, f2, 0:128], f32p[:, f, 0:128], [[-1, 128]], ALU.is_ge, NEG, 0, 1, [fk(f)], [fk(f2)])
            cp("dve", masks[:, 3 * gi + 2, :], f32p[:, f2, 0:128], [fk(f2)], ["masks"])
            asel(f32p[:, f2, 0:128], f32p[:, f, 0:128], [[1, 128]], ALU.is_ge, NEG, 0, -1, [fk(f)], [fk(f2)])
            v3 = f32p[:, f2, 0:128].rearrange("p (b l) -> p b l", l=8)
            asel(v3, v3, [[-8, 16], [0, 8]], ALU.is_ge, NEG, 0, 1, [fk(f2)], [fk(f2)])
            cp("dve", maskS[:, gi, :], f32p[:, f2, 0:128], [fk(f2)], ["maskS"])
            fpool.put(f)
            fpool.put(f2)
            for hh in range(4):
                cp("pool", maskC[:, 2 * gi + 0, hh, :], masks[:, 3 * gi + 2, 0:8], ["masks"], ["maskC"])
                cp("pool", maskC[:, 2 * gi + 1, hh, :], masks[:, 3 * gi + 1, 0:8], ["masks"], ["maskC"])

        def rms_stats(src_ap, n, reads, tag):
            s = spool.get()
            jb = bpool.get()
            mset("pool", smallp[:, s, 0:1], 0.0, [sk(s)])
            act(bfp[:, jb, 0:n], src_ap, AF.Square, list(reads) + [sk(s)], [bk(jb), sk(s)],
                accum_out=smallp[:, s, 0:1])
            bpool.put(jb)
            act(smallp[:, s, 1:2], smallp[:, s, 0:1], AF.Ln, [sk(s), "eps_c"], [sk(s)], bias=eps_c[:], scale=1.0 / n)
            act(smallp[:, s, 0:1], smallp[:, s, 1:2], AF.Exp, [sk(s)], [sk(s)], scale=-0.5)
            return s

        def transposes_to(dst3, src2, nblk, reads, writes):
            t = trpool.get()
            for j in range(nblk):
                tr(pstr[t][:, j * 128:(j + 1) * 128], src2[:, j * 128:(j + 1) * 128], reads, [tk(t)])
            cp("dve", dst3, pstr[t][:, 0:nblk * 128].rearrange("p (k c) -> p k c", k=nblk), [tk(t)], writes)
            trpool.put(t)

        def front_a(x_src, grep, grep_key):
            x = xpool.get()
            dma("q_sp", xpl[:, x, :], x_src, (), [xk(x)])
            s = rms_stats(xpl[:, x, :], D, [xk(x)], "fr")
            b = bpool.get()
            stt(bfp[:, b, :], xpl[:, x, :], smallp[:, s, 0:1], grep, ALU.mult, ALU.mult,
                [xk(x), sk(s), grep_key], [bk(b)])
            spool.put(s)
            xpool.put(x)
            return b

        def front_b(b, dstT, dst_key):
            transposes_to(dstT, bfp[:, b, :], 8, [bk(b)], [dst_key])
            bpool.put(b)

        def front(x_src, grep, grep_key, dstT, dst_key, keep_x=False):
            front_b(front_a(x_src, grep, grep_key), dstT, dst_key)

        def wload(src3, kk, cols):
            src3, skey = src3
            w = wpool.get()
            dma("q_pool", wbuf[:, w, 0:kk, 0:cols], src3, [skey], [wk(w)])
            return w

        conv_blocks = ([(C_QA + i * 256, 256) for i in range(9)] + [(C_VB, 512), (C_RB, 512), (C_QC, 512), (C_QB, 512)]
                       + [(C_GL + i * 512, 512) for i in range(6)])
        for (c0, cols) in conv_blocks:
            dma("q_pool", wbf_in[:, c0:c0 + cols], w_in[:, c0:c0 + cols], (), ["wbfin%d" % c0])
        for b_ in range(3):
            dma("q_pool", wbf_br[b_][:, :], w_br[b_][:, :], (), ["wbfbr%d" % b_])
        for hf in range(2):
            dma("q_pool", wbf_out[:, hf * 512:(hf + 1) * 512], w_out[:, hf * 512:(hf + 1) * 512], (),
                ["wbfout%d" % hf])

        def w_in_blk(c0, cols):
            return (wbf_in[:, c0:c0 + cols].rearrange("(k p) c -> p k c", p=128), "wbfin%d" % c0)

        def proj_tok(actT, act_key, t0, w, cols, ps_ap, ps_key, wc0=0):
            for k in range(8):
                mm(ps_ap, actT[:, k, t0:t0 + 128], wbuf[:, w, k, wc0:wc0 + cols], k == 0, k == 7,
                   [act_key, wk(w)], [ps_key])

        def headnorm(ps_ap, ps_key, nh, hd, grep, grep_key, outs):
            f = fpool.get()
            s = spool.get()
            act(f32p[:, f, 0:nh * hd], ps_ap, AF.Square, [ps_key], [fk(f)])
            red(smallp[:, s, 0:nh], f32p[:, f, 0:nh * hd].rearrange("p (h d) -> p h d", h=nh), ALU.add,
                [fk(f)], [sk(s)])
            fpool.put(f)
            s2 = spool.get()
            act(smallp[:, s2, 0:nh], smallp[:, s, 0:nh], AF.Ln, [sk(s), "eps_c"], [sk(s2)], bias=eps_c[:],
                scale=1.0 / hd)
            act(smallp[:, s, 0:nh], smallp[:, s2, 0:nh], AF.Exp, [sk(s2)], [sk(s)], scale=-0.5)
            spool.put(s2)
            for (dst3, dkey) in outs:
                for h in range(nh):
                    stt(dst3[:, h, :], ps_ap[:, h * hd:(h + 1) * hd], smallp[:, s, h:h + 1], grep[:, 0:hd],
                        ALU.mult, ALU.mult, [ps_key, sk(s), grep_key], [dkey])
            spool.put(s)

        state = {"tile_no": 0}

        def attn_blocks(blocks, hsel=None):
            for _ in attn_blocks_gen(blocks):
                pass

        def attn_blocks_gen(blocks):
            groups = []
            i = 0
            while i < len(blocks):
                grp = []
                tot = 0
                while i < len(blocks) and tot + blocks[i]["ncol"] <= 512:
                    grp.append((blocks[i], tot))
                    tot += blocks[i]["ncol"]
                    i += 1
                groups.append((grp, tot))

            def pv_stage(grp, pb):
                for (b, c0) in grp:
                    for (v, cc, cn, oN, oL) in b["pv"]:
                        mm(oN, v, bfp[:, pb, c0 + cc:c0 + cc + cn], False, False, [b["vkey"], bk(pb)], ["psN"])
                        mm(oL, ones_bf[:, 0:64], bfp[:, pb, c0 + cc:c0 + cc + cn], False, False,
                           ["ones_bf", bk(pb)], ["psL"])
                bpool.put(pb)

            prev = None
            for (grp, tot) in groups:
                g = gpool.get()
                for (b, c0) in grp:
                    n = b["ncol"]
                    if len(b["sub"]) == 1:
                        mm(psg[g][:, c0:c0 + n], ident[:], b["mask"], True, False, ["ident", b["mkey"]], [gk(g)])
                        (kT, q, cc, cn) = b["sub"][0]
                        mm(psg[g][:, c0 + cc:c0 + cc + cn], kT, q, False, True, [b["kkey"], b["qkey"]], [gk(g)])
                    else:
                        hn = n // 2
                        first = True
                        for half in range(2):
                            mm(psg[g][:, c0 + half * hn:c0 + (half + 1) * hn], ident[:],
                               b["mask"][:, half * hn:(half + 1) * hn], first, False, ["ident", b["mkey"]], [gk(g)])
                            first = False
                            for (kT, q, cc, cn) in b["sub"][half * 2:half * 2 + 2]:
                                mm(psg[g][:, c0 + cc:c0 + cc + cn], kT, q, False, True, [b["kkey"], b["qkey"]],
                                   [gk(g)])
                pb = bpool.get()
                act(bfp[:, pb, 0:tot], psg[g][:, 0:tot], AF.Exp, [gk(g)], [bk(pb)])
                gpool.put(g)
                if prev is not None:
                    pv_stage(*prev)
                prev = (grp, pb)
                yield
            if prev is not None:
                pv_stage(*prev)
            yield

        def attn_finish(t0):
            f = fpool.get()
            recip(f32p[0:64, f, :], psL[0:64, :], ["psL"], [fk(f)])
            tt("dve", oaT[:, :, t0:t0 + 128], psN[0:64, :].rearrange("p (h t) -> p h t", h=4),
               f32p[0:64, f, :].rearrange("p (h t) -> p h t", h=4), ALU.mult, ["psN", fk(f)], ["oaT"])
            fpool.put(f)

        def attn_prompt_tile(qt, t0):
            mset("dve", psN[0:64, :], 0.0, ["psN"])
            mset("dve", psL[0:64, :], 0.0, ["psL"])
            blocks = []
            for h in range(4):
                hp, pr = (h % 2) * 64, h // 2
                for gi in range(3):
                    nd = WIN[gi] // 128
                    ch = gi * 2 + pr
                    for kt in range(max(0, qt - nd), qt + 1):
                        dlt = qt - kt
                        typ = 0 if dlt == 0 else (2 if dlt == nd else 1)
                        blocks.append(dict(
                            mask=masks[:, 3 * gi + typ, :], mkey="masks", kkey="kAT", qkey="qAT", vkey="vA", ncol=128,
                            sub=[(kAT[hp:hp + 64, ch, kt * 128:(kt + 1) * 128], qAT[hp:hp + 64, ch, t0:t0 + 128], 0, 128)],
                            pv=[(vA[:, kt, (gi * 4 + h) * 64:(gi * 4 + h + 1) * 64], 0, 128,
                                 psN[0:64, h * 128:(h + 1) * 128], psL[0:64, h * 128:(h + 1) * 128])]))
            yield from attn_blocks_gen(blocks)
            attn_finish(t0)
            yield

        def run_threads(gens):
            gens = list(gens)
            while gens:
                for gtor in list(gens):
                    try:
                        next(gtor)
                    except StopIteration:
                        gens.remove(gtor)

        def chain(makers):
            for mk in makers:
                yield from mk()

        def gla_tile(ti, t0, tri, tri_key, blk, blk_key, nblk, s_loader, s_store):
            bw = 128 // nblk
            g = gpool.get()
            for k in range(8):
                mm(psg[g][:, 0:16], xnT[:, k, t0:t0 + 128], wab[:, k, :], k == 0, k == 7, ["xnT", "wab"], [gk(g)])
            b1 = bpool.get()
            cp("act", bfp[:, b1, 0:16], psg[g][:, 0:16], [gk(g)], [bk(b1)])
            gpool.put(g)
            yield
            t = trpool.get()
            tr(pstr[t][0:16, 0:128], bfp[:, b1, 0:16], [bk(b1)], [tk(t)])
            cp("dve", bfp[0:16, b1, 128:256], pstr[t][0:16, 0:128], [tk(t)], [bk(b1)])
            trpool.put(t)
            yield
            g = gpool.get()
            mm(psg[g][:, 0:256], bfp[0:16, b1, 128:256], gup_bf[:], True, True, [bk(b1), "gup_bf"], [gk(g)])
            bpool.put(b1)
            fz = fpool.get()
            tt("dve", f32p[:, fz, 0:256], psg[g][:, 0:256], gbias_rep[:], ALU.add, [gk(g), "gbias_rep"], [fk(fz)])
            gpool.put(g)
            act(f32p[:, fz, 256:512], f32p[:, fz, 0:256], AF.Exp, [fk(fz)], [fk(fz)], scale=-1.0)
            act(f32p[:, fz, 0:256], f32p[:, fz, 256:512], AF.Ln, [fk(fz)], [fk(fz)], bias=1.0)
            yield
            g = gpool.get()
            mm(psg[g][:, 0:256], tri, f32p[:, fz, 0:256], True, True, [tri_key, fk(fz)], [gk(g)])
            for pr in range(2):
                mm(psg[g][:, 256 + pr * 16:256 + pr * 16 + nblk], f32p[:, fz, pr * 128:(pr + 1) * 128], blk[:, 0:nblk],
                   True, True, [fk(fz), blk_key], [gk(g)])
            fe = fpool.get()
            act(f32p[:, fe, 0:256], psg[g][:, 0:256], AF.Exp, [gk(g)], [fk(fe)], scale=-1.0 / 16)
            act(f32p[:, fe, 256:512], psg[g][:, 0:256], AF.Exp, [gk(g)], [fk(fe)], scale=1.0 / 16)
            ebl = f32p[:, fz, 256:256 + 32]
            act(ebl, psg[g][:, 256:288], AF.Exp, [gk(g), fk(fz)], [fk(fz)], scale=-1.0 / 16)
            gpool.put(g)
            yield
            w = state["w_qkb"]
            g = gpool.get()
            proj_tok(xnT, "xnT", t0, w, 512, psg[g][:, :], gk(g))
            bq = bpool.get()
            stt(bfp[:, bq, 0:256], psg[g][:, 0:256], 0.125, f32p[:, fe, 0:256], ALU.mult, ALU.mult,
                [gk(g), fk(fe)], [bk(bq)])
            tt("dve", bfp[:, bq, 256:512], psg[g][:, 256:512], f32p[:, fe, 256:512], ALU.mult, [gk(g), fk(fe)],
               [bk(bq)])
            gpool.put(g)
            fpool.put(fe)
            yield
            bT = bpool.get()
            transposes_to(bfp[:, bT, 0:512].rearrange("p (k c) -> p k c", k=4), bfp[:, bq, 0:512], 4, [bk(bq)],
                          [bk(bT)])
            DBG.setdefault("gla_slots", dict(bq=bq, bT=bT, fz=fz))
            yield
            gAB = [gpool.get(), gpool.get()]
            for h in (0, 2, 1, 3):
                hp, pr = (h % 2) * 64, h // 2
                g = gAB[h % 2]
                mm(psg[g][:, pr * 128:(pr + 1) * 128], bfp[hp:hp + 64, bT, 256 + pr * 128:256 + (pr + 1) * 128],
                   bfp[hp:hp + 64, bT, pr * 128:(pr + 1) * 128], True, True, [bk(bT)], [gk(g)])
            ba = bpool.get()
            for h in range(4):
                g = gAB[h % 2]
                pr = h // 2
                tt("dve", bfp[:, ba, h * 128:(h + 1) * 128], psg[g][:, pr * 128:(pr + 1) * 128], tri, ALU.mult,
                   [gk(g), tri_key], [bk(ba)])
            gpool.put(gAB[0])
            gpool.put(gAB[1])
            yield
            go = gpool.get()
            if nblk == 1:
                slot, skey = s_loader(0, "o")
                for h in range(4):
                    hp, pr = (h % 2) * 64, h // 2
                    mm(psg[go][:, h * 128:(h + 1) * 128], vb_bf[:, ti, h * 128:(h + 1) * 128],
                       bfp[:, ba, h * 128:(h + 1) * 128], True, False, ["big1", bk(ba)], [gk(go)])
                    mm(psg[go][:, h * 128:(h + 1) * 128], Sbf[hp:hp + 64, slot, pr, :],
                       bfp[hp:hp + 64, bT, pr * 128:(pr + 1) * 128], False, True, [skey + "bf", bk(bT)], [gk(go)])
            else:
                mset("dve", psg[go][:, :], 0.0, [gk(go)])
                for h in range(4):
                    mm(psg[go][:, h * 128:(h + 1) * 128], vb_bf[:, ti, h * 128:(h + 1) * 128],
                       bfp[:, ba, h * 128:(h + 1) * 128], False, False, ["big1", bk(ba)], [gk(go)])
                for bi in range(nblk):
                    slot, skey = s_loader(bi, "o")
                    for h in (0, 2, 1, 3):
                        hp, pr = (h % 2) * 64, h // 2
                        if h in (0, 1):
                            mm(psg[go][:, 0:8], zeros_bf[:, :], ones_bf[:, 0:8], False, False,
                               ["zeros_bf", "ones_bf"], [gk(go)])
                        mm(psg[go][:, h * 128 + bi * bw:h * 128 + (bi + 1) * bw], Sbf[hp:hp + 64, slot, pr, :],
                           bfp[hp:hp + 64, bT, pr * 128 + bi * bw:pr * 128 + (bi + 1) * bw], False, False,
                           [skey + "bf", bk(bT)], [gk(go)])
            bpool.put(ba)
            yield
            for bi in range(nblk):
                slot, skey = s_loader(bi, "u")
                if nblk == 1:
                    kesrc, kkey, kb_ = bfp[:, bq, 256:512], bk(bq), None
                else:
                    kb_ = bpool.get()
                    ts(bfp[:, kb_, 0:256], bfp[:, bq, 256:512], blk[:, bi:bi + 1], None, ALU.mult, None,
                       [bk(bq), blk_key], [bk(kb_)])
                    kesrc, kkey = bfp[:, kb_, 0:256], bk(kb_)
                g = gpool.get()
                for h in range(4):
                    pr = h // 2
                    mm(psg[g][:, h * 128:(h + 1) * 128], kesrc[:, pr * 128:(pr + 1) * 128],
                       vb_bf[:, ti, h * 128:(h + 1) * 128], True, True, [kkey, "big1"], [gk(g)])
                if kb_ is not None:
                    bpool.put(kb_)
                for h in range(4):
                    hp, pr = (h % 2) * 64, h // 2
                    tt("dve", S32[hp:hp + 64, slot, pr, :], psg[g][hp:hp + 64, h * 128:(h + 1) * 128],
                       S32[hp:hp + 64, slot, pr, :], ALU.add, [gk(g), skey], [skey])
                    ts(S32[hp:hp + 64, slot, pr, :], S32[hp:hp + 64, slot, pr, :],
                       f32p[hp:hp + 64, fz, 256 + pr * 16 + bi:256 + pr * 16 + bi + 1], None, ALU.mult, None,
                       [skey, fk(fz)], [skey])
                gpool.put(g)
                s_store(bi, slot, skey)
            bpool.put(bq)
            bpool.put(bT)
            fpool.put(fz)
            yield
            fo = fpool.get()
            fq = fpool.get()
            cp("dve", f32p[:, fo, :], psg[go][:, :], [gk(go)], [fk(fo)])
            act(f32p[:, fq, :], psg[go][:, :], AF.Square, [gk(go)], [fk(fq)])
            gpool.put(go)
            yield
            gm = gpool.get()
            gs = gpool.get()
            mm(psg[gm][:, :], onesdiv[:], f32p[:, fo, :], True, True, ["onesdiv", fk(fo)], [gk(gm)])
            mm(psg[gs][:, :], onesdiv[:], f32p[:, fq, :], True, True, ["onesdiv", fk(fq)], [gk(gs)])
            act(f32p[:, fq, :], psg[gm][:, :], AF.Square, [gk(gm), fk(fq)], [fk(fq)])
            tt("dve", f32p[:, fq, :], psg[gs][:, :], f32p[:, fq, :], ALU.subtract, [gk(gs), fk(fq)], [fk(fq)])
            gpool.put(gs)
            ts(f32p[:, fq, :], f32p[:, fq, :], 0.0, None, ALU.max, None, [fk(fq)], [fk(fq)])
            act(f32p[:, fq, :], f32p[:, fq, :], AF.Ln, [fk(fq), "eps_c"], [fk(fq)], bias=eps_c[:])
            act(f32p[:, fq, :], f32p[:, fq, :], AF.Exp, [fk(fq)], [fk(fq)], scale=-0.5)
            tt("dve", f32p[:, fo, :], f32p[:, fo, :], psg[gm][:, :], ALU.subtract, [fk(fo), gk(gm)], [fk(fo)])
            gpool.put(gm)
            tt("dve", f32p[:, fo, :], f32p[:, fo, :], f32p[:, fq, :], ALU.mult, [fk(fo), fk(fq)], [fk(fo)])
            fpool.put(fq)
            stt(ob2T[:, :, t0:t0 + 128], f32p[:, fo, :].rearrange("p (h t) -> p h t", h=4), gnorm_c[:, 0:1],
                rbT[:, :, t0:t0 + 128], ALU.mult, ALU.mult, [fk(fo), "gnorm_c", "big1"], ["ob2T"])
            fpool.put(fo)
            yield

        def cross_tile(t0, qc_b, groups):
            goc = gpool.get()
            gl = gpool.get()
            for pair in range(2):
                g = gpool.get()
                for hh in range(2):
                    h = pair * 2 + hh
                    for blk_i in range(2):
                        for (slot, key, c0, cn) in groups:
                            col = (hh * 2 + blk_i) * 128 + c0
                            mm(psg[g][:, col:col + cn], kcT[:, slot, h, blk_i * 128:(blk_i + 1) * 128],
                               bfp[:, qc_b, h * 128 + c0:h * 128 + c0 + cn], True, True, [key + "k", bk(qc_b)],
                               [gk(g)])
                pb = bpool.get()
                act(bfp[:, pb, 0:512], psg[g][:, :], AF.Exp, [gk(g)], [bk(pb)])
                gpool.put(g)
                for hh in range(2):
                    h = pair * 2 + hh
                    for (slot, key, c0, cn) in groups:
                        for blk_i in range(2):
                            col = (hh * 2 + blk_i) * 128 + c0
                            mm(psg[goc][:, h * 128 + c0:h * 128 + c0 + cn],
                               vc[:, slot, blk_i, h * 128:(h + 1) * 128], bfp[:, pb, col:col + cn],
                               blk_i == 0, blk_i == 1, [key + "v", bk(pb)], [gk(goc)])
                        for blk_i in range(2):
                            col = (hh * 2 + blk_i) * 128 + c0
                            mm(psg[gl][:, h * 128 + c0:h * 128 + c0 + cn], ones_bf[:, :], bfp[:, pb, col:col + cn],
                               blk_i == 0, blk_i == 1, ["ones_bf", bk(pb)], [gk(gl)])
                bpool.put(pb)
            f = fpool.get()
            recip(f32p[:, f, :], psg[gl][:, :], [gk(gl)], [fk(f)])
            gpool.put(gl)
            tt("dve", ocT[:, :, t0:t0 + 128], psg[goc][:, :].rearrange("p (h t) -> p h t", h=4),
               f32p[:, f, :].rearrange("p (h t) -> p h t", h=4), ALU.mult, [gk(goc), fk(f)], ["ocT"])
            gpool.put(goc)
            fpool.put(f)

        def route_tile(x, tile_no):
            s = rms_stats(xpl[:, x, :], D, [xk(x)], "ffn")
            bx = bpool.get()
            stt(bfp[:, bx, :], xpl[:, x, :], smallp[:, s, 0:1], nffn_rep[:], ALU.mult, ALU.mult,
                [xk(x), sk(s), "nffn_rep"], [bk(bx)])
            spool.put(s)
            bT = bpool.get()
            transposes_to(bfp[:, bT, :].rearrange("p (k c) -> p k c", k=8), bfp[:, bx, :], 8, [bk(bx)], [bk(bT)])
            g = gpool.get()
            for k in range(8):
                mm(psg[g][:, 0:36], bfp[:, bT, k * 128:(k + 1) * 128], wr_bf[:, k, :], k == 0, k == 7,
                   [bk(bT), "wr_bf"], [gk(g)])
            bpool.put(bT)
            f = fpool.get()
            F_ = f32p[:, f, :]
            tt("dve", F_[:, 0:36], psg[g][:, 0:36], rbias_rep[:], ALU.add, [gk(g), "rbias_rep"], [fk(f)])
            gpool.put(g)
            s = spool.get()
            sm = smallp[:, s, :]
            red(sm[:, 0:1], F_[:, 0:4], ALU.max, [fk(f)], [sk(s)])
            ts(F_[:, 40:44], F_[:, 0:4], sm[:, 0:1], None, ALU.is_ge, None, [fk(f), sk(s)], [fk(f)])
            ts(sm[:, 1:2], sm[:, 0:1], -1.0, None, ALU.mult, None, [sk(s)], [sk(s)])
            mset("pool", sm[:, 2:3], 0.0, [sk(s)])
            act(F_[:, 44:48], F_[:, 0:4], AF.Exp, [fk(f), sk(s)], [fk(f), sk(s)], bias=sm[:, 1:2], accum_out=sm[:, 2:3])
            recip(sm[:, 3:4], sm[:, 2:3], [sk(s)], [sk(s)])
            ts(F_[:, 44:48], F_[:, 40:44], -NEG, NEG, ALU.mult, ALU.add, [fk(f)], [fk(f)])
            tt("dve", F_[:, 64:96].rearrange("p (g e) -> p g e", g=4),
               F_[:, 4:36].rearrange("p (g e) -> p g e", g=4),
               F_[:, 44:48].unsqueeze(2).to_broadcast([128, 4, 8]), ALU.add, [fk(f)], [fk(f)])
            S.op("dve", lambda e: e.max(out=F_[:, 96:104], in_=F_[:, 64:96]), [fk(f)], [fk(f)])
            ts(F_[:, 128:160], F_[:, 64:96], F_[:, 96:97], None, ALU.is_ge, None, [fk(f)], [fk(f)])
            ts(F_[:, 160:192], F_[:, 64:96], F_[:, 97:98], None, ALU.is_ge, None, [fk(f)], [fk(f)])
            tt("dve", F_[:, 160:192], F_[:, 160:192], F_[:, 128:160], ALU.subtract, [fk(f)], [fk(f)])
            ts(sm[:, 4:5], F_[:, 96:97], -1.0, None, ALU.mult, None, [fk(f)], [sk(s)])
            act(sm[:, 5:6], F_[:, 97:98], AF.Exp, [fk(f), sk(s)], [sk(s)], bias=sm[:, 4:5])
            ts(sm[:, 6:7], sm[:, 5:6], 1.0, None, ALU.add, None, [sk(s)], [sk(s)])
            recip(sm[:, 6:7], sm[:, 6:7], [sk(s)], [sk(s)])
            tt("dve", wts[:, tile_no, 0:1], sm[:, 6:7], sm[:, 3:4], ALU.mult, [sk(s)], ["wts"])
            tt("dve", wts[:, tile_no, 1:2], wts[:, tile_no, 0:1], sm[:, 5:6], ALU.mult, [sk(s), "wts"], ["wts"])
            ba = bpool.get()
            cp("dve", bfp[:, ba, 0:64], F_[:, 128:192], [fk(f)], [bk(ba)])
            g = gpool.get()
            mm(psg[g][:, 0:64], stri_bf[:], bfp[:, ba, 0:64], True, True, ["stri_bf", bk(ba)], [gk(g)])
            mm(psg[g][:, 64:128], ones_bf[:], bfp[:, ba, 0:64], True, True, ["ones_bf", bk(ba)], [gk(g)])
            bpool.put(ba)
            tt("dve", F_[:, 256:288], rbase[:], ecap[:], ALU.add, ["rbase", "ecap"], [fk(f)])
            tt("dve", F_[:, 288:320], F_[:, 256:288], psg[g][:, 64:96], ALU.add, [fk(f), gk(g)], [fk(f)])
            tt("dve", F_[:, 256:288], F_[:, 256:288], psg[g][:, 0:32], ALU.add, [fk(f), gk(g)], [fk(f)])
            tt("dve", F_[:, 288:320], F_[:, 288:320], psg[g][:, 32:64], ALU.add, [fk(f), gk(g)], [fk(f)])
            tt("dve", F_[:, 256:288], F_[:, 256:288], F_[:, 128:160], ALU.mult, [fk(f)], [fk(f)])
            tt("dve", F_[:, 288:320], F_[:, 288:320], F_[:, 160:192], ALU.mult, [fk(f)], [fk(f)])
            red(sm[:, 0:2], F_[:, 256:320].rearrange("p (a e) -> p a e", a=2), ALU.add, [fk(f)], [sk(s)])
            tt("dve", rbase[:], rbase[:], psg[g][:, 64:96], ALU.add, ["rbase", gk(g)], ["rbase"])
            tt("dve", rbase[:], rbase[:], psg[g][:, 96:128], ALU.add, ["rbase", gk(g)], ["rbase"])
            gpool.put(g)
            cp("dve", dsti[:, tile_no, :], sm[:, 0:2], [sk(s)], ["dsti"])
            spool.put(s)
            fpool.put(f)
            for j in range(2):
                S.op("q_pool", lambda e, j=j: e.indirect_dma_start(
                    out=xsc[:, :], out_offset=bass.IndirectOffsetOnAxis(ap=dsti[:, tile_no, j:j + 1], axis=0),
                    in_=bfp[:, bx, :], in_offset=None),
                    [bk(bx), "dsti"] + ["xsc_z%d" % r for r in range(NEXP * CAP // 512)], ["xsc"])
            bpool.put(bx)

        def merge_supertile(ntile, x_src_of, tok0, prefetch=None):
            T = ntile * 128
            if prefetch is not None:
                prefetch()
            srcs = [(oaT, "oaT", 64, 4), (ob2T, "ob2T", 128, 4), (ocT, "ocT", 128, 4)]
            order = [("g", half, b) for half in range(2) for b in range(3)] + [("o", hf, 0) for hf in range(2)]
            wq = {}

            def ensure(n):
                if n < len(order) and n not in wq:
                    kind, p0, p1 = order[n]
                    if kind == "g":
                        wq[n] = wload(w_in_blk(C_GL + p1 * 1024 + p0 * 512, 512), 8, 512)
                    else:
                        wq[n] = wload((wbf_out[:, p0 * 512:(p0 + 1) * 512].rearrange("(k p) c -> p k c", p=128),
                                       "wbfout%d" % p0), 8, 512)
            ensure(0)
            acc = None
            for n in range(6):
                ensure(n + 1)
                _, half, b = order[n]
                w = wq[n]
                if b == 0:
                    acc = [fpool.get() for _ in range(4)]
                src, skey, np_, nj = srcs[b]
                for cc in range(4):
                    c = half * 4 + cc
                    par = cc % 2
                    wkey = "wbr%d_%d" % (b, par)
                    dma("q_pool", wbr_sb[b][:, par, :, :],
                        wbf_br[b][:, c * 128:(c + 1) * 128].rearrange("(j p) c -> p j c", p=np_), ["wbfbr%d" % b],
                        [wkey])
                    g = gpool.get()
                    for k in range(8):
                        mm(psg[g][:, 0:T], wbuf[:, w, k, cc * 128:(cc + 1) * 128], xnT[:, k, 0:T],
                           k == 0, k == 7, [wk(w), "xnT"], [gk(g)])
                    sgb = bpool.get()
                    act(bfp[:, sgb, 0:T], psg[g][:, 0:T], AF.Sigmoid, [gk(g)], [bk(sgb)])
                    gpool.put(g)
                    g = gpool.get()
                    for j in range(nj):
                        mm(psg[g][:, 0:T], wbr_sb[b][0:np_, par, j, :], src[0:np_, j, 0:T], j == 0, j == nj - 1,
                           [wkey, skey], [gk(g)])
                    if b == 0:
                        tt("dve", f32p[:, acc[cc], 0:T], psg[g][:, 0:T], bfp[:, sgb, 0:T], ALU.mult,
                           [gk(g), bk(sgb)], [fk(acc[cc])])
                    else:
                        ft = fpool.get()
                        tt("dve", f32p[:, ft, 0:T], psg[g][:, 0:T], bfp[:, sgb, 0:T], ALU.mult, [gk(g), bk(sgb)],
                           [fk(ft)])
                        if b == 1:
                            tt("dve", f32p[:, acc[cc], 0:T], f32p[:, acc[cc], 0:T], f32p[:, ft, 0:T], ALU.add,
                               [fk(acc[cc]), fk(ft)], [fk(acc[cc])])
                        else:
                            tt("dve", mergedT[:, c, 0:T], f32p[:, acc[cc], 0:T], f32p[:, ft, 0:T], ALU.add,
                               [fk(acc[cc]), fk(ft)], ["big1"])
                        fpool.put(ft)
                    gpool.put(g)
                    bpool.put(sgb)
                wpool.put(w)
                if b == 2:
                    for a_ in acc:
                        fpool.put(a_)
            ensure(7)
            wo = [wq[6], wq[7]]
            if prefetch is not None and state.get("more_tiles"):
                state["w_pre"] = wload(w_in_blk(C_QA, 256), 8, 256)
            prev = None
            for i in range(ntile):
                x = xpool.get()
                dma("q_sp", xpl[:, x, :], x_src_of(i), (), [xk(x)])
                for hf in range(2):
                    g = gpool.get()
                    for c in range(8):
                        mm(psg[g][:, :], mergedT[:, c, i * 128:(i + 1) * 128], wbuf[:, wo[hf], c, :], c == 0, c == 7,
                           ["big1", wk(wo[hf])], [gk(g)])
                    tt("dve", xpl[:, x, hf * 512:(hf + 1) * 512], xpl[:, x, hf * 512:(hf + 1) * 512], psg[g][:, :],
                       ALU.add, [xk(x), gk(g)], [xk(x)])
                    gpool.put(g)
                tn = state["tile_no"]
                state["tile_no"] += 1
                dma("q_sp", h2s[tn * 128:(tn + 1) * 128, :], xpl[:, x, :], [xk(x)], ["h2s"])
                if prev is not None:
                    route_tile(*prev)
                    xpool.put(prev[0])
                prev = (x, tn)
            route_tile(*prev)
            xpool.put(prev[0])
            for w in wo:
                wpool.put(w)

        def run_blocks(blocks, depth=2):
            pend = []
            wq = {}

            def ensure_w(n):
                if n < len(blocks) and n not in wq:
                    wq[n] = wload(*blocks[n]["wsrc"])
            if state.get("w_pre") is not None:
                wq[0] = state.pop("w_pre")
            ensure_w(0)
            ensure_w(1)
            for n, blk in enumerate(blocks):
                ensure_w(n + 2)
                w = wq[n]
                for (fa, fb) in blk["items"]:
                    ctx = fa(w)
                    pend.append((fb, ctx))
                    if len(pend) > depth:
                        fb_, ctx_ = pend.pop(0)
                        fb_(ctx_)
                wpool.put(w)
            while pend:
                fb_, ctx_ = pend.pop(0)
                fb_(ctx_)

        def project_supertile(ntile, seq_tile0, is_prompt, out_kv, cross_fn):
            T = ntile * 128
            blocks = []

            def A_tok(i, cols):
                def fa(w):
                    g = gpool.get()
                    proj_tok(xnT, "xnT", i * 128, w, cols, psg[g][:, 0:cols], gk(g))
                    return g
                return fa

            for gi in range(3):
                items = []
                for i in range(ntile):
                    def fb(g, gi=gi, i=i):
                        bq = bpool.get()
                        headnorm(psg[g][:, 0:256], gk(g), 4, 64, qna_rep, "qna_rep",
                                 [(bfp[:, bq, 0:256].rearrange("p (h d) -> p h d", h=4), bk(bq))])
                        gpool.put(g)
                        transposes_to(qAT[:, gi * 2:gi * 2 + 2, i * 128:(i + 1) * 128], bfp[:, bq, 0:256], 2,
                                      [bk(bq)], ["qAT"])
                        bpool.put(bq)
                    items.append((A_tok(i, 256), fb))
                blocks.append(dict(wsrc=(w_in_blk(C_QA + gi * 256, 256), 8, 256), items=items))
            for gi in range(3):
                items = []
                for i in range(ntile):
                    def fb(g, gi=gi, i=i):
                        bq = bpool.get()
                        f = fpool.get()
                        headnorm(psg[g][:, 0:256], gk(g), 4, 64, kna_rep, "kna_rep",
                                 [(f32p[:, f, 0:256].rearrange("p (h d) -> p h d", h=4), fk(f))])
                        gpool.put(g)
                        cp("act", bfp[:, bq, 0:256], f32p[:, f, 0:256], [fk(f)], [bk(bq)])
                        out_kv(0, gi, i, f32p[:, f, 0:256], fk(f))
                        fpool.put(f)
                        kt = seq_tile0 + i
                        transposes_to(kAT[:, gi * 2:gi * 2 + 2, kt * 128:(kt + 1) * 128], bfp[:, bq, 0:256], 2,
                                      [bk(bq)], ["kAT"])
                        bpool.put(bq)
                    items.append((A_tok(i, 256), fb))
                blocks.append(dict(wsrc=(w_in_blk(C_KA + gi * 256, 256), 8, 256), items=items))
            for gi in range(3):
                items = []
                for i in range(ntile):
                    def fb(g, gi=gi, i=i):
                        f = fpool.get()
                        cp("dve", f32p[:, f, 0:256], psg[g][:, 0:256], [gk(g)], [fk(f)])
                        cp("act", vA[:, seq_tile0 + i, gi * 256:(gi + 1) * 256], psg[g][:, 0:256], [gk(g)], ["vA"])
                        gpool.put(g)
                        out_kv(1, gi, i, f32p[:, f, 0:256], fk(f))
                        fpool.put(f)
                    items.append((A_tok(i, 256), fb))
                blocks.append(dict(wsrc=(w_in_blk(C_VA + gi * 256, 256), 8, 256), items=items))
            items = []
            for i in range(ntile):
                def fb(g, i=i):
                    cp("act", vb_bf[:, i, :], psg[g][:, :], [gk(g)], ["big1"])
                    gpool.put(g)
                items.append((A_tok(i, 512), fb))
            blocks.append(dict(wsrc=(w_in_blk(C_VB, 512), 8, 512), items=items))
            items = []
            for j in range(4):
                def fa(w, j=j):
                    g = gpool.get()
                    for k in range(8):
                        mm(psg[g][:, 0:T], wbuf[:, w, k, j * 128:(j + 1) * 128], xnT[:, k, 0:T], k == 0, k == 7,
                           [wk(w), "xnT"], [gk(g)])
                    return g

                def fb(g, j=j):
                    act(rbT[:, j, 0:T], psg[g][:, 0:T], AF.Silu, [gk(g)], ["big1"])
                    gpool.put(g)
                items.append((fa, fb))
            blocks.append(dict(wsrc=(w_in_blk(C_RB, 512), 8, 512), items=items))
            run_blocks(blocks)
            w = wload(w_in_blk(C_QC, 512), 8, 512)
            for i in range(ntile):
                g = gpool.get()
                proj_tok(xnT, "xnT", i * 128, w, 512, psg[g][:, :], gk(g))
                bq = bpool.get()
                headnorm(psg[g][:, :], gk(g), 4, 128, qnc_rep, "qnc_rep",
                         [(bfp[:, bq, 0:512].rearrange("p (h d) -> p h d", h=4), bk(bq))])
                gpool.put(g)
                bT = bpool.get()
                transposes_to(bfp[:, bT, 0:512].rearrange("p (k c) -> p k c", k=4), bfp[:, bq, 0:512], 4, [bk(bq)],
                              [bk(bT)])
                bpool.put(bq)
                cross_fn(i, bT)
                bpool.put(bT)
            wpool.put(w)
            state["w_qkb"] = wload(w_in_blk(C_QB, 512), 8, 512)

        def mem_kv_prompt(sq):
            mT = [bpool.get(), bpool.get()]
            x = xpool.get()
            dma("q_sp", xpl[:, x, :], mem_norm.partition_broadcast(128), (), [xk(x)])
            for t in range(2):
                front(memp[sq, t * 128:(t + 1) * 128, :], xpl[:, x, :], xk(x),
                      bfp[:, mT[t], :].rearrange("p (k c) -> p k c", k=8), bk(mT[t]))
            xpool.put(x)
            for part in range(2):
                w = wload((w_mem_kv[:, part * 512:(part + 1) * 512].rearrange("(k p) c -> p k c", p=128),
                           "nokey"), 8, 512)
                for t in range(2):
                    g = gpool.get()
                    for k in range(8):
                        mm(psg[g][:, :], bfp[:, mT[t], k * 128:(k + 1) * 128], wbuf[:, w, k, :], k == 0, k == 7,
                           [bk(mT[t]), wk(w)], [gk(g)])
                    f = fpool.get()
                    if part == 0:
                        headnorm(psg[g][:, :], gk(g), 4, 128, knc_rep, "knc_rep",
                                 [(f32p[:, f, :].rearrange("p (h d) -> p h d", h=4), fk(f))])
                        gpool.put(g)
                        bq = bpool.get()
                        cp("act", bfp[:, bq, 0:512], f32p[:, f, :], [fk(f)], [bk(bq)])
                        transposes_to(kcT[:, 0, :, t * 128:(t + 1) * 128], bfp[:, bq, 0:512], 4, [bk(bq)], ["kv0k"])
                        bpool.put(bq)
                    else:
                        cp("dve", f32p[:, f, :], psg[g][:, :], [gk(g)], [fk(f)])
                        cp("act", vc[:, 0, t, :], psg[g][:, :], [gk(g)], ["kv0v"])
                        gpool.put(g)
                    dma("q_sp", memkv[sq, t * 128:(t + 1) * 128, part, :, :],
                        f32p[:, f, :].rearrange("p (h d) -> p h d", h=4), [fk(f)], ["memkv"])
                    fpool.put(f)
                wpool.put(w)
            for m in mT:
                bpool.put(m)

        pending_copies = [(gi, b) for b in range(NSB) for gi in range(3)]

        def issue_copies(n):
            for _ in range(n):
                if not pending_copies:
                    return
                gi, b = pending_copies.pop(0)
                Wb = WIN[gi]
                dma("q_act", ws[gi][b, 0:Wb - LS], cw[gi][b, LS:Wb], (), ["wsc%d_%d" % (gi, b)])

        def prompt_seq(sq):
            mem_kv_prompt(sq)
            mset("pool", S32[:, 0, :, :], 0.0, ["S0"])
            mset("pool", Sbf[:, 0, :, :], 0.0, ["S0bf"])
            for st in range(DBG["nst"]):
                tile0 = st * 4
                pre = state.pop("pre", None)
                if pre is None:
                    pre = [front_a(xp[sq, (tile0 + i) * 128:(tile0 + i + 1) * 128, :], nmix_rep[:], "nmix_rep")
                           for i in range(4)]
                for r in state.pop("zero_rest", []):
                    dma("q_sp", xsc[r * 512:(r + 1) * 512, :], xsc[0:512, :], ["xsc_z0"], ["xsc_z%d" % r])
                for i in range(4):
                    front_b(pre[i], xnT[:, :, i * 128:(i + 1) * 128], "xnT")

                def prefetch(sq=sq, st=st):
                    state["more_tiles"] = (st + 1 < DBG["nst"]) or (sq + 1 < DBG["nseq"]) or DBG["sample"]
                    if st + 1 < DBG["nst"]:
                        nsq, nt0 = sq, (st + 1) * 4
                    elif sq + 1 < DBG["nseq"]:
                        nsq, nt0 = sq + 1, 0
                    else:
                        if DBG["sample"]:
                            state["pre"] = [front_a(xsm[:, :], nmix_rep[:], "nmix_rep")]
                        return
                    state["pre"] = [front_a(xp[nsq, (nt0 + i) * 128:(nt0 + i + 1) * 128, :], nmix_rep[:],
                                            "nmix_rep") for i in range(4)]

                def out_kv(kind, gi, i, f32view, key, tile0=tile0):
                    pos0 = (tile0 + i) * 128
                    lo = SEQ - WIN[gi]
                    if pos0 >= lo:
                        dma("q_sp", wp[gi][sq, pos0 - lo:pos0 - lo + 128, kind, :, :],
                            f32view.rearrange("p (h d) -> p h d", h=4), [key], ["wp%d" % gi])

                project_supertile(4, tile0, True, out_kv,
                                  (lambda i, bT: cross_tile(i * 128, bT, [(0, "kv0", 0, 128)])) if "cross" in DBG["mix"]
                                  else (lambda i, bT: None))
                issue_copies(6)

                def s_loader(bi, phase):
                    return 0, "S0"

                def s_store(bi, slot, skey):
                    cp("act", Sbf[:, 0, :, :], S32[:, 0, :, :], ["S0"], ["S0bf"])

                threads = []
                if "attn" in DBG["mix"]:
                    threads.append(chain([(lambda i=i: attn_prompt_tile(tile0 + i, i * 128)) for i in range(4)]))
                if "gla" in DBG["mix"]:
                    threads.append(chain([(lambda i=i: gla_tile(i, i * 128, tri_p[:], "tri_p", blk_p, "blk_p", 1,
                                                                s_loader, s_store)) for i in range(4)]))
                run_threads(threads)
                wpool.put(state["w_qkb"])
                if DBG["merge"]:
                    merge_supertile(4, lambda i, tile0=tile0: xp[sq, (tile0 + i) * 128:(tile0 + i + 1) * 128, :], 0,
                                    prefetch)
            dma("q_sp", glap[sq].rearrange("(pr hh) dk dv -> (hh dk) pr dv", hh=2), S32[:, 0, :, :], ["S0"], ["glap"])

        def sample_tile():
            issue_copies(1000)
            pre = state.pop("pre", None)
            if pre is None:
                pre = [front_a(xsm[:, :], nmix_rep[:], "nmix_rep")]
            front_b(pre[0], xnT[:, :, 0:128], "xnT")

            def out_kv(kind, gi, i, f32view, key):
                Wb = WIN[gi]
                for b in range(NSB):
                    dma("q_sp", ws[gi][b, Wb - LS:Wb, kind, :, :],
                        f32view[b * LS:(b + 1) * LS, :].rearrange("p (h d) -> p h d", h=4), [key],
                        ["wsn%d_%d_%d" % (gi, b, kind)])

            project_supertile(1, 0, False, out_kv, lambda i, bT: cross_sample(bT))
            mset("dve", psN[0:64, :], 0.0, ["psN"])
            mset("dve", psL[0:64, :], 0.0, ["psL"])
            blocks = []
            for h in range(4):
                hp, pr = (h % 2) * 64, h // 2
                for gi in range(3):
                    ch = gi * 2 + pr
                    blocks.append(dict(
                        mask=maskS[:, gi, :], mkey="maskS", kkey="kAT", qkey="qAT", vkey="vA", ncol=128,
                        sub=[(kAT[hp:hp + 64, ch, 0:128], qAT[hp:hp + 64, ch, 0:128], 0, 128)],
                        pv=[(vA[:, 0, (gi * 4 + h) * 64:(gi * 4 + h + 1) * 64], 0, 128,
                             psN[0:64, h * 128:(h + 1) * 128], psL[0:64, h * 128:(h + 1) * 128])]))
            attn_blocks(blocks, None)
            ld = 0
            for b in range(NSB):
                for gi in range(3):
                    nkt_all = WIN[gi] // 128
                    for k0 in range(0, nkt_all, 4):
                        nkt = min(4, nkt_all - k0)
                        slot = ld % 2
                        ld += 1
                        ckey = "cache%d" % slot
                        dma("q_pool", kcache[:, slot, 0:nkt, :],
                            cw[gi][b, k0 * 128:(k0 + nkt) * 128, 0].rearrange("(kt p) h d -> p kt (h d)", p=128),
                            (), [ckey + "k"])
                        dma("q_pool", vcache[:, slot, 0:nkt, :],
                            cw[gi][b, k0 * 128:(k0 + nkt) * 128, 1].rearrange("(kt p) h d -> p kt (h d)", p=128),
                            (), [ckey + "v"])
                        t = trpool.get()
                        for kt in range(nkt):
                            for pr in range(2):
                                tr(pstr[t][:, (kt * 2 + pr) * 128:(kt * 2 + pr + 1) * 128],
                                   kcache[:, slot, kt, pr * 128:(pr + 1) * 128], [ckey + "k"], [tk(t)])
                        cp("dve", kcTA[:, slot, 0:nkt, :, :],
                           pstr[t][:, 0:nkt * 256].rearrange("p (kt pr c) -> p kt pr c", kt=nkt, pr=2),
                           [tk(t)], [ckey + "kT"])
                        trpool.put(t)
                        blocks = []
                        for kt in range(nkt):
                            typ = 0 if (k0 + kt) == 0 else 1
                            sub, pv = [], []
                            for ci, h in enumerate((0, 2, 1, 3)):
                                hp, pr = (h % 2) * 64, h // 2
                                sub.append((kcTA[hp:hp + 64, slot, kt, pr, :],
                                            qAT[hp:hp + 64, gi * 2 + pr, b * LS:(b + 1) * LS], ci * 8, 8))
                                pv.append((vcache[:, slot, kt, h * 64:(h + 1) * 64], ci * 8, 8,
                                           psN[0:64, h * 128 + b * LS:h * 128 + (b + 1) * LS],
                                           psL[0:64, h * 128 + b * LS:h * 128 + (b + 1) * LS]))
                            blocks.append(dict(mask=maskC[:, 2 * gi + typ, :, :].rearrange("p h l -> p (h l)"),
                                               mkey="maskC", kkey=ckey + "kT", qkey="qAT", vkey=ckey + "v", ncol=32,
                                               sub=sub, pv=pv))
                        attn_blocks(blocks, None)
            attn_finish(0)
            def s_loader(bi, phase):
                slot = bi % 2
                key = "S%d" % slot
                src = sgla[bi].rearrange("(pr hh) dk dv -> (hh dk) pr dv", hh=2)
                if phase == "o":
                    dma("q_pool", Sbf[:, slot, :, :], src, (), [key + "bf"])
                else:
                    dma("q_sp", S32[:, slot, :, :], src, (), [key])
                return slot, key

            def s_store(bi, slot, skey):
                dma("q_sp", glas[bi].rearrange("(pr hh) dk dv -> (hh dk) pr dv", hh=2), S32[:, slot, :, :], [skey],
                    ["glas"])

            for _ in gla_tile(0, 0, tri_s[:], "tri_s", blk_s, "blk_s", NSB, s_loader, s_store):
                pass
            wpool.put(state["w_qkb"])
            merge_supertile(1, lambda i: xsm[:, :], 0)

        def cross_sample(qc_b):
            goc = gpool.get()
            gl = gpool.get()
            for b in range(NSB):
                slot = b % 2
                key = "kvs%d" % slot
                bk_ = bpool.get()
                dma("q_pool", bfp[:, bk_, :].rearrange("p (blk c) -> p blk c", blk=2),
                    cmem[b, :, 0].rearrange("(blk p) h d -> p blk (h d)", p=128), (), [bk(bk_)])
                dma("q_pool", vc[:, slot, :, :], cmem[b, :, 1].rearrange("(blk p) h d -> p blk (h d)", p=128), (),
                    [key + "v"])
                t = trpool.get()
                for blk_i in range(2):
                    for h in range(4):
                        tr(pstr[t][:, (h * 2 + blk_i) * 128:(h * 2 + blk_i + 1) * 128],
                           bfp[:, bk_, blk_i * 512 + h * 128:blk_i * 512 + (h + 1) * 128], [bk(bk_)], [tk(t)])
                cp("dve", kcT[:, slot, :, :], pstr[t][:, :].rearrange("p (h n) -> p h n", h=4), [tk(t)], [key + "k"])
                trpool.put(t)
                bpool.put(bk_)
                c0, cn = b * LS, LS
                g = gpool.get()
                for h in range(4):
                    for blk_i in range(2):
                        col = (h * 2 + blk_i) * 8
                        mm(psg[g][:, col:col + 8], kcT[:, slot, h, blk_i * 128:(blk_i + 1) * 128],
                           bfp[:, qc_b, h * 128 + c0:h * 128 + c0 + cn], True, True, [key + "k", bk(qc_b)], [gk(g)])
                pb = bpool.get()
                act(bfp[:, pb, 0:64], psg[g][:, 0:64], AF.Exp, [gk(g)], [bk(pb)])
                gpool.put(g)
                for h in range(4):
                    for blk_i in range(2):
                        col = (h * 2 + blk_i) * 8
                        mm(psg[goc][:, h * 128 + c0:h * 128 + c0 + cn], vc[:, slot, blk_i, h * 128:(h + 1) * 128],
                           bfp[:, pb, col:col + 8], blk_i == 0, blk_i == 1,
                           [key + "v", bk(pb)], [gk(goc)])
                        mm(psg[gl][:, h * 128 + c0:h * 128 + c0 + cn], ones_bf[:, :], bfp[:, pb, col:col + 8],
                           blk_i == 0, blk_i == 1, ["ones_bf", bk(pb)], [gk(gl)])
                bpool.put(pb)
            f = fpool.get()
            recip(f32p[:, f, :], psg[gl][:, :], [gk(gl)], [fk(f)])
            gpool.put(gl)
            tt("dve", ocT[:, :, 0:128], psg[goc][:, :].rearrange("p (h t) -> p h t", h=4),
               f32p[:, f, :].rearrange("p (h t) -> p h t", h=4), ALU.mult, [gk(goc), fk(f)], ["ocT"])
            gpool.put(goc)
            fpool.put(f)

        def experts_phase():
            XeT = kAT[:, 0:4, :].rearrange("p a (k c) -> p (a k) c", c=512)
            Wv = vA[:, :, :].rearrange("p a c -> p (a c)")

            def views(e_):
                sl = e_ % 2
                base = sl * 6144
                return (sl, Wv[:, base:base + 2048].rearrange("p (k f) -> p k f", k=8),
                        Wv[:, base + 2048:base + 4096].rearrange("p (k f) -> p k f", k=8),
                        Wv[:, base + 4096:base + 6144].rearrange("p (k c) -> p k c", k=2))

            def loads_w(e_):
                sl, wg_v, wu_v, wd_v = views(e_)
                wkey = "ew%d" % sl
                dma("q_pool", wg_v, w_eg[e_].rearrange("(k p) f -> p k f", p=128), (), [wkey + "g"])
                dma("q_pool", wu_v, w_eu[e_].rearrange("(k p) f -> p k f", p=128), (), [wkey + "u"])
                dma("q_pool", wd_v, w_ed[e_].rearrange("(k p) c -> p k c", p=128), (), [wkey + "d"])

            def loads_x(e_):
                xs_slots = []
                for sb_ in range(4):
                    bx = bpool.get()
                    dma("q_sp", bfp[:, bx, :], xsc[e_ * CAP + sb_ * 128:e_ * CAP + (sb_ + 1) * 128, :], ["xsc"],
                        [bk(bx)])
                    xs_slots.append(bx)
                return xs_slots

            def tstage(e_, xs_slots):
                sl, wg_v, wu_v, wd_v = views(e_)
                xkey = "xe%d" % sl
                for sb_, bx in enumerate(xs_slots):
                    t = trpool.get()
                    for k in range(8):
                        tr(pstr[t][:, k * 128:(k + 1) * 128], bfp[:, bx, k * 128:(k + 1) * 128], [bk(bx)], [tk(t)])
                    cp("dve" if sb_ % 2 == 0 else "act", XeT[:, sl * 8:(sl + 1) * 8, sb_ * 128:(sb_ + 1) * 128],
                       pstr[t][:, :].rearrange("p (k c) -> p k c", k=8), [tk(t)], [xkey])
                    trpool.put(t)
                    bpool.put(bx)

            def compute_h(e_):
                sl, wg_v, wu_v, wd_v = views(e_)
                wkey = "ew%d" % sl
                xkey = "xe%d" % sl
                hb = bpool.get()
                for fc in range(2):
                    gg = gpool.get()
                    gu = gpool.get()
                    for k in range(8):
                        mm(psg[gg][:, :], wg_v[:, k, fc * 128:(fc + 1) * 128], XeT[:, sl * 8 + k, :], k == 0, k == 7,
                           [wkey + "g", xkey], [gk(gg)])
                    for k in range(8):
                        mm(psg[gu][:, :], wu_v[:, k, fc * 128:(fc + 1) * 128], XeT[:, sl * 8 + k, :], k == 0, k == 7,
                           [wkey + "u", xkey], [gk(gu)])
                    f = fpool.get()
                    act(f32p[:, f, :], psg[gg][:, :], AF.Silu, [gk(gg)], [fk(f)])
                    gpool.put(gg)
                    tt("dve", bfp[:, hb, fc * 512:(fc + 1) * 512], psg[gu][:, :], f32p[:, f, :], ALU.mult,
                       [gk(gu), fk(f)], [bk(hb)])
                    gpool.put(gu)
                    fpool.put(f)
                return hb

            def compute_y(e_, hb):
                sl, wg_v, wu_v, wd_v = views(e_)
                wkey = "ew%d" % sl
                for sb_ in range(4):
                    x = xpool.get()
                    for hf in range(2):
                        g = gpool.get()
                        for fc in range(2):
                            mm(psg[g][:, :], bfp[:, hb, fc * 512 + sb_ * 128:fc * 512 + (sb_ + 1) * 128],
                               wd_v[:, fc, hf * 512:(hf + 1) * 512], fc == 0, fc == 1, [bk(hb), wkey + "d"], [gk(g)])
                        if hf == 0:
                            cp("act", xpl[:, x, 0:512], psg[g][:, :], [gk(g)], [xk(x)])
                        else:
                            cp("dve", xpl[:, x, 512:1024], psg[g][:, :], [gk(g)], [xk(x)])
                        gpool.put(g)
                    dma("q_sp", ysc[e_ * CAP + sb_ * 128:e_ * CAP + (sb_ + 1) * 128, :], xpl[:, x, :], [xk(x)],
                        ["ysc"])
                    xpool.put(x)
                bpool.put(hb)

            loads_w(0)
            tstage(0, loads_x(0))
            nxt = loads_x(1)
            hb_cur = compute_h(0)
            for e_ in range(NEXP):
                hb_next = None
                if e_ + 1 < NEXP:
                    loads_w(e_ + 1)
                    tstage(e_ + 1, nxt)
                    if e_ + 2 < NEXP:
                        nxt = loads_x(e_ + 2)
                    hb_next = compute_h(e_ + 1)
                compute_y(e_, hb_cur)
                hb_cur = hb_next

        def combine_phase():
            npair = NF // 2
            ntl = state["tile_no"]

            def loads(tn):
                x = xpool.get()
                dma("q_sp", xpl[:, x, :], h2s[tn * 128:(tn + 1) * 128, :], ["h2s"], [xk(x)])
                ys = []
                for j in range(2):
                    pi = (tn * 2 + j) % npair
                    yv = f32p[:, 2 * pi:2 * pi + 2, :].rearrange("p a c -> p (a c)")
                    keys = [fk(2 * pi), fk(2 * pi + 1)]
                    S.op("q_pool", lambda e, tn=tn, j=j, yv=yv: e.indirect_dma_start(
                        out=yv, out_offset=None, in_=ysc[:, :],
                        in_offset=bass.IndirectOffsetOnAxis(ap=dsti[:, tn, j:j + 1], axis=0)),
                        ["ysc", "dsti"], keys)
                    ys.append((yv, keys))
                return x, ys

            def finish(tn, x, ys):
                for j, (yv, keys) in enumerate(ys):
                    stt(xpl[:, x, :], yv, wts[:, tn, j:j + 1], xpl[:, x, :], ALU.mult, ALU.add,
                        keys + ["wts", xk(x)], [xk(x)])
                if tn < NPS * 16:
                    sq, tt_ = tn // 16, tn % 16
                    dst = yp[sq, tt_ * 128:(tt_ + 1) * 128, :]
                else:
                    dst = ysm[:, :]
                dma("q_sp", dst, xpl[:, x, :], [xk(x)], ["yout%d" % tn])
                xpool.put(x)

            if ntl == 0:
                return
            q = [loads(t) for t in range(min(2, ntl))]
            for tn in range(ntl):
                cur = q.pop(0)
                if tn + 2 < ntl:
                    q.append(loads(tn + 2))
                finish(tn, *cur)

        mset("pool", big1[:], 0.0, ["big1"])
        dma("q_act", xsc[0:512, :].rearrange("(p j) d -> p (j d)", j=4), big1[:].rearrange("p a c -> p (a c)"),
            ["big1"], ["xsc_z0"])
        nz = NEXP * CAP // 512
        for r in range(1, 8):
            dma("q_act", xsc[r * 512:(r + 1) * 512, :], xsc[0:512, :], ["xsc_z0"], ["xsc_z%d" % r])
        state["zero_rest"] = list(range(8, nz))
        for sq in range(DBG["nseq"]):
            prompt_seq(sq)
        if DBG["sample"]:
            S.barrier(lambda e: e.memset(eps_c[:], EPS))
            sample_tile()
        if DBG["moe"]:
            S.barrier(lambda e: e.memset(eps_c[:], EPS))
            experts_phase()
            S.barrier(lambda e: e.memset(eps_c[:], EPS))
            combine_phase()
        if DBG.get("dump"):
            DBG["_in_dump"] = True
            DBG["dump"](locals())
        S.emit(nc)
    return nc


_PROG = {}


def _get_prog():
    if "nc" not in _PROG:
        _PROG["nc"] = build_program()
    return _PROG["nc"]


def kernel(x_prompt, x_sample, mem_prompt, cache_win1, cache_win2, cache_win3, state_gla, cache_mem,
           norm_mix, w_in, qn_a, kn_a, qn_c, kn_c, gla_gate_up, gla_gate_bias, gla_norm, mem_norm,
           w_mem_kv, w_branch_a, w_branch_b, w_branch_c, w_out, norm_ffn, w_router_group, b_router_group,
           w_router_expert, b_router_expert, w_exp_gate, w_exp_up, w_exp_down):
    nc = _get_prog()
    f = lambda a: np.ascontiguousarray(np.asarray(a, dtype=np.float32))
    shared = {
        "norm_mix": f(norm_mix), "w_in": f(w_in), "qn_a": f(qn_a), "kn_a": f(kn_a), "qn_c": f(qn_c), "kn_c": f(kn_c),
        "gla_gate_up": f(gla_gate_up), "gla_gate_bias": f(gla_gate_bias), "gla_norm": f(gla_norm),
        "mem_norm": f(mem_norm), "w_mem_kv": f(w_mem_kv), "w_branch_a": f(w_branch_a), "w_branch_b": f(w_branch_b),
        "w_branch_c": f(w_branch_c), "w_out": f(w_out), "norm_ffn": f(norm_ffn), "w_router_group": f(w_router_group),
        "b_router_group": f(b_router_group), "w_router_expert": f(w_router_expert),
        "b_router_expert": f(b_router_expert), "w_exp_gate": f(w_exp_gate), "w_exp_up": f(w_exp_up),
        "w_exp_down": f(w_exp_down),
    }
    x_prompt, x_sample, mem_prompt = f(x_prompt), f(x_sample), f(mem_prompt)
    cache_win1, cache_win2, cache_win3 = f(cache_win1), f(cache_win2), f(cache_win3)
    state_gla, cache_mem = f(state_gla), f(cache_mem)
    in_maps = []
    for c in range(NCORES):
        m = dict(shared)
        m["xp"] = x_prompt[c * NPS:(c + 1) * NPS]
        m["xs"] = x_sample[c * NSB:(c + 1) * NSB].reshape(128, D)
        m["memp"] = mem_prompt[c * NPS:(c + 1) * NPS]
        m["cw1"] = cache_win1[c * NSB:(c + 1) * NSB]
        m["cw2"] = cache_win2[c * NSB:(c + 1) * NSB]
        m["cw3"] = cache_win3[c * NSB:(c + 1) * NSB]
        m["sgla"] = state_gla[c * NSB:(c + 1) * NSB]
        m["cmem"] = cache_mem[c * NSB:(c + 1) * NSB]
        in_maps.append(m)
    ncr = DBG["ncores"]
    res = run_bass_kernel_spmd(nc, in_maps[:ncr], core_ids=list(range(ncr)))
    R = res.results
    cat = lambda k: np.concatenate([np.asarray(r[k]) for r in R], axis=0)
    y_prompt = cat("yp")
    y_sample = cat("ys").reshape(NCORES * NSB, LS, D)
    return (y_prompt, y_sample, cat("w1p"), cat("w2p"), cat("w3p"), cat("glap"), cat("memkv"),
            cat("w1s"), cat("w2s"), cat("w3s"), cat("glas"))
```
